# Optimizing a Trainium2 kernel written in Bass

```python
import math
import jax, jax.numpy as jnp
from jax import lax
import numpy as np

D_MODEL = 1024
BATCH = 4
SEQ = 8192
DEPTH = 2

DN_ALPHA = (2 * DEPTH) ** 0.25
DN_BETA = (8 * DEPTH) ** -0.25
N_EVEN = (DEPTH + 1) // 2
N_ODD = DEPTH // 2

CHUNK = 64
CONV_K = 4
LN_EPS = 1e-5
RMS_EPS = 1e-6
D_FF = 256 * ((8 * D_MODEL // 3 + 255) // 256)

GDN_HEADS = 8
GDN_DK = 64
GDN_DV = 64
MLSTM_HEADS = 4
MLSTM_DK = 64
MLSTM_DV = 128
HYB_WIDTH = GDN_HEADS * GDN_DV + MLSTM_HEADS * MLSTM_DV
GDN_CONV_DIM = 2 * GDN_HEADS * GDN_DK + GDN_HEADS * GDN_DV
HYB_SPLITS = (GDN_HEADS * GDN_DK, GDN_HEADS * GDN_DK, GDN_HEADS * GDN_DV, GDN_HEADS * GDN_DV, GDN_HEADS, GDN_HEADS,
              MLSTM_HEADS * MLSTM_DK, MLSTM_HEADS * MLSTM_DK, MLSTM_HEADS * MLSTM_DV, MLSTM_HEADS * MLSTM_DV,
              MLSTM_HEADS, MLSTM_HEADS)
HYB_IN = sum(HYB_SPLITS)

SSD_D_INNER = 2 * D_MODEL
SSD_HEADDIM = 64
SSD_HEADS = SSD_D_INNER // SSD_HEADDIM
SSD_GROUPS = 4
SSD_HPG = SSD_HEADS // SSD_GROUPS
SSD_STATE = 128
SSD_CONV_DIM = SSD_D_INNER + 2 * SSD_GROUPS * SSD_STATE
SSD_IN = SSD_D_INNER + SSD_CONV_DIM + SSD_HEADS

kernel_name = 'hybrid_gdn_mlstm_ssd_macaron_deepnorm'


def _split(t, sizes):
    return jnp.split(t, np.cumsum(sizes)[:-1].tolist(), axis=-1)


def _layer_norm(x, g, b):
    xf = x.astype(jnp.float32)
    xc = xf - jnp.mean(xf, -1, keepdims=True)
    var = jnp.mean(xc * xc, -1, keepdims=True)
    return (xc * lax.rsqrt(var + LN_EPS) * g.astype(jnp.float32) + b.astype(jnp.float32)).astype(x.dtype)


def _rms_norm(x, w):
    xf = x.astype(jnp.float32)
    return xf * lax.rsqrt(jnp.mean(xf * xf, -1, keepdims=True) + RMS_EPS) * w.astype(jnp.float32)


def _l2_normalize(t):
    return t * lax.rsqrt(jnp.sum(t * t, -1, keepdims=True) + 1e-6)


def _causal_dwconv(x, w):
    return lax.conv_general_dilated(x, w[:, None, :].astype(x.dtype), window_strides=(1,),
                                    padding=[(CONV_K - 1, 0)], dimension_numbers=('NWC', 'WIO', 'NWC'),
                                    feature_group_count=x.shape[-1])


def _swiglu(x, w_gate, w_up, w_down):
    return (jax.nn.silu(x @ w_gate) * (x @ w_up)) @ w_down


def _to_chunks(t):
    b, s = t.shape[:2]
    t = t.reshape(b, s // CHUNK, CHUNK, *t.shape[2:])
    return jnp.moveaxis(t, 3, 1)


def _from_chunks(t):
    t = jnp.moveaxis(t, 1, 3)
    return t.reshape(t.shape[0], t.shape[1] * t.shape[2], *t.shape[3:])


def _gated_delta_rule(q, k, v, g, beta):
    bsz, s, nh, dk = q.shape
    dv = v.shape[-1]
    q, k, v, g, beta = map(_to_chunks, (q, k, v, g, beta))
    causal = jnp.tril(jnp.ones((CHUNK, CHUNK), bool))
    strict = jnp.tril(jnp.ones((CHUNK, CHUNK), bool), -1)
    gc = jnp.cumsum(g, axis=-1)
    decay = jnp.exp(jnp.where(causal, gc[..., :, None] - gc[..., None, :], -jnp.inf))
    kb = k * beta[..., None]
    lower = jnp.where(strict, jnp.einsum('bhncd,bhnjd->bhncj', kb, k) * decay, 0.0)
    rhs = jnp.concatenate([v * beta[..., None], kb * jnp.exp(gc)[..., None]], axis=-1)
    sol = lax.linalg.triangular_solve(lower + jnp.eye(CHUNK, dtype=lower.dtype), rhs, left_side=True, lower=True)
    u_base, w_dec = sol[..., :dv], sol[..., dv:]
    attn = jnp.einsum('bhncd,bhnjd->bhncj', q, k) * decay
    q_dec = q * jnp.exp(gc)[..., None]
    k_dec = k * jnp.exp(gc[..., -1:] - gc)[..., None]
    g_last = jnp.exp(gc[..., -1])
    xs = tuple(jnp.moveaxis(t, 2, 0) for t in (u_base, w_dec, attn, q_dec, k_dec, g_last))

    def step(state, inp):
        u_b, w_c, a_c, q_c, k_c, gl_c = inp
        u = u_b - jnp.einsum('bhcd,bhde->bhce', w_c, state)
        o = jnp.einsum('bhcd,bhde->bhce', q_c, state) + jnp.einsum('bhcj,bhje->bhce', a_c, u)
        state = gl_c[..., None, None] * state + jnp.einsum('bhcd,bhce->bhde', k_c, u)
        return state, o

    _, o = lax.scan(step, jnp.zeros((bsz, nh, dk, dv), q.dtype), xs)
    return _from_chunks(jnp.moveaxis(o, 0, 2))


def _mlstm_chunkwise(q, k, v, log_i, log_f):
    bsz, s, nh, dk = q.shape
    dv = v.shape[-1]
    q, k, v, log_i, log_f = map(_to_chunks, (q, k, v, log_i, log_f))
    causal = jnp.tril(jnp.ones((CHUNK, CHUNK), bool))
    b = jnp.cumsum(log_f, axis=-1)
    d_log = jnp.where(causal, b[..., :, None] - b[..., None, :] + log_i[..., None, :], -jnp.inf)
    m_intra = jnp.max(d_log, axis=-1)
    s_qk = jnp.einsum('bhncd,bhnjd->bhncj', q, k) * jnp.exp(d_log - m_intra[..., None])
    num_intra = jnp.einsum('bhncj,bhnje->bhnce', s_qk, v)
    den_intra = jnp.sum(s_qk, axis=-1)
    w_end = b[..., -1:] - b + log_i
    m_end = jnp.max(w_end, axis=-1)
    k_end = k * jnp.exp(w_end - m_end[..., None])[..., None]
    c_end = jnp.einsum('bhncd,bhnce->bhnde', k_end, v)
    n_end = jnp.sum(k_end, axis=-2)
    b_last = b[..., -1]
    xs = tuple(jnp.moveaxis(t, 2, 0) for t in (q, b, m_intra, num_intra, den_intra, b_last, m_end, c_end, n_end))

    def step(carry, inp):
        c_st, n_st, m_st = carry
        q_c, b_c, mi_c, num_c, den_c, bl_c, me_c, ce_c, ne_c = inp
        a = b_c + m_st[..., None]
        m_t = jnp.maximum(a, mi_c)
        ea, ei = jnp.exp(a - m_t), jnp.exp(mi_c - m_t)
        num = ea[..., None] * jnp.einsum('bhcd,bhde->bhce', q_c, c_st) + ei[..., None] * num_c
        den = ea * jnp.einsum('bhcd,bhd->bhc', q_c, n_st) + ei * den_c
        h_c = num / jnp.maximum(jnp.abs(den), jnp.exp(-m_t))[..., None]
        m_new = jnp.maximum(bl_c + m_st, me_c)
        sc, se = jnp.exp(bl_c + m_st - m_new), jnp.exp(me_c - m_new)
        c_st = sc[..., None, None] * c_st + se[..., None, None] * ce_c
        n_st = sc[..., None] * n_st + se[..., None] * ne_c
        return (c_st, n_st, m_new), h_c

    init = (jnp.zeros((bsz, nh, dk, dv), q.dtype), jnp.zeros((bsz, nh, dk), q.dtype), jnp.zeros((bsz, nh), q.dtype))
    _, hs = lax.scan(step, init, xs)
    return _from_chunks(jnp.moveaxis(hs, 0, 2))


def _ssd_chunked(xh, dt, a, bm, cm):
    bsz, s = xh.shape[:2]
    n = s // CHUNK
    xh = xh.reshape(bsz, n, CHUNK, *xh.shape[2:])
    dt = dt.reshape(bsz, n, CHUNK, *dt.shape[2:])
    bm = bm.reshape(bsz, n, CHUNK, *bm.shape[2:])
    cm = cm.reshape(bsz, n, CHUNK, *cm.shape[2:])
    causal = jnp.tril(jnp.ones((CHUNK, CHUNK), bool))
    a_cum = jnp.cumsum(jnp.moveaxis(dt * a, 2, -1), axis=-1)
    xdt = xh * dt[..., None]
    cb = jnp.einsum('bncgs,bnjgs->bngcj', cm, bm)
    seg = jnp.exp(jnp.where(causal, a_cum[..., :, None] - a_cum[..., None, :], -jnp.inf))
    y_diag = jnp.einsum('bngcj,bngrcj,bnjgrp->bncgrp', cb, seg, xdt)
    e_cum = jnp.exp(a_cum)
    d_end = jnp.exp(a_cum[..., -1:] - a_cum)
    g_last = e_cum[..., -1]
    xs = tuple(jnp.moveaxis(t, 1, 0) for t in (cm, bm, xdt, e_cum, d_end, g_last))

    def step(h, inp):
        c_c, b_c, x_c, ec_c, de_c, gl_c = inp
        y_off = jnp.einsum('bcgs,bgrps,bgrc->bcgrp', c_c, h, ec_c)
        h = gl_c[..., None, None] * h + jnp.einsum('bjgs,bgrj,bjgrp->bgrps', b_c, de_c, x_c)
        return h, y_off

    h0 = jnp.zeros((bsz, SSD_GROUPS, SSD_HPG, SSD_HEADDIM, SSD_STATE), xh.dtype)
    _, y_off = lax.scan(step, h0, xs)
    y = y_diag + jnp.moveaxis(y_off, 0, 1)
    return y.reshape(bsz, s, SSD_GROUPS, SSD_HPG, SSD_HEADDIM)


def _hybrid_mixer(x, w_in, conv_w, a_log, dt_bias, norm_w, i_bias, f_bias, w_out):
    bsz, s, _ = x.shape
    f32 = jnp.float32
    gq, gk, gv, gz, gb, ga, mq, mk, mv, mo, mi, mf = _split(x @ w_in, HYB_SPLITS)
    qkv = jax.nn.silu(_causal_dwconv(jnp.concatenate([gq, gk, gv], -1), conv_w)).astype(f32)
    gq, gk, gv = _split(qkv, (GDN_HEADS * GDN_DK, GDN_HEADS * GDN_DK, GDN_HEADS * GDN_DV))
    gq = _l2_normalize(gq.reshape(bsz, s, GDN_HEADS, GDN_DK)) * GDN_DK ** -0.5
    gk = _l2_normalize(gk.reshape(bsz, s, GDN_HEADS, GDN_DK))
    gv = gv.reshape(bsz, s, GDN_HEADS, GDN_DV)
    beta = jax.nn.sigmoid(gb.astype(f32))
    g = -jnp.exp(a_log.astype(f32)) * jax.nn.softplus(ga.astype(f32) + dt_bias.astype(f32))
    o_a = _gated_delta_rule(gq, gk, gv, g, beta)
    o_a = _rms_norm(o_a, norm_w) * jax.nn.silu(gz.astype(f32)).reshape(bsz, s, GDN_HEADS, GDN_DV)
    mq = mq.astype(f32).reshape(bsz, s, MLSTM_HEADS, MLSTM_DK)
    mk = mk.astype(f32).reshape(bsz, s, MLSTM_HEADS, MLSTM_DK) * MLSTM_DK ** -0.5
    mv = mv.astype(f32).reshape(bsz, s, MLSTM_HEADS, MLSTM_DV)
    log_i = mi.astype(f32) + i_bias.astype(f32)
    log_f = jax.nn.log_sigmoid(mf.astype(f32) + f_bias.astype(f32))
    h = _mlstm_chunkwise(mq, mk, mv, log_i, log_f)
    o_b = jax.nn.sigmoid(mo.astype(f32)).reshape(bsz, s, MLSTM_HEADS, MLSTM_DV) * h
    y = jnp.concatenate([o_a.reshape(bsz, s, -1), o_b.reshape(bsz, s, -1)], axis=-1).astype(x.dtype)
    return y @ w_out


def _ssd_mixer(x, w_in, conv_w, conv_b, a_log, dt_bias, d_skip, norm_w, w_out):
    bsz, s, _ = x.shape
    f32 = jnp.float32
    z, xbc, dt = _split(x @ w_in, (SSD_D_INNER, SSD_CONV_DIM, SSD_HEADS))
    xbc = jax.nn.silu(_causal_dwconv(xbc, conv_w) + conv_b).astype(f32)
    xs, bm, cm = _split(xbc, (SSD_D_INNER, SSD_GROUPS * SSD_STATE, SSD_GROUPS * SSD_STATE))
    xs = xs.reshape(bsz, s, SSD_GROUPS, SSD_HPG, SSD_HEADDIM)
    dt = jax.nn.softplus(dt.astype(f32) + dt_bias.astype(f32)).reshape(bsz, s, SSD_GROUPS, SSD_HPG)
    a = -jnp.exp(a_log.astype(f32)).reshape(SSD_GROUPS, SSD_HPG)
    y = _ssd_chunked(xs, dt, a, bm.reshape(bsz, s, SSD_GROUPS, SSD_STATE), cm.reshape(bsz, s, SSD_GROUPS, SSD_STATE))
    y = y + d_skip.astype(f32).reshape(SSD_GROUPS, SSD_HPG)[..., None] * xs
    y = y.reshape(bsz, s, SSD_D_INNER) * jax.nn.silu(z.astype(f32))
    y = _rms_norm(y.reshape(bsz, s, SSD_GROUPS, SSD_D_INNER // SSD_GROUPS), norm_w.reshape(SSD_GROUPS, -1))
    return y.reshape(bsz, s, SSD_D_INNER).astype(x.dtype) @ w_out


def setup_inputs(seed: int = 0) -> dict:
    key = jax.random.key(seed)
    keys = iter(jax.random.split(key, 64))

    def nrm(shape, scale):
        return scale * jax.random.normal(next(keys), shape, jnp.float32)

    def unif(shape, lo, hi):
        return jax.random.uniform(next(keys), shape, jnp.float32, lo, hi)

    def gain(shape):
        return 1.0 + nrm(shape, 0.02)

    def dt_bias(shape):
        dt = jnp.exp(unif(shape, math.log(1e-3), math.log(1e-1)))
        return dt + jnp.log(-jnp.expm1(-dt))

    return {
        'x': nrm((BATCH, SEQ, D_MODEL), 1.0),
        'ffn_pre_w_gate': nrm((DEPTH, D_MODEL, D_FF), D_MODEL ** -0.5),
        'ffn_pre_w_up': nrm((DEPTH, D_MODEL, D_FF), D_MODEL ** -0.5),
        'ffn_pre_w_down': nrm((DEPTH, D_FF, D_MODEL), DN_BETA * D_FF ** -0.5),
        'ln_pre_g': gain((DEPTH, D_MODEL)),
        'ln_pre_b': nrm((DEPTH, D_MODEL), 0.02),
        'hyb_w_in': nrm((N_EVEN, D_MODEL, HYB_IN), D_MODEL ** -0.5),
        'hyb_conv_w': nrm((N_EVEN, CONV_K, GDN_CONV_DIM), CONV_K ** -0.5),
        'gdn_a_log': jnp.log(unif((N_EVEN, GDN_HEADS), 1.0, 16.0)),
        'gdn_dt_bias': dt_bias((N_EVEN, GDN_HEADS)),
        'gdn_norm_w': gain((N_EVEN, GDN_DV)),
        'mlstm_i_bias': nrm((N_EVEN, MLSTM_HEADS), 0.1),
        'mlstm_f_bias': jnp.linspace(3.0, 6.0, MLSTM_HEADS)[None, :] + nrm((N_EVEN, MLSTM_HEADS), 0.1),
        'hyb_w_out': nrm((N_EVEN, HYB_WIDTH, D_MODEL), DN_BETA * HYB_WIDTH ** -0.5),
        'ssd_w_in': nrm((N_ODD, D_MODEL, SSD_IN), D_MODEL ** -0.5),
        'ssd_conv_w': nrm((N_ODD, CONV_K, SSD_CONV_DIM), CONV_K ** -0.5),
        'ssd_conv_b': nrm((N_ODD, SSD_CONV_DIM), 0.02),
        'ssd_a_log': jnp.log(unif((N_ODD, SSD_HEADS), 1.0, 16.0)),
        'ssd_dt_bias': dt_bias((N_ODD, SSD_HEADS)),
        'ssd_d_skip': gain((N_ODD, SSD_HEADS)),
        'ssd_norm_w': gain((N_ODD, SSD_D_INNER)),
        'ssd_w_out': nrm((N_ODD, SSD_D_INNER, D_MODEL), DN_BETA * SSD_D_INNER ** -0.5),
        'ln_mix_g': gain((DEPTH, D_MODEL)),
        'ln_mix_b': nrm((DEPTH, D_MODEL), 0.02),
        'ffn_post_w_gate': nrm((DEPTH, D_MODEL, D_FF), D_MODEL ** -0.5),
        'ffn_post_w_up': nrm((DEPTH, D_MODEL, D_FF), D_MODEL ** -0.5),
        'ffn_post_w_down': nrm((DEPTH, D_FF, D_MODEL), DN_BETA * D_FF ** -0.5),
        'ln_post_g': gain((DEPTH, D_MODEL)),
        'ln_post_b': nrm((DEPTH, D_MODEL), 0.02),
    }


def reference(x, ffn_pre_w_gate, ffn_pre_w_up, ffn_pre_w_down, ln_pre_g, ln_pre_b,
              hyb_w_in, hyb_conv_w, gdn_a_log, gdn_dt_bias, gdn_norm_w, mlstm_i_bias, mlstm_f_bias, hyb_w_out,
              ssd_w_in, ssd_conv_w, ssd_conv_b, ssd_a_log, ssd_dt_bias, ssd_d_skip, ssd_norm_w, ssd_w_out,
              ln_mix_g, ln_mix_b, ffn_post_w_gate, ffn_post_w_up, ffn_post_w_down, ln_post_g, ln_post_b):
    for l in range(DEPTH):
        x = _layer_norm(DN_ALPHA * x + 0.5 * _swiglu(x, ffn_pre_w_gate[l], ffn_pre_w_up[l], ffn_pre_w_down[l]),
                        ln_pre_g[l], ln_pre_b[l])
        j = l // 2
        if l % 2 == 0:
            mix = _hybrid_mixer(x, hyb_w_in[j], hyb_conv_w[j], gdn_a_log[j], gdn_dt_bias[j], gdn_norm_w[j],
                                mlstm_i_bias[j], mlstm_f_bias[j], hyb_w_out[j])
        else:
            mix = _ssd_mixer(x, ssd_w_in[j], ssd_conv_w[j], ssd_conv_b[j], ssd_a_log[j], ssd_dt_bias[j],
                             ssd_d_skip[j], ssd_norm_w[j], ssd_w_out[j])
        x = _layer_norm(DN_ALPHA * x + mix, ln_mix_g[l], ln_mix_b[l])
        x = _layer_norm(DN_ALPHA * x + 0.5 * _swiglu(x, ffn_post_w_gate[l], ffn_post_w_up[l], ffn_post_w_down[l]),
                        ln_post_g[l], ln_post_b[l])
    return x
```

```python
import math
from contextlib import ExitStack

import numpy as np
import concourse.bass as bass
import concourse.mybir as mybir
from concourse.bass_utils import run_bass_kernel_spmd

F32 = mybir.dt.float32
BF16 = mybir.dt.bfloat16
ALU = mybir.AluOpType
AF = mybir.ActivationFunctionType
AX = mybir.AxisListType

D_MODEL = 1024
BATCH = 4
SEQ = 8192
DEPTH = 2
DN_ALPHA = (2 * DEPTH) ** 0.25
LN_EPS = 1e-5
D_FF = 2816
NFC = D_FF // 128
TS = 512
NT = TS // 128
WBLK = 2048
NSLOT = 5
NDS = 12
NBP = 256
BP_SSD_DTB, BP_SSD_ALOG, BP_SSD_D = 0, 32, 64
BP_G_ALOG, BP_G_DTB, BP_G_NW, BP_M_IB, BP_M_FB = 96, 104, 112, 176, 180
C_ID, C_ONES, C_MINC, C_MSTR, C_TRI = 0, 1, 2, 3, 4
RMS_EPS = 1e-6
BP_H_SGN, BP_H_BIAS = 192, 216

ENGS = ("sp", "act", "dve", "pe", "pool")


class Tile:
    __slots__ = ("ap", "w", "rs", "name")

    def __init__(self, ap, name=""):
        self.ap = ap
        self.name = name
        self.w = None
        self.rs = {}


class Plan:
    def __init__(self):
        self.needed = set()
        self.cnt = None

    def finalize(self):
        per = {}
        for (p, s) in self.needed:
            per.setdefault(p, []).append(s)
        self.cnt = {}
        for p, lst in per.items():
            lst.sort()
            self.cnt[p] = {s: i + 1 for i, s in enumerate(lst)}


class KB:
    def __init__(self, nc, plan, sems, dsems, tiles):
        self.nc = nc
        self.plan = plan
        self.sems = sems
        self.dsems = dsems
        self.tiles = tiles
        self.cur = None
        self.eng = None

    def begin(self, cur, eng):
        self.cur = cur
        self.eng = eng
        self.seq = {e: 0 for e in ENGS}
        self.known = {e: {} for e in ENGS}
        self.ndma = 0
        self.ndma_q = [0, 0]
        self.psum_rr = 0
        self.wr_rr = 0
        self.misc = {}
        for t in self.tiles:
            t.w = None
            t.rs = {}

    def _need(self, E, P, ps, s_cur):
        if P == E:
            if E == "pe":
                return
            if ps < s_cur - 3:
                return
        kn = self.known[E]
        if kn.get(P, 0) >= ps:
            return
        kn[P] = ps
        if self.cur is None:
            self.plan.needed.add((P, ps))
        elif self.cur == E:
            if P[0] == "q":
                self.eng.wait_ge(self.dsems[int(P[1:])], 16 * ps)
            else:
                self.eng.wait_ge(self.sems[P], self.plan.cnt[P][ps])

    def _deps(self, E, reads, writes, s_cur, extra=()):
        for t in reads:
            if t.w is not None:
                self._need(E, t.w[0], t.w[1], s_cur)
        for t in writes:
            if t.w is not None:
                self._need(E, t.w[0], t.w[1], s_cur)
            for p, s in t.rs.items():
                self._need(E, p, s, s_cur)
        for (p, s) in extra:
            self._need(E, p, s, s_cur)

    def _mark(self, tok, reads, writes):
        for t in reads:
            if t.rs.get(tok[0], 0) < tok[1]:
                t.rs[tok[0]] = tok[1]
        for t in writes:
            t.w = tok
            t.rs = {}

    def op(self, E, fn, reads=(), writes=()):
        s = self.seq[E] + 1
        self.seq[E] = s
        self._deps(E, reads, writes, s)
        tok = (E, s)
        if self.cur == E:
            ins = fn(self.eng)
            if tok in self.plan.needed:
                ins.then_inc(self.sems[E], 1)
        self._mark(tok, reads, writes)
        return tok

    def dma(self, Q, out, in_, reads=(), writes=(), deps=()):
        half = NDS // 2
        qi = 0 if Q == "sp" else 1
        i = self.ndma_q[qi]
        self.ndma_q[qi] += 1
        self.ndma += 1
        j = qi * half + (i % half)
        ds = i // half + 1
        s = self.seq[Q] + 1
        self.seq[Q] = s
        extra = ((("q%d" % j), ds - 1),) if ds > 1 else ()
        extra = extra + tuple(deps)
        self._deps(Q, reads, writes, s, extra)
        tok = ("q%d" % j, ds)
        if self.cur == Q:
            self.eng.dma_start(out=out, in_=in_).then_inc(self.dsems[j], 16)
        self._mark(tok, reads, writes)
        return tok

    def wait_all_dma(self, E):
        half = NDS // 2
        s = self.seq[E] + 1
        for qi in range(2):
            n = self.ndma_q[qi]
            for jj in range(half):
                if n > jj:
                    ds = (n - 1 - jj) // half + 1
                    self._need(E, "q%d" % (qi * half + jj), ds, s)

    def finish(self):
        self.wait_all_dma("sp")


def _blocks_kc(w, cols_per_blk=256):
    K, C = w.shape
    nkc = K // 128
    assert nkc * cols_per_blk == WBLK
    nb = (C + cols_per_blk - 1) // cols_per_blk
    wp = np.zeros((K, nb * cols_per_blk), np.float32)
    wp[:, :C] = w
    wp = wp.reshape(nkc, 128, nb, cols_per_blk).transpose(2, 1, 0, 3)
    return np.ascontiguousarray(wp).reshape(nb, 128, WBLK)


def _blocks_rows(w, rows_per_blk=2):
    K, C = w.shape
    assert C == 1024 and rows_per_blk * C == WBLK
    nkc = K // 128
    nb = nkc // rows_per_blk
    wp = w.reshape(nb, rows_per_blk, 128, C).transpose(0, 2, 1, 3)
    return np.ascontiguousarray(wp).reshape(nb, 128, WBLK)


def _blocks_gu(wg, wu):
    g = wg.reshape(8, 128, NFC, 128).transpose(2, 1, 0, 3)
    u = wu.reshape(8, 128, NFC, 128).transpose(2, 1, 0, 3)
    gu = np.stack([g, u], axis=2)
    return np.ascontiguousarray(gu).reshape(NFC, 128, WBLK)


class WStream:
    def __init__(self):
        self.parts = []
        self.index = {}
        self.n = 0

    def add(self, name, blocks):
        self.index[name] = (self.n, blocks.shape[0])
        self.parts.append(blocks)
        self.n += blocks.shape[0]

    def array(self):
        return np.concatenate(self.parts, axis=0)


def make_wstream(inp, layers):
    ws = WStream()
    for l in layers:
        ws.add("pre_gu%d" % l, _blocks_gu(inp["ffn_pre_w_gate"][l], inp["ffn_pre_w_up"][l]))
        ws.add("pre_d%d" % l, _blocks_rows(inp["ffn_pre_w_down"][l]))
        ws.add("post_gu%d" % l, _blocks_gu(inp["ffn_post_w_gate"][l], inp["ffn_post_w_up"][l]))
        ws.add("post_d%d" % l, _blocks_rows(inp["ffn_post_w_down"][l]))
        if l % 2 == 1:
            w = inp["ssd_w_in"][0]
            ws.add("ssd_in", np.concatenate([_blocks_kc(w[:, 2048:5120]), _blocks_kc(w[:, 0:2048]),
                                             _blocks_kc(w[:, 5120:5152])], axis=0))
            ws.add("ssd_out", _blocks_rows(inp["ssd_w_out"][0]))
        else:
            ws.add("hyb_in", _blocks_kc(hyb_perm(inp["hyb_w_in"][0])))
            ws.add("hyb_out", _blocks_rows(inp["hyb_w_out"][0]))
    return ws


def hyb_perm(w):
    o = np.cumsum([0, 512, 512, 512, 512, 8, 8, 256, 256, 512, 512, 4, 4])
    gq, gk, gv, gz, gb, ga, mq, mk, mv, mo, mi, mf = [w[:, o[i]:o[i + 1]] for i in range(12)]
    small = np.zeros((w.shape[0], 256), np.float32)
    small[:, 0:8] = gb
    small[:, 8:16] = ga
    small[:, 16:20] = mi
    small[:, 20:24] = mf
    return np.concatenate([gq, gk, gv, mq, mk, gz, mo, mv, mk, small], axis=1)


class Prog:
    def __init__(self, ntok, layers, nblk, windex, debug_stop=None):
        self.ntok = ntok
        self.layers = layers
        self.nblk = nblk
        self.windex = windex
        self.nsc = ntok // TS
        self.debug_stop = debug_stop
        nc = bass.Bass("TRN2", target_bir_lowering=False)
        self.nc = nc
        self.x = nc.dram_tensor("x", [ntok, D_MODEL], F32, kind="ExternalInput").ap()
        self.wsrc = nc.dram_tensor("wsrc", [nblk, 128, WBLK], F32, kind="ExternalInput").ap()
        self.lnp_d = nc.dram_tensor("lnp", [DEPTH * 3, 2, D_MODEL], F32, kind="ExternalInput").ap()
        self.cst_d = nc.dram_tensor("cst", [128, 5, 128], F32, kind="ExternalInput").ap()
        self.bpar_d = nc.dram_tensor("bpar", [NBP], F32, kind="ExternalInput").ap()
        self.convw_d = nc.dram_tensor("convw", [128, 36, 5], F32, kind="ExternalInput").ap()
        self.colp_d = nc.dram_tensor("colp", [128, 16], F32, kind="ExternalInput").ap()
        self.y = nc.dram_tensor("y", [ntok, D_MODEL], F32, kind="ExternalOutput").ap()
        self.wbf = nc.dram_tensor("wbf", [nblk, 128, WBLK], BF16).ap()

    def build(self):
        nc = self.nc
        with ExitStack() as es:
            def sb(name, shape, dt):
                return es.enter_context(nc.sbuf_tensor(name, shape, dt))

            self.tiles = []

            def T(ap, name=""):
                t = Tile(ap, name)
                self.tiles.append(t)
                return t

            xtm = sb("xtm", [128, NT, D_MODEL], F32)
            self.xtm = [T(xtm[:, t, :], "xtm%d" % t) for t in range(NT)]
            xT = sb("xT", [128, 8, TS], BF16)
            self.xT_full = xT
            self.xT = [T(xT[:, :, t * 128:(t + 1) * 128], "xT%d" % t) for t in range(NT)]
            hT = sb("hT", [128, NFC, TS], BF16)
            self.hT_full = hT
            self.hT = [T(hT[:, j, :], "hT%d" % j) for j in range(NFC)]
            wring = sb("wring", [128, NSLOT, WBLK], BF16)
            self.wslot = [T(wring[:, s, :], "w%d" % s) for s in range(NSLOT)]
            lnp = sb("lnpb", [128, 1, 2, D_MODEL], F32)
            self.lnp = [T(lnp[:, 0, :, :], "lnp0")]
            cf = sb("cf", [128, 5, 128], F32)
            self.cf = T(cf[:], "cf")
            bp = sb("bp", [128, NBP], F32)
            self.bp = T(bp[:], "bp")
            cw = sb("cw", [128, 36, 5], F32)
            self.cw = T(cw[:], "cw")
            colp = sb("colp_sb", [128, 16], F32)
            self.colp = T(colp[:], "colp")
            aneg = sb("aneg", [128, 32], F32)
            self.aneg = T(aneg[:], "aneg")
            hist = sb("hist", [128, 36, 3], F32)
            self.hist_full = hist
            self.hist = [T(hist[:, c, :], "hist%d" % c) for c in range(36)]
            fm = sb("fm", [128, 24, TS], BF16)
            self.fm = [T(fm[:, c, :], "fm%d" % c) for c in range(24)]
            gates = sb("gates", [128, NT, 2048], BF16)
            self.gates = [T(gates[:, t, :], "gates%d" % t) for t in range(NT)]
            sm = sb("sm", [128, NT, 4, 32], F32)
            self.sm = [T(sm[:, t, :, :], "sm%d" % t) for t in range(NT)]
            sv = sb("sv", [128, 8, 32], F32)
            self.sv = T(sv[:], "sv")
            cst2 = sb("cstage", [128, 1, TS + 3], F32)
            self.cstage = [T(cst2[:, i, :], "cstage%d" % i) for i in range(1)]
            cacc = sb("cacc", [128, 2, TS], F32)
            self.cacc_full = cacc
            self.cacc = [T(cacc[:, i, :], "cacc%d" % i) for i in range(2)]
            self.gact = self.cacc
            xs_tm = sb("xs_tm", [128, 2048], BF16)
            self.xs_tm = T(xs_tm[:], "xs_tm")
            xdt = sb("xdt", [128, 2048], BF16)
            self.xdt = T(xdt[:], "xdt")
            xw = sb("xw", [128, 2048], BF16)
            self.xw = T(xw[:], "xw")
            xD = sb("xD", [128, 2048], BF16)
            self.xD = T(xD[:], "xD")
            b_tm = sb("b_tm", [128, 512], BF16)
            self.b_tm = T(b_tm[:], "b_tm")
            yb = sb("ybuf", [128, 2048], F32)
            self.ybuf = T(yb[:], "ybuf")
            ysq = sb("ysq", [128, 2048], F32)
            self.ysq_full = ysq
            self.ysq_h = [T(ysq[:, 0:1024], "ysq_a"), T(ysq[:, 1024:2048], "ysq_b")]
            self.wtmp = [(self.ybuf, yb[:, 0:1024]), (self.ybuf, yb[:, 1024:2048]),
                         (self.ysq_h[0], ysq[:, 0:1024]), (self.ysq_h[1], ysq[:, 1024:2048])]
            ybf = sb("ybf", [128, 2048], BF16)
            self.ybf = T(ybf[:], "ybf")
            self.xbf = [(self.ybf, ybf[:, 0:1024]), (self.ybf, ybf[:, 1024:2048])]
            ytmp = sb("ytmp", [128, 512], F32)
            self.ytmp = T(ytmp[:], "ytmp")
            sst = sb("sstate", [128, 4, 512], F32)
            self.sst = [T(sst[:, g, :], "sst%d" % g) for g in range(4)]
            sstb = sb("sstate_bf", [128, 4, 512], BF16)
            self.sstb = [T(sstb[:, g, :], "sstb%d" % g) for g in range(4)]
            m5 = sb("m_AT8", [128, 8, 128], BF16)
            self.m_AT8 = T(m5[:], "m_AT8")
            kqsb = sb("kq_sb", [128, 2, 128], F32)
            self.kq_sb = [T(kqsb[:, i, :], "kq_sb%d" % i) for i in range(2)]
            ss = sb("ss", [128, 4, 4], F32)
            self.ss = T(ss[:], "ss")
            self.raw = dict(xs_tm=xs_tm, xdt=xdt, xw=xw, xD=xD, ybuf=yb, ysq=ysq)
            qk_tm = sb("qk_tm", [128, 1024], BF16)
            self.qk_tm = T(qk_tm[:], "qk_tm")
            v_tm = sb("v_tm", [128, 512], BF16)
            self.v_tm = T(v_tm[:], "v_tm")
            bvk = sb("bvk", [128, 2, 512], F32)
            self.bv = T(bvk[:, 0, :], "bv")
            self.bk = T(bvk[:, 1, :], "bk")
            u0 = sb("u0sb", [128, 512], F32)
            self.u0 = T(u0[:], "u0sb")
            ukw = sb("ukw", [128, 2, 512], BF16)
            self.u = T(ukw[:, 0, :], "u")
            self.kw = T(ukw[:, 1, :], "kw")
            wtb = sb("wtb", [128, 8, 128], BF16)
            self.wtb = T(wtb[:], "wtb")
            hv = sb("hv", [128, 16, 8], F32)
            self.hv = T(hv[:], "hv")
            hs = sb("hs", [128, 4, 16], F32)
            self.hs = T(hs[:], "hs")
            hmul = sb("hmul", [128, 24], F32)
            self.hmul = T(hmul[:], "hmul")
            vext = sb("vext", [128, 4, 129], BF16)
            self.vext = T(vext[:], "vext")
            atm = sb("atm", [128, 4, 128], BF16)
            self.atm = T(atm[:], "atm")
            hraw = sb("hraw", [128, 4, 129], F32)
            self.hraw = T(hraw[:], "hraw")
            self.raw["hraw"] = hraw
            kwm = sb("kwm", [128, 256], BF16)
            self.kwm = T(kwm[:], "kwm")
            sg = sb("sg", [128, 4, 64], F32)
            self.sg = T(sg[:], "sg")
            sgb = sb("sgb", [128, 4, 64], BF16)
            self.sgb = T(sgb[:], "sgb")
            cn = sb("cn", [128, 2, 129], F32)
            self.cn = T(cn[:], "cn")
            cnb = sb("cnb", [128, 2, 129], BF16)
            self.cnb = T(cnb[:], "cnb")
            st = sb("st", [128, NT, 2, 6], F32)
            self.st = [T(st[:, t, :, :], "st%d" % t) for t in range(NT)]
            mv = sb("mv", [128, NT, 2], F32)
            self.mv = T(mv[:], "mv")
            rs = sb("rs", [128, 3, NT], F32)
            self.rs = T(rs[:], "rs")
            identb = sb("identb", [128, 128], BF16)
            self.identb = T(identb[:], "identb")
            self.ps = []
            for b in range(8):
                p = es.enter_context(nc.psum_tensor("ps%d" % b, [128, 512], F32))
                self.ps.append(T(p[:], "ps%d" % b))

            sems = {e: es.enter_context(nc.semaphore("s_" + e)) for e in ENGS}
            dsems = [es.enter_context(nc.semaphore("q%d" % j)) for j in range(NDS)]
            plan = Plan()
            kb = KB(nc, plan, sems, dsems, self.tiles)
            self.kb = kb
            kb.begin(None, None)
            self.body()
            plan.finalize()
            block = es.enter_context(nc.Block())

            def run(name):
                def f(e):
                    kb.begin(name, e)
                    self.body()
                return f

            block.sync(run("sp"))
            block.scalar(run("act"))
            block.vector(run("dve"))
            block.tensor(run("pe"))
            block.gpsimd(run("pool"))
        return nc

    def psum(self, n=8):
        kb = self.kb
        t = self.ps[kb.psum_rr % n]
        kb.psum_rr += 1
        return t

    def rot(self, lst, key):
        kb = self.kb
        i = kb.misc.get(key, 0)
        kb.misc[key] = i + 1
        return lst[i % len(lst)]

    def wload(self, blk):
        kb = self.kb
        slot = self.wslot[kb.wr_rr % NSLOT]
        kb.wr_rr += 1
        tok = self.wbf_toks.pop(blk, None)
        kb.dma("sp", slot.ap, self.wbf[blk], reads=(), writes=(slot,), deps=(tok,) if tok is not None else ())
        return slot

    def prologue(self):
        kb = self.kb
        kb.dma("pool", self.identb.ap, self.cst_d[:, 0, :], writes=(self.identb,))
        kb.dma("pool", self.cf.ap, self.cst_d, writes=(self.cf,))
        kb.dma("pool", self.bp.ap, self.bpar_d.partition_broadcast(128), writes=(self.bp,))
        kb.dma("pool", self.cw.ap, self.convw_d, writes=(self.cw,))
        kb.dma("pool", self.colp.ap, self.colp_d, writes=(self.colp,))
        self.load_x(0)
        self.wbf_toks = {}
        for b in range(self.nblk):
            self.wbf_toks[b] = kb.dma("pool", self.wbf[b], self.wsrc[b])
        self.setup_state()
        self.setup_hyb()

    def load_x(self, sc):
        kb = self.kb
        for t in range(NT):
            r0 = sc * TS + t * 128
            kb.dma("pool", self.xtm[t].ap, self.x[r0:r0 + 128, :], writes=(self.xtm[t],))

    def store_y(self, sc):
        kb = self.kb
        for t in range(NT):
            r0 = sc * TS + t * 128
            kb.dma("pool", self.y[r0:r0 + 128, :], self.xtm[t].ap, reads=(self.xtm[t],))

    def to_xT_cast(self, t):
        kb = self.kb
        xb, xb_ap = self.xbf[t % 2]
        src = self.xtm[t]
        kb.op("act", lambda e: e.activation(out=xb_ap, in_=src.ap, func=AF.Copy),
              reads=(src,), writes=(xb,))

    def to_xT_T(self, t):
        kb = self.kb
        xb, xb_ap = self.xbf[t % 2]
        p = self.psum()
        pb = p.ap.bitcast(BF16)
        for kc in range(8):
            kb.op("pe", lambda e, kc=kc: e.transpose(pb[:, kc * 128:(kc + 1) * 128],
                                                     xb_ap[:, kc * 128:(kc + 1) * 128],
                                                     self.identb.ap),
                  reads=(xb, self.identb), writes=(p,))
        dst = self.xT[t]
        kb.op("dve", lambda e: e.tensor_copy(out=dst.ap, in_=pb.rearrange("p (k c) -> p k c", k=8)),
              reads=(p,), writes=(dst,))

    def to_xT(self, t):
        self.to_xT_cast(t)
        self.to_xT_T(t)

    def down_ln(self, blk0, nkc, c, ln_idx):
        kb = self.kb
        lp = self.lnp[0]
        kb.dma("pool", lp.ap, self.lnp_d[ln_idx].partition_broadcast(128), writes=(lp,))
        for tp in range(NT // 2):
            banks = [self.psum(), self.psum(), self.psum(), self.psum()]
            for b in range(nkc // 2):
                wsl = self.wload(blk0 + b)
                wv = wsl.ap.rearrange("p (r c) -> p r c", r=2)
                for r in range(2):
                    kc = b * 2 + r
                    for ti in range(2):
                        t = tp * 2 + ti
                        for dh in range(2):
                            pt = banks[ti * 2 + dh]
                            kb.op("pe", lambda e, r=r, kc=kc, t=t, dh=dh, pt=pt, wv=wv: e.matmul(
                                pt.ap, lhsT=self.hT[kc].ap[:, t * 128:(t + 1) * 128],
                                rhs=wv[:, r, dh * 512:(dh + 1) * 512],
                                start=(kc == 0), stop=(kc == nkc - 1)),
                                reads=(wsl, self.hT[kc]), writes=(pt,))
            if tp > 0:
                self.to_xT_T(tp * 2 - 2)
                self.to_xT_T(tp * 2 - 1)
            for ti in range(2):
                self.resid_ln_tile(tp * 2 + ti, banks[ti * 2:ti * 2 + 2], c, lp)
        self.to_xT_T(NT - 2)
        self.to_xT_T(NT - 1)

    def resid_ln_tile(self, t, banks, c, lp):
        kb = self.kb
        eps = LN_EPS / (DN_ALPHA * DN_ALPHA)
        wt, wap = self.wtmp[t]
        x = self.xtm[t]
        rs = self.rs
        for dh in range(2):
            pt = banks[dh]
            sl = slice(dh * 512, (dh + 1) * 512)
            kb.op("dve", lambda e, pt=pt, sl=sl: e.scalar_tensor_tensor(
                out=wap[:, sl], in0=pt.ap, scalar=float(c), in1=x.ap[:, sl],
                op0=ALU.mult, op1=ALU.add), reads=(pt, x), writes=(wt,))
        stt = self.st[t]
        for dh in range(2):
            kb.op("dve", lambda e, dh=dh: e.bn_stats(out=stt.ap[:, dh, :], in_=wap[:, dh * 512:(dh + 1) * 512]),
                  reads=(wt,), writes=(stt,))
        kb.op("dve", lambda e: e.bn_aggr(out=self.mv.ap[:, t, :], in_=stt.ap), reads=(stt,), writes=(self.mv,))
        kb.op("act", lambda e: e.activation(out=rs.ap[:, 0, t:t + 1], in_=self.mv.ap[:, t, 1:2],
                                            func=AF.Sqrt, bias=float(eps), scale=1.0),
              reads=(self.mv,), writes=(rs,))
        kb.op("dve", lambda e: e.reciprocal(out=rs.ap[:, 1, t:t + 1], in_=rs.ap[:, 0, t:t + 1]),
              reads=(rs,), writes=(rs,))
        kb.op("dve", lambda e: e.scalar_tensor_tensor(out=rs.ap[:, 2, t:t + 1], in0=self.mv.ap[:, t, 0:1],
                                                      scalar=-1.0, in1=rs.ap[:, 1, t:t + 1],
                                                      op0=ALU.mult, op1=ALU.mult),
              reads=(self.mv, rs), writes=(rs,))
        kb.op("act", lambda e: e.activation(out=wap, in_=wap, func=AF.Identity, bias=rs.ap[:, 2, t:t + 1],
                                            scale=rs.ap[:, 1, t:t + 1]), reads=(wt, rs), writes=(wt,))
        kb.op("pool", lambda e: e.tensor_tensor(out=wap, in0=wap, in1=lp.ap[:, 0, :], op=ALU.mult),
              reads=(wt, lp), writes=(wt,))
        kb.op("dve", lambda e: e.tensor_tensor(out=x.ap, in0=wap, in1=lp.ap[:, 1, :], op=ALU.add),
              reads=(wt, lp), writes=(x,))
        self.to_xT_cast(t)

    def ffn_ln(self, l, which, sc, ln_idx):
        kb = self.kb
        gu0, ngu = self.windex["%s_gu%d" % (which, l)]
        d0, nd = self.windex["%s_d%d" % (which, l)]
        for j in range(NFC):
            wsl = self.wload(gu0 + j)
            wv = wsl.ap.rearrange("p (g k f) -> p g k f", g=2, k=8)
            pg = self.psum()
            pu = self.psum()
            for g, pt in ((0, pg), (1, pu)):
                for kc in range(8):
                    kb.op("pe", lambda e, g=g, kc=kc, pt=pt: e.matmul(
                        pt.ap, lhsT=wv[:, g, kc, :], rhs=self.xT_full[:, kc, :],
                        start=(kc == 0), stop=(kc == 7)),
                        reads=(wsl,) + tuple(self.xT), writes=(pt,))
            ga = self.gact[j % 2]
            kb.op("act", lambda e: e.activation(out=ga.ap, in_=pg.ap, func=AF.Silu),
                  reads=(pg,), writes=(ga,))
            h = self.hT[j]
            kb.op("dve", lambda e: e.tensor_tensor(out=h.ap, in0=ga.ap, in1=pu.ap, op=ALU.mult),
                  reads=(ga, pu), writes=(h,))
        self.down_ln(d0, NFC, 0.5 / DN_ALPHA, ln_idx)

    def resid_ln(self, banks, c, lp):
        kb = self.kb
        eps = LN_EPS / (DN_ALPHA * DN_ALPHA)
        for t in range(NT):
            wt, wap = self.wtmp[t]
            x = self.xtm[t]
            for dh in range(2):
                pt = banks[t * 2 + dh]
                sl = slice(dh * 512, (dh + 1) * 512)
                kb.op("dve", lambda e, pt=pt, sl=sl: e.scalar_tensor_tensor(
                    out=wap[:, sl], in0=pt.ap, scalar=float(c), in1=x.ap[:, sl],
                    op0=ALU.mult, op1=ALU.add), reads=(pt, x), writes=(wt,))
            stt = self.st[t]
            for dh in range(2):
                kb.op("dve", lambda e, dh=dh: e.bn_stats(out=stt.ap[:, dh, :],
                                                         in_=wap[:, dh * 512:(dh + 1) * 512]),
                      reads=(wt,), writes=(stt,))
            kb.op("dve", lambda e, t=t: e.bn_aggr(out=self.mv.ap[:, t, :], in_=stt.ap),
                  reads=(stt,), writes=(self.mv,))
        rs = self.rs
        kb.op("act", lambda e: e.activation(out=rs.ap[:, 0, :], in_=self.mv.ap[:, :, 1],
                                            func=AF.Sqrt, bias=float(eps), scale=1.0),
              reads=(self.mv,), writes=(rs,))
        kb.op("dve", lambda e: e.reciprocal(out=rs.ap[:, 1, :], in_=rs.ap[:, 0, :]),
              reads=(rs,), writes=(rs,))
        kb.op("dve", lambda e: e.scalar_tensor_tensor(out=rs.ap[:, 2, :], in0=self.mv.ap[:, :, 0],
                                                      scalar=-1.0, in1=rs.ap[:, 1, :],
                                                      op0=ALU.mult, op1=ALU.mult),
              reads=(self.mv, rs), writes=(rs,))
        for t in range(NT):
            wt, wap = self.wtmp[t]
            x = self.xtm[t]
            kb.op("act", lambda e, t=t, wap=wap: e.activation(out=wap, in_=wap, func=AF.Identity,
                                                              bias=rs.ap[:, 2, t:t + 1],
                                                              scale=rs.ap[:, 1, t:t + 1]),
                  reads=(wt, rs), writes=(wt,))
            kb.op("pool", lambda e, wap=wap: e.tensor_tensor(out=wap, in0=wap, in1=lp.ap[:, 0, :], op=ALU.mult),
                  reads=(wt, lp), writes=(wt,))
            kb.op("dve", lambda e, wap=wap, x=x: e.tensor_tensor(out=x.ap, in0=wap, in1=lp.ap[:, 1, :], op=ALU.add),
                  reads=(wt, lp), writes=(x,))
            self.to_xT(t)


    def setup_state(self):
        kb = self.kb
        for g in range(4):
            kb.op("pool", lambda e, g=g: e.memset(self.sst[g].ap, 0.0), writes=(self.sst[g],))
            kb.op("pool", lambda e, g=g: e.memset(self.sstb[g].ap, 0.0), writes=(self.sstb[g],))
        kb.op("pool", lambda e: e.memset(self.hist_full[:], 0.0), writes=tuple(self.hist))
        kb.op("act", lambda e: e.activation(out=self.aneg.ap, in_=self.bp.ap[:, BP_SSD_ALOG:BP_SSD_ALOG + 32],
                                            func=AF.Exp), reads=(self.bp,), writes=(self.aneg,))
        kb.op("dve", lambda e: e.tensor_scalar(out=self.aneg.ap, in0=self.aneg.ap, scalar1=-1.0, scalar2=None,
                                               op0=ALU.mult), reads=(self.aneg,), writes=(self.aneg,))

    def conv_silu(self, pt, ci, dst):
        kb = self.kb
        st = self.rot(self.cstage, "cstage")
        acc = self.rot(self.cacc, "cacc")
        hs = self.hist[ci]
        cw = self.cw
        kb.op("act", lambda e: e.activation(out=st.ap[:, 3:TS + 3], in_=pt.ap, func=AF.Copy),
              reads=(pt,), writes=(st,))
        kb.op("pool", lambda e: e.tensor_copy(out=st.ap[:, 0:3], in_=hs.ap), reads=(hs,), writes=(st,))
        kb.op("pool", lambda e: e.tensor_copy(out=hs.ap, in_=st.ap[:, TS:TS + 3]), reads=(st,), writes=(hs,))
        kb.op("dve", lambda e: e.tensor_scalar(out=acc.ap, in0=st.ap[:, 0:TS], scalar1=cw.ap[:, ci, 0:1],
                                               scalar2=None, op0=ALU.mult), reads=(st, cw), writes=(acc,))
        for k in range(1, 4):
            kb.op("dve", lambda e, k=k: e.scalar_tensor_tensor(
                out=acc.ap, in0=st.ap[:, k:k + TS], scalar=cw.ap[:, ci, k:k + 1], in1=acc.ap,
                op0=ALU.mult, op1=ALU.add), reads=(st, cw, acc), writes=(acc,))
        kb.op("act", lambda e: e.activation(out=dst.ap, in_=acc.ap, func=AF.Silu, bias=cw.ap[:, ci, 4:5],
                                            scale=1.0), reads=(acc, cw), writes=(dst,))

    def fm_proj(self, wsl, half, pt):
        kb = self.kb
        wv = wsl.ap.rearrange("p (k c) -> p k c", k=8)
        for kc in range(8):
            kb.op("pe", lambda e, kc=kc: e.matmul(pt.ap, lhsT=wv[:, kc, half * 128:(half + 1) * 128],
                                                  rhs=self.xT_full[:, kc, :], start=(kc == 0), stop=(kc == 7)),
                  reads=(wsl,) + tuple(self.xT), writes=(pt,))

    def tm_proj(self, wsl, t, pt, ncols=256):
        kb = self.kb
        wv = wsl.ap.rearrange("p (k c) -> p k c", k=8)
        for kc in range(8):
            kb.op("pe", lambda e, kc=kc: e.matmul(pt.ap[:, 0:ncols],
                                                  lhsT=self.xT_full[:, kc, t * 128:(t + 1) * 128],
                                                  rhs=wv[:, kc, 0:ncols], start=(kc == 0), stop=(kc == 7)),
                  reads=(wsl, self.xT[t]), writes=(pt,))

    def out_proj_ln(self, o0, nkc, ln_idx):
        self.down_ln(o0, nkc, 1.0 / DN_ALPHA, ln_idx)

    def ssd_mixer(self, sc, ln_idx):
        kb = self.kb
        w0, _ = self.windex["ssd_in"]
        o0, _ = self.windex["ssd_out"]
        wsl = None
        for cc in range(24):
            if cc % 2 == 0:
                wsl = self.wload(w0 + cc // 2)
            pt = self.psum(5)
            self.fm_proj(wsl, cc % 2, pt)
            self.conv_silu(pt, 12 + cc, self.fm[cc])
        for b in range(8):
            wsl = self.wload(w0 + 12 + b)
            for t in range(NT):
                pt = self.psum(5)
                self.tm_proj(wsl, t, pt)
                gt = self.gates[t]
                kb.op("act", lambda e, b=b, pt=pt, gt=gt: e.activation(
                    out=gt.ap[:, b * 256:(b + 1) * 256], in_=pt.ap[:, 0:256], func=AF.Silu),
                    reads=(pt,), writes=(gt,))
        wsl = self.wload(w0 + 20)
        for t in range(NT):
            pt = self.psum(5)
            self.tm_proj(wsl, t, pt, 32)
            sm = self.sm[t]
            kb.op("dve", lambda e, pt=pt, sm=sm: e.tensor_tensor(
                out=sm.ap[:, 0, :], in0=pt.ap[:, 0:32], in1=self.bp.ap[:, BP_SSD_DTB:BP_SSD_DTB + 32],
                op=ALU.add), reads=(pt, self.bp), writes=(sm,))
            kb.op("act", lambda e, sm=sm: e.activation(out=sm.ap[:, 1, :], in_=sm.ap[:, 0, :], func=AF.Exp),
                  reads=(sm,), writes=(sm,))
            kb.op("act", lambda e, sm=sm: e.activation(out=sm.ap[:, 2, :], in_=sm.ap[:, 1, :], func=AF.Ln,
                                                       bias=1.0, scale=1.0), reads=(sm,), writes=(sm,))
            kb.op("dve", lambda e, sm=sm: e.tensor_tensor(out=sm.ap[:, 3, :], in0=sm.ap[:, 2, :],
                                                          in1=self.aneg.ap, op=ALU.mult),
                  reads=(sm, self.aneg), writes=(sm,))
        for t in range(NT):
            self.ssd_chunk(t)
        self.out_proj_ln(o0, 16, ln_idx)

    def transposes_to(self, srcs, dst_tile, dst_ap, evac="dve", scale_ap=None):
        kb = self.kb
        p = self.psum(5)
        pb = p.ap.bitcast(BF16)
        n = len(srcs)
        for q, (tl, ap) in enumerate(srcs):
            kb.op("pe", lambda e, q=q, ap=ap: e.transpose(pb[:, q * 128:(q + 1) * 128], ap, self.identb.ap),
                  reads=(tl, self.identb), writes=(p,))
        if scale_ap is None:
            if evac == "act":
                kb.op("act", lambda e: e.activation(out=dst_ap, in_=pb[:, 0:n * 128], func=AF.Copy),
                      reads=(p,), writes=dst_tile)
            else:
                src = pb[:, 0:n * 128]
                if len(dst_ap.shape) == 3:
                    src = src.rearrange("p (k c) -> p k c", k=n)
                kb.op("dve", lambda e: e.tensor_copy(out=dst_ap, in_=src),
                      reads=(p,), writes=dst_tile)
        else:
            kb.op("dve", lambda e: e.tensor_tensor(out=dst_ap, in0=pb[:, 0:n * 128].rearrange("p (k c) -> p k c", k=n),
                                                   in1=scale_ap, op=ALU.mult),
                  reads=(p, self.colp), writes=dst_tile)

    def decay_block(self, row_ap, col_ap, mask_idx, n, dg, dg_t, tt, tt_t, col_t, nb=5):
        kb = self.kb
        cf = self.cf
        tt_w = tt_t if isinstance(tt_t, tuple) else (tt_t,)
        identf = cf.ap[:, C_ID, :]
        onesf = cf.ap[:, C_ONES, :]
        maskf = cf.ap[:, mask_idx if mask_idx is not None else 0, :]
        for h in range(n):
            kb.op("act", lambda e, h=h: e.activation(out=dg[:, h, :], in_=identf, func=AF.Copy,
                                                     scale=row_ap[:, h:h + 1]),
                  reads=(cf, col_t), writes=(dg_t,))
        for half in range((n + 3) // 4):
            pb = self.psum(nb)
            kb.op("pe", lambda e, half=half, pb=pb: e.matmul(
                pb.ap, lhsT=onesf, rhs=dg[:, half * 4:(half + 1) * 4, :], start=True, stop=True),
                reads=(cf, dg_t), writes=(pb,))
            kb.op("dve", lambda e, half=half, pb=pb: e.tensor_tensor(
                out=tt[:, half * 4:(half + 1) * 4, :], in0=pb.ap.rearrange("p (h c) -> p h c", h=4),
                in1=col_ap[:, half * 4:(half + 1) * 4].unsqueeze(2).to_broadcast([128, 4, 128]), op=ALU.subtract),
                reads=(pb, col_t), writes=tt_w)
        if mask_idx is None:
            kb.op("act", lambda e: e.activation(out=tt[:, 0:n, :], in_=tt[:, 0:n, :], func=AF.Relu, scale=-1.0),
                  reads=tt_w, writes=tt_w)
            kb.op("act", lambda e: e.activation(out=tt[:, 0:n, :], in_=tt[:, 0:n, :], func=AF.Exp, scale=-1.0),
                  reads=tt_w, writes=tt_w)
            return
        kb.op("pool", lambda e: e.tensor_tensor(out=tt[:, 0:n, :], in0=tt[:, 0:n, :],
                                                in1=maskf.unsqueeze(1).to_broadcast([128, n, 128]), op=ALU.add),
              reads=tt_w + (cf,), writes=tt_w)
        kb.op("act", lambda e: e.activation(out=tt[:, 0:n, :], in_=tt[:, 0:n, :], func=AF.Exp),
              reads=tt_w, writes=tt_w)

    def ssd_chunk(self, t):
        kb = self.kb
        tc = slice(t * 128, (t + 1) * 128)
        cf, sv, sm = self.cf, self.sv, self.sm[t]
        for half in range(2):
            srcs = [(self.fm[half * 8 + q], self.fm[half * 8 + q].ap[:, tc]) for q in range(8)]
            self.transposes_to(srcs, (self.xs_tm,), self.xs_tm.ap[:, half * 1024:(half + 1) * 1024],
                               evac="act" if half else "dve")
        srcs = [(self.fm[16 + g], self.fm[16 + g].ap[:, tc]) for g in range(4)]
        self.transposes_to(srcs, (self.b_tm,), self.b_tm.ap, evac="dve")
        p = self.psum(5)
        kb.op("pe", lambda e: e.matmul(p.ap[:, 0:32], lhsT=cf.ap[:, C_TRI, :], rhs=sm.ap[:, 3, :],
                                       start=True, stop=True), reads=(cf, sm), writes=(p,))
        kb.op("pe", lambda e: e.matmul(p.ap[:, 32:64], lhsT=cf.ap[:, C_ONES, :], rhs=sm.ap[:, 3, :],
                                       start=True, stop=True), reads=(cf, sm), writes=(p,))
        kb.op("dve", lambda e: e.tensor_copy(out=sv.ap[:, 0, :], in_=p.ap[:, 0:32]), reads=(p,), writes=(sv,))
        kb.op("act", lambda e: e.activation(out=sv.ap[:, 1, :], in_=p.ap[:, 0:32], func=AF.Exp),
              reads=(p,), writes=(sv,))
        kb.op("dve", lambda e: e.tensor_tensor(out=sv.ap[:, 2, :], in0=p.ap[:, 32:64], in1=sv.ap[:, 0, :],
                                               op=ALU.subtract), reads=(p, sv), writes=(sv,))
        kb.op("act", lambda e: e.activation(out=sv.ap[:, 3, :], in_=sv.ap[:, 2, :], func=AF.Exp),
              reads=(sv,), writes=(sv,))
        kb.op("dve", lambda e: e.tensor_tensor(out=sv.ap[:, 4, :], in0=sv.ap[:, 3, :], in1=sm.ap[:, 2, :],
                                               op=ALU.mult), reads=(sv, sm), writes=(sv,))
        kb.op("act", lambda e: e.activation(out=sv.ap[:, 5, :], in_=p.ap[:, 32:64], func=AF.Exp),
              reads=(p,), writes=(sv,))
        xs3 = self.xs_tm.ap.rearrange("p (h c) -> p h c", h=32)

        def bc32(ap2d):
            return ap2d.unsqueeze(2).to_broadcast([128, 32, 64])

        kb.op("dve", lambda e: e.tensor_tensor(out=self.xdt.ap.rearrange("p (h c) -> p h c", h=32), in0=xs3,
                                               in1=bc32(sm.ap[:, 2, :]), op=ALU.mult),
              reads=(self.xs_tm, sm), writes=(self.xdt,))
        kb.op("pool", lambda e: e.tensor_tensor(out=self.xw.ap.rearrange("p (h c) -> p h c", h=32), in0=xs3,
                                                in1=bc32(sv.ap[:, 4, :]), op=ALU.mult),
              reads=(self.xs_tm, sv), writes=(self.xw,))
        kb.op("dve", lambda e: e.tensor_tensor(out=self.xD.ap.rearrange("p (h c) -> p h c", h=32), in0=xs3,
                                               in1=bc32(self.bp.ap[:, BP_SSD_D:BP_SSD_D + 32]), op=ALU.mult),
              reads=(self.xs_tm, self.bp), writes=(self.xD,))
        YD, QS, SU = self.ps[5], self.ps[6], self.ps[7]
        raw = self.raw
        dg8 = raw["ysq"][:, 0:1024].rearrange("p (h c) -> p h c", h=8)
        TT = [(self.ysq_h[1], raw["ysq"][:, 1024:2048].rearrange("p (h c) -> p h c", h=8)),
              (tuple(self.cacc), self.cacc_full[:].rearrange("p a c -> p (a c)").rearrange("p (h c) -> p h c", h=8))]
        hraw_bf = self.raw["hraw"][:].rearrange("p a c -> p (a c)")[:, 0:512].bitcast(BF16).rearrange("p (h c) -> p h c", h=8)
        ATB = [(self.m_AT8, self.m_AT8.ap), (self.hraw, hraw_bf)]
        for g in range(4):
            bt, ct = self.fm[16 + g], self.fm[20 + g]
            tt_tile, tt8 = TT[g % 2]
            tt_tiles = tt_tile if isinstance(tt_tile, tuple) else (tt_tile,)
            AT8t, AT8 = ATB[g % 2]
            pkq = self.psum(5)
            kb.op("pe", lambda e, bt=bt, ct=ct, pkq=pkq: e.matmul(pkq.ap[:, 0:128], lhsT=bt.ap[:, tc], rhs=ct.ap[:, tc],
                                                                 start=True, stop=True),
                  reads=(bt, ct), writes=(pkq,))
            kqs = self.kq_sb[g % 2]
            kb.op("dve", lambda e, pkq=pkq, kqs=kqs: e.tensor_tensor(out=kqs.ap, in0=pkq.ap[:, 0:128],
                                                                    in1=cf.ap[:, C_TRI, :], op=ALU.mult),
                  reads=(pkq, cf), writes=(kqs,))
            gcg = sv.ap[:, 0, g * 8:(g + 1) * 8]
            self.decay_block(gcg, gcg, None, 8, dg8, self.ysq_h[0], tt8, tt_tile, sv)
            kb.op("pool", lambda e, kqs=kqs, tt8=tt8, AT8=AT8: e.tensor_tensor(
                out=AT8, in0=tt8, in1=kqs.ap.unsqueeze(1).to_broadcast([128, 8, 128]), op=ALU.mult),
                reads=tt_tiles + (kqs,), writes=(AT8t,))
            for r in range(8):
                h = g * 8 + r
                kb.op("pe", lambda e, r=r, h=h, AT8=AT8: e.matmul(YD.ap[:, r * 64:(r + 1) * 64], lhsT=AT8[:, r, :],
                                                         rhs=self.xdt.ap[:, h * 64:(h + 1) * 64],
                                                         start=True, stop=False),
                      reads=(AT8t, self.xdt), writes=(YD,))
                kb.op("pe", lambda e, r=r, h=h: e.matmul(YD.ap[:, r * 64:(r + 1) * 64], lhsT=self.identb.ap,
                                                         rhs=self.xD.ap[:, h * 64:(h + 1) * 64],
                                                         start=False, stop=True),
                      reads=(self.identb, self.xD), writes=(YD,))
            sb_, s_ = self.sstb[g], self.sst[g]
            kb.op("pe", lambda e, ct=ct, sb_=sb_: e.matmul(QS.ap, lhsT=ct.ap[:, tc], rhs=sb_.ap, start=True, stop=True),
                  reads=(ct, sb_), writes=(QS,))
            kb.op("pe", lambda e, g=g: e.matmul(SU.ap, lhsT=self.b_tm.ap[:, g * 128:(g + 1) * 128],
                                                rhs=self.xw.ap[:, g * 512:(g + 1) * 512], start=True, stop=True),
                  reads=(self.b_tm, self.xw), writes=(SU,))

            def bc8(ap2d):
                return ap2d.unsqueeze(2).to_broadcast([128, 8, 64])

            yt = self.ytmp
            kb.op("dve", lambda e, g=g: e.tensor_tensor(out=yt.ap.rearrange("p (h c) -> p h c", h=8),
                                                        in0=QS.ap.rearrange("p (h c) -> p h c", h=8),
                                                        in1=bc8(sv.ap[:, 1, g * 8:(g + 1) * 8]), op=ALU.mult),
                  reads=(QS, sv), writes=(yt,))
            kb.op("dve", lambda e, g=g: e.tensor_tensor(out=self.ybuf.ap[:, g * 512:(g + 1) * 512], in0=yt.ap,
                                                        in1=YD.ap, op=ALU.add),
                  reads=(yt, YD), writes=(self.ybuf,))
            kb.op("dve", lambda e, g=g, s_=s_: e.tensor_tensor(out=s_.ap.rearrange("p (h c) -> p h c", h=8),
                                                               in0=s_.ap.rearrange("p (h c) -> p h c", h=8),
                                                               in1=bc8(sv.ap[:, 5, g * 8:(g + 1) * 8]), op=ALU.mult),
                  reads=(s_, sv), writes=(s_,))
            kb.op("dve", lambda e, s_=s_: e.tensor_tensor(out=s_.ap, in0=s_.ap, in1=SU.ap, op=ALU.add),
                  reads=(s_, SU), writes=(s_,))
            kb.op("act", lambda e, s_=s_, sb_=sb_: e.activation(out=sb_.ap, in_=s_.ap, func=AF.Copy),
                  reads=(s_,), writes=(sb_,))
        yb, gt, ss = self.ybuf, self.gates[t], self.ss
        kb.op("dve", lambda e: e.tensor_tensor(out=yb.ap, in0=yb.ap, in1=gt.ap, op=ALU.mult),
              reads=(yb, gt), writes=(yb,))
        kb.op("act", lambda e: e.activation(out=self.ysq_full[:], in_=yb.ap, func=AF.Square),
              reads=(yb,), writes=tuple(self.ysq_h))
        kb.op("dve", lambda e: e.reduce_sum(out=ss.ap[:, 0, :], in_=self.ysq_full[:].rearrange("p (g c) -> p g c", g=4),
                                            axis=AX.X), reads=tuple(self.ysq_h), writes=(ss,))
        kb.op("act", lambda e: e.activation(out=ss.ap[:, 1, :], in_=ss.ap[:, 0, :], func=AF.Sqrt,
                                            bias=float(RMS_EPS), scale=1.0 / 512.0), reads=(ss,), writes=(ss,))
        kb.op("dve", lambda e: e.reciprocal(out=ss.ap[:, 2, :], in_=ss.ap[:, 1, :]), reads=(ss,), writes=(ss,))
        for g in range(4):
            kb.op("dve", lambda e, g=g: e.tensor_scalar(out=self.ybf.ap[:, g * 512:(g + 1) * 512],
                                                        in0=yb.ap[:, g * 512:(g + 1) * 512],
                                                        scalar1=ss.ap[:, 2, g:g + 1], scalar2=None, op0=ALU.mult),
                  reads=(yb, ss), writes=(self.ybf,))
        for half in range(2):
            srcs = [(self.ybf, self.ybf.ap[:, (half * 8 + q) * 128:(half * 8 + q + 1) * 128]) for q in range(8)]
            dst_tiles = tuple(self.hT[half * 8 + q] for q in range(8))
            dst_ap = self.hT_full[:, half * 8:(half + 1) * 8, tc]
            sc_ap = self.colp.ap[:, half * 8:(half + 1) * 8].unsqueeze(2).to_broadcast([128, 8, 128])
            self.transposes_to(srcs, dst_tiles, dst_ap, scale_ap=sc_ap)

    def setup_hyb(self):
        kb = self.kb
        for tl in (self.sg, self.sgb, self.cn, self.cnb):
            kb.op("pool", lambda e, tl=tl: e.memset(tl.ap, 0.0), writes=(tl,))
        kb.op("pool", lambda e: e.memset(self.vext.ap, 1.0), writes=(self.vext,))
        hm = self.hmul
        kb.op("pool", lambda e: e.memset(hm.ap, -1.0), writes=(hm,))
        kb.op("pool", lambda e: e.memset(hm.ap[:, 16:20], 0.0), writes=(hm,))
        kb.op("act", lambda e: e.activation(out=hm.ap[:, 8:16], in_=self.bp.ap[:, BP_G_ALOG:BP_G_ALOG + 8],
                                            func=AF.Exp), reads=(self.bp, hm), writes=(hm,))
        kb.op("dve", lambda e: e.tensor_scalar(out=hm.ap[:, 8:16], in0=hm.ap[:, 8:16], scalar1=-1.0, scalar2=None,
                                               op0=ALU.mult), reads=(hm,), writes=(hm,))

    def hyb_mixer(self, sc, ln_idx):
        kb = self.kb
        w0, _ = self.windex["hyb_in"]
        o0, _ = self.windex["hyb_out"]
        wsl = None
        for cc in range(16):
            if cc % 2 == 0:
                wsl = self.wload(w0 + cc // 2)
            pt = self.psum()
            self.fm_proj(wsl, cc % 2, pt)
            if cc < 12:
                self.conv_silu(pt, cc, self.fm[cc])
            else:
                dst = self.fm[cc]
                scl = 0.125 if cc >= 14 else 1.0
                kb.op("act", lambda e, pt=pt, dst=dst, scl=scl: e.activation(out=dst.ap, in_=pt.ap, func=AF.Copy,
                                                                             scale=scl),
                      reads=(pt,), writes=(dst,))
        for b in range(7):
            wsl = self.wload(w0 + 8 + b)
            for t in range(NT):
                pt = self.psum()
                self.tm_proj(wsl, t, pt)
                gt = self.gates[t]
                if b < 2:
                    fn, scl = AF.Silu, 1.0
                elif b < 4:
                    fn, scl = AF.Sigmoid, 1.0
                elif b < 6:
                    fn, scl = AF.Copy, 1.0
                else:
                    fn, scl = AF.Copy, 0.125
                kb.op("act", lambda e, b=b, pt=pt, gt=gt, fn=fn, scl=scl: e.activation(
                    out=gt.ap[:, b * 256:(b + 1) * 256], in_=pt.ap[:, 0:256], func=fn, scale=scl),
                    reads=(pt,), writes=(gt,))
        wsl = self.wload(w0 + 15)
        bp = self.bp
        for t in range(NT):
            pt = self.psum()
            self.tm_proj(wsl, t, pt, 32)
            sm = self.sm[t]
            kb.op("dve", lambda e, pt=pt, sm=sm: e.tensor_tensor(
                out=sm.ap[:, 0, 0:24], in0=pt.ap[:, 0:24], in1=bp.ap[:, BP_H_BIAS:BP_H_BIAS + 24], op=ALU.add),
                reads=(pt, bp), writes=(sm,))
            kb.op("dve", lambda e, sm=sm: e.tensor_tensor(
                out=sm.ap[:, 0, 0:24], in0=sm.ap[:, 0, 0:24], in1=bp.ap[:, BP_H_SGN:BP_H_SGN + 24], op=ALU.mult),
                reads=(sm, bp), writes=(sm,))
            kb.op("act", lambda e, sm=sm: e.activation(out=sm.ap[:, 1, 0:24], in_=sm.ap[:, 0, 0:24], func=AF.Exp),
                  reads=(sm,), writes=(sm,))
            kb.op("act", lambda e, sm=sm: e.activation(out=sm.ap[:, 2, 0:24], in_=sm.ap[:, 1, 0:24], func=AF.Ln,
                                                       bias=1.0, scale=1.0), reads=(sm,), writes=(sm,))
            kb.op("dve", lambda e, sm=sm: e.tensor_tensor(out=sm.ap[:, 3, 0:24], in0=sm.ap[:, 2, 0:24],
                                                          in1=self.hmul.ap, op=ALU.mult),
                  reads=(sm, self.hmul), writes=(sm,))
            kb.op("dve", lambda e, sm=sm: e.tensor_copy(out=sm.ap[:, 3, 16:20], in_=sm.ap[:, 0, 16:20]),
                  reads=(sm,), writes=(sm,))
        for t in range(NT):
            self.hyb_chunk(t)
        self.out_proj_ln(o0, 8, ln_idx)

    def hyb_chunk(self, t):
        kb = self.kb
        tc = slice(t * 128, (t + 1) * 128)
        cf, hv, hs, sm, gt = self.cf, self.hv, self.hs, self.sm[t], self.gates[t]
        raw = self.raw
        A_xs, A_xdt, A_xw, A_xD, A_yb = (self.xs_tm, self.xdt, self.xw, self.xD, self.ybuf)

        def v8(h):
            return h[:].bitcast(F32).rearrange("p (h c) -> p h c", h=8)

        Y = [(A_xs, v8(raw["xs_tm"])), (A_xdt, v8(raw["xdt"]))]
        X = [(A_xw, v8(raw["xw"])), (A_xD, v8(raw["xD"]))]
        P_t, P = A_yb, raw["ybuf"][:, 0:1024].rearrange("p (h c) -> p h c", h=8)
        dg_t, dg = A_yb, raw["ybuf"][:, 1024:2048].rearrange("p (h c) -> p h c", h=8)
        tt_t, tt = self.ysq_h[0], raw["ysq"][:, 0:1024].rearrange("p (h c) -> p h c", h=8)
        at_t, AT = self.ysq_h[1], raw["ysq"][:, 1024:1536].bitcast(BF16).rearrange("p (h c) -> p h c", h=8)
        identf = cf.ap[:, C_ID, :]
        onesf = cf.ap[:, C_ONES, :]

        def bcl(ap2d, n, c):
            return ap2d.unsqueeze(2).to_broadcast([128, n, c])

        def bcm(ap2d, n, c):
            return ap2d.unsqueeze(1).to_broadcast([128, n, c])

        srcs = [(self.fm[q], self.fm[q].ap[:, tc]) for q in range(8)]
        self.transposes_to(srcs, (self.qk_tm,), self.qk_tm.ap, evac="dve")
        srcs = [(self.fm[8 + q], self.fm[8 + q].ap[:, tc]) for q in range(4)]
        self.transposes_to(srcs, (self.v_tm,), self.v_tm.ap, evac="act")
        sqv = raw["ybuf"][:, 1024:2048]
        kb.op("act", lambda e: e.activation(out=sqv, in_=self.qk_tm.ap, func=AF.Square),
              reads=(self.qk_tm,), writes=(dg_t,))
        kb.op("dve", lambda e: e.reduce_sum(out=hs.ap[:, 0, :], in_=sqv.rearrange("p (h c) -> p h c", h=16),
                                            axis=AX.X), reads=(dg_t,), writes=(hs,))
        kb.op("act", lambda e: e.activation(out=hs.ap[:, 1, :], in_=hs.ap[:, 0, :], func=AF.Ln, bias=1e-6,
                                            scale=1.0), reads=(hs,), writes=(hs,))
        p = self.psum()
        kb.op("pe", lambda e: e.matmul(p.ap[:, 0:24], lhsT=cf.ap[:, C_TRI, :], rhs=sm.ap[:, 3, 0:24],
                                       start=True, stop=True), reads=(cf, sm), writes=(p,))
        kb.op("pe", lambda e: e.matmul(p.ap[:, 32:56], lhsT=onesf, rhs=sm.ap[:, 3, 0:24],
                                       start=True, stop=True), reads=(cf, sm), writes=(p,))
        H = lambda i, n=8: hv.ap[:, i, 0:n]

        def dv(fn, reads, writes=(hv,)):
            kb.op("dve", fn, reads=reads, writes=writes)

        def ac(fn, reads, writes=(hv,)):
            kb.op("act", fn, reads=reads, writes=writes)

        dv(lambda e: e.tensor_copy(out=H(0), in_=p.ap[:, 8:16]), (p,))
        dv(lambda e: e.tensor_scalar(out=H(1), in0=hs.ap[:, 1, 8:16], scalar1=-0.5, scalar2=None,
                                     op0=ALU.mult), (hs,))
        dv(lambda e: e.tensor_scalar(out=H(2), in0=hs.ap[:, 1, 0:8], scalar1=-0.5, scalar2=-math.log(8.0),
                                     op0=ALU.mult, op1=ALU.add), (hs,))
        dv(lambda e: e.tensor_tensor(out=H(2), in0=H(2), in1=H(0), op=ALU.add), (hv,))
        dv(lambda e: e.tensor_tensor(out=H(3), in0=H(0), in1=H(1), op=ALU.subtract), (hv,))
        dv(lambda e: e.tensor_tensor(out=H(4), in0=H(0), in1=H(1), op=ALU.add), (hv,))
        dv(lambda e: e.tensor_tensor(out=H(4), in0=H(4), in1=sm.ap[:, 3, 0:8], op=ALU.add), (hv, sm))
        ac(lambda e: e.activation(out=H(5), in_=H(2), func=AF.Exp), (hv,))
        dv(lambda e: e.tensor_tensor(out=H(6), in0=p.ap[:, 40:48], in1=H(3), op=ALU.subtract), (p, hv))
        ac(lambda e: e.activation(out=H(6), in_=H(6), func=AF.Exp), (hv,))
        ac(lambda e: e.activation(out=H(7), in_=H(4), func=AF.Exp), (hv,))
        ac(lambda e: e.activation(out=H(8), in_=sm.ap[:, 3, 0:8], func=AF.Exp), (sm,))
        ac(lambda e: e.activation(out=H(9), in_=p.ap[:, 40:48], func=AF.Exp), (p,))
        dv(lambda e: e.tensor_copy(out=H(10, 4), in_=p.ap[:, 20:24]), (p,))
        dv(lambda e: e.tensor_tensor(out=H(11, 4), in0=H(10, 4), in1=sm.ap[:, 3, 16:20], op=ALU.subtract),
           (hv, sm))
        ac(lambda e: e.activation(out=H(12, 4), in_=H(10, 4), func=AF.Exp), (hv,))
        dv(lambda e: e.tensor_tensor(out=H(13, 4), in0=p.ap[:, 52:56], in1=H(11, 4), op=ALU.subtract), (p, hv))
        ac(lambda e: e.activation(out=H(13, 4), in_=H(13, 4), func=AF.Exp), (hv,))
        ac(lambda e: e.activation(out=H(14, 4), in_=p.ap[:, 52:56], func=AF.Exp), (p,))

        def kT(h):
            return self.fm[4 + h // 2], self.fm[4 + h // 2].ap[(h % 2) * 64:(h % 2) * 64 + 64, tc]

        def qT(h):
            return self.fm[h // 2], self.fm[h // 2].ap[(h % 2) * 64:(h % 2) * 64 + 64, tc]

        def decay(row_ap, col_ap, mask_idx, n):
            self.decay_block(row_ap, col_ap, mask_idx, n, dg, dg_t, tt, tt_t, hv, nb=8)

        pkk = [self.psum(), self.psum()]
        pkq = [self.psum(), self.psum()]
        for h in range(8):
            kt, kap = kT(h)
            qt, qap = qT(h)
            cs = slice((h // 2) * 128, (h // 2) * 128 + 128)
            kb.op("pe", lambda e, h=h, kap=kap, cs=cs: e.matmul(pkk[h % 2].ap[:, cs], lhsT=kap, rhs=kap,
                                                               start=True, stop=True),
                  reads=(kt,), writes=(pkk[h % 2],))
            kb.op("pe", lambda e, h=h, kap=kap, qap=qap, cs=cs: e.matmul(pkq[h % 2].ap[:, cs], lhsT=kap, rhs=qap,
                                                                        start=True, stop=True),
                  reads=(kt, qt), writes=(pkq[h % 2],))
        decay(H(4), H(3), C_MSTR, 8)
        Y0t, Y0 = Y[0]
        for par in range(2):
            kb.op("dve", lambda e, par=par: e.scalar_tensor_tensor(
                out=Y0[:, par:8:2, :], in0=tt[:, par:8:2, :], scalar=-1.0,
                in1=pkk[par].ap.rearrange("p (h c) -> p h c", h=4), op0=ALU.mult, op1=ALU.mult),
                reads=(tt_t, pkk[par]), writes=(Y0t,))
        decay(H(2), H(3), C_MINC, 8)
        for par in range(2):
            kb.op("dve", lambda e, par=par: e.tensor_tensor(
                out=AT[:, par:8:2, :], in0=tt[:, par:8:2, :],
                in1=pkq[par].ap.rearrange("p (h c) -> p h c", h=4), op=ALU.mult),
                reads=(tt_t, pkq[par]), writes=(at_t,))
        X0t, X0 = X[0]
        px = [self.psum(), self.psum()]
        for h in range(8):
            cs = slice((h % 4) * 128, (h % 4) * 128 + 128)
            kb.op("pe", lambda e, h=h, cs=cs: e.transpose(px[h // 4].ap[:, cs], Y0[:, h, :], identf),
                  reads=(Y0t, cf), writes=(px[h // 4],))
        kb.op("act", lambda e: e.activation(out=X0[:, 0:4, :], in_=px[0].ap.rearrange("p (h c) -> p h c", h=4),
                                            func=AF.Copy), reads=(px[0],), writes=(X0t,))
        kb.op("dve", lambda e: e.tensor_copy(out=X0[:, 4:8, :], in_=px[1].ap.rearrange("p (h c) -> p h c", h=4)),
              reads=(px[1],), writes=(X0t,))
        kb.op("pool", lambda e: e.tensor_tensor(out=P, in0=Y0, in1=bcm(identf, 8, 128), op=ALU.add),
              reads=(Y0t, cf), writes=(P_t,))
        dg_m = self.cacc[0].ap.rearrange("p (h c) -> p h c", h=4)
        tt_m = self.cacc[1].ap.rearrange("p (h c) -> p h c", h=4)
        mstate = {}

        def mlstm_part1():
            def mqT(m):
                return self.fm[12 + m // 2], self.fm[12 + m // 2].ap[(m % 2) * 64:(m % 2) * 64 + 64, tc]

            def mkT(m):
                return self.fm[14 + m // 2], self.fm[14 + m // 2].ap[(m % 2) * 64:(m % 2) * 64 + 64, tc]

            vext = self.vext
            kb.op("pool", lambda e: e.tensor_copy(out=vext.ap[:, :, 0:128],
                                                  in_=gt.ap[:, 1024:1536].rearrange("p (h c) -> p h c", h=4)),
                  reads=(gt,), writes=(vext,))
            kb.op("pool", lambda e: e.tensor_tensor(out=self.kwm.ap.rearrange("p (h c) -> p h c", h=4),
                                                    in0=gt.ap[:, 1536:1792].rearrange("p (h c) -> p h c", h=4),
                                                    in1=bcl(H(13, 4), 4, 64), op=ALU.mult),
                  reads=(gt, hv), writes=(self.kwm,))
            pmk = [self.psum(), self.psum()]
            for m in range(4):
                kt, kap = mkT(m)
                qt, qap = mqT(m)
                kb.op("pe", lambda e, m=m, kap=kap, qap=qap: e.matmul(pmk[m % 2].ap[:, (m // 2) * 128:(m // 2) * 128 + 128],
                                                                     lhsT=kap, rhs=qap, start=True, stop=True),
                      reads=(kt, qt), writes=(pmk[m % 2],))
            self.decay_block(H(10, 4), H(11, 4), C_MINC, 4, dg_m, self.cacc[0], tt_m, self.cacc[1], hv, nb=8)
            for par in range(2):
                kb.op("dve", lambda e, par=par: e.tensor_tensor(
                    out=self.atm.ap[:, par:4:2, :], in0=tt_m[:, par:4:2, :],
                    in1=pmk[par].ap[:, 0:256].rearrange("p (h c) -> p h c", h=2), op=ALU.mult),
                    reads=(self.cacc[1], pmk[par]), writes=(self.atm,))
            mstate["vext"] = vext
            mstate["mqT"] = mqT

        def mlstm_part2():
            vext = mstate["vext"]
            mqT = mstate["mqT"]
            pn = [self.psum(), self.psum()]
            pq = [self.psum(), self.psum()]
            for m in range(4):
                r0 = (m % 2) * 64
                cs = slice((m % 2) * 256, (m % 2) * 256 + 129)
                cq = slice((m // 2) * 256, (m // 2) * 256 + 129)
                qt, qap = mqT(m)
                kb.op("pe", lambda e, m=m, cs=cs: e.matmul(pn[m // 2].ap[:, cs], lhsT=self.atm.ap[:, m, :],
                                                           rhs=vext.ap[:, m, :], start=True, stop=True),
                      reads=(self.atm, vext), writes=(pn[m // 2],))
                kb.op("pe", lambda e, m=m, cq=cq, r0=r0, qap=qap: e.matmul(pq[m % 2].ap[:, cq], lhsT=qap,
                                                                           rhs=self.cnb.ap[r0:r0 + 64, m // 2, :],
                                                                           start=True, stop=True),
                      reads=(qt, self.cnb), writes=(pq[m % 2],))
            hr = self.hraw
            for par in range(2):
                kb.op("dve", lambda e, par=par: e.tensor_tensor(
                    out=hr.ap[:, par:4:2, :], in0=pq[par].ap.rearrange("p (h c) -> p h c", h=2)[:, :, 0:129],
                    in1=bcl(hv.ap[:, 12, par:4:2], 2, 129), op=ALU.mult), reads=(pq[par], hv), writes=(hr,))
            for b in range(2):
                kb.op("dve", lambda e, b=b: e.tensor_tensor(
                    out=hr.ap[:, 2 * b:2 * b + 2, :], in0=hr.ap[:, 2 * b:2 * b + 2, :],
                    in1=pn[b].ap.rearrange("p (h c) -> p h c", h=2)[:, :, 0:129], op=ALU.add),
                    reads=(hr, pn[b]), writes=(hr,))
            kb.op("act", lambda e: e.activation(out=hs.ap[:, 3, 8:12], in_=hr.ap[:, :, 128], func=AF.Abs),
                  reads=(hr,), writes=(hs,))
            kb.op("dve", lambda e: e.tensor_scalar(out=hs.ap[:, 3, 8:12], in0=hs.ap[:, 3, 8:12], scalar1=1.0,
                                                   scalar2=None, op0=ALU.max), reads=(hs,), writes=(hs,))
            kb.op("dve", lambda e: e.reciprocal(out=hs.ap[:, 3, 12:16], in_=hs.ap[:, 3, 8:12]), reads=(hs,), writes=(hs,))
            kb.op("dve", lambda e: e.tensor_tensor(out=hr.ap[:, :, 0:128], in0=hr.ap[:, :, 0:128],
                                                   in1=bcl(hs.ap[:, 3, 12:16], 4, 128), op=ALU.mult),
                  reads=(hr, hs), writes=(hr,))
            kb.op("dve", lambda e: e.tensor_tensor(out=self.ybf.ap[:, 512:1024].rearrange("p (h c) -> p h c", h=4),
                                                   in0=hr.ap[:, :, 0:128],
                                                   in1=gt.ap[:, 512:1024].rearrange("p (h c) -> p h c", h=4),
                                                   op=ALU.mult), reads=(hr, gt), writes=(self.ybf,))

        def mlstm_part3():
            vext = mstate["vext"]
            psm = [self.psum(), self.psum()]
            for m in range(4):
                cs = slice((m % 2) * 256, (m % 2) * 256 + 129)
                kb.op("pe", lambda e, m=m, cs=cs: e.matmul(psm[m // 2].ap[:, cs],
                                                           lhsT=self.kwm.ap[:, (m // 2) * 128:(m // 2) * 128 + 128],
                                                           rhs=vext.ap[:, m, :], start=True, stop=True),
                      reads=(self.kwm, vext), writes=(psm[m // 2],))
            for m in range(4):
                r0 = (m % 2) * 64
                cs = slice((m % 2) * 256, (m % 2) * 256 + 129)
                kb.op("dve", lambda e, m=m, r0=r0, cs=cs: e.scalar_tensor_tensor(
                    out=self.cn.ap[r0:r0 + 64, m // 2, :], in0=self.cn.ap[r0:r0 + 64, m // 2, :],
                    scalar=hv.ap[r0:r0 + 64, 14, m:m + 1], in1=psm[m // 2].ap[r0:r0 + 64, cs],
                    op0=ALU.mult, op1=ALU.add), reads=(self.cn, hv, psm[m // 2]), writes=(self.cn,))
            kb.op("act", lambda e: e.activation(out=self.cnb.ap, in_=self.cn.ap, func=AF.Copy),
                  reads=(self.cn,), writes=(self.cnb,))

        for k in range(1, 7):
            if k == 2:
                mlstm_part1()
            elif k == 4:
                mlstm_part2()
            elif k == 6:
                mlstm_part3()
            (Ypt, Yp), (Xpt, Xp) = Y[(k - 1) % 2], X[(k - 1) % 2]
            (Ynt, Yn), (Xnt, Xn) = Y[k % 2], X[k % 2]
            px = [self.psum(), self.psum()]
            for h in range(8):
                cs = slice((h % 4) * 128, (h % 4) * 128 + 128)
                kb.op("pe", lambda e, h=h, cs=cs, Yp=Yp, Xp=Xp, px=px: e.matmul(
                    px[h // 4].ap[:, cs], lhsT=Yp[:, h, :], rhs=Xp[:, h, :], start=True, stop=True),
                    reads=(Ypt, Xpt), writes=(px[h // 4],))
            kb.op("act", lambda e, Xn=Xn, px=px: e.activation(out=Xn[:, 0:4, :],
                                                              in_=px[0].ap.rearrange("p (h c) -> p h c", h=4),
                                                              func=AF.Copy), reads=(px[0],), writes=(Xnt,))
            kb.op("dve", lambda e, Xn=Xn, px=px: e.tensor_copy(out=Xn[:, 4:8, :],
                                                               in_=px[1].ap.rearrange("p (h c) -> p h c", h=4)),
                  reads=(px[1],), writes=(Xnt,))
            if k < 6:
                py = [self.psum(), self.psum()]
                for h in range(8):
                    cs = slice((h % 4) * 128, (h % 4) * 128 + 128)
                    kb.op("pe", lambda e, h=h, cs=cs, Yp=Yp, Xp=Xp, py=py: e.matmul(
                        py[h // 4].ap[:, cs], lhsT=Xp[:, h, :], rhs=Yp[:, h, :], start=True, stop=True),
                        reads=(Ypt, Xpt), writes=(py[h // 4],))
                kb.op("act", lambda e, Yn=Yn, py=py: e.activation(out=Yn[:, 0:4, :],
                                                                  in_=py[0].ap.rearrange("p (h c) -> p h c", h=4),
                                                                  func=AF.Copy), reads=(py[0],), writes=(Ynt,))
                kb.op("dve", lambda e, Yn=Yn, py=py: e.tensor_copy(out=Yn[:, 4:8, :],
                                                                   in_=py[1].ap.rearrange("p (h c) -> p h c", h=4)),
                      reads=(py[1],), writes=(Ynt,))
            pp = [self.psum(), self.psum()]
            for h in range(8):
                cs = slice((h % 4) * 128, (h % 4) * 128 + 128)
                kb.op("pe", lambda e, h=h, cs=cs, Xn=Xn, pp=pp: e.matmul(
                    pp[h // 4].ap[:, cs], lhsT=Xn[:, h, :], rhs=P[:, h, :], start=True, stop=True),
                    reads=(Xnt, P_t), writes=(pp[h // 4],))
            for half in range(2):
                kb.op("dve", lambda e, half=half, pp=pp: e.tensor_tensor(
                    out=P[:, half * 4:(half + 1) * 4, :], in0=P[:, half * 4:(half + 1) * 4, :],
                    in1=pp[half].ap.rearrange("p (h c) -> p h c", h=4), op=ALU.add),
                    reads=(P_t, pp[half]), writes=(P_t,))
        k_tm = self.qk_tm.ap[:, 512:1024]
        kb.op("dve", lambda e: e.tensor_tensor(out=self.bv.ap.rearrange("p (h c) -> p h c", h=8),
                                               in0=self.v_tm.ap.rearrange("p (h c) -> p h c", h=8),
                                               in1=bcl(H(8), 8, 64), op=ALU.mult),
              reads=(self.v_tm, hv), writes=(self.bv,))
        kb.op("dve", lambda e: e.tensor_tensor(out=self.bk.ap.rearrange("p (h c) -> p h c", h=8),
                                               in0=k_tm.rearrange("p (h c) -> p h c", h=8),
                                               in1=bcl(H(7), 8, 64), op=ALU.mult),
              reads=(self.qk_tm, hv), writes=(self.bk,))
        kb.op("pool", lambda e: e.tensor_tensor(out=self.kw.ap.rearrange("p (h c) -> p h c", h=8),
                                                in0=k_tm.rearrange("p (h c) -> p h c", h=8),
                                                in1=bcl(H(6), 8, 64), op=ALU.mult),
              reads=(self.qk_tm, hv), writes=(self.kw,))
        pu0 = self.psum()
        pwt = [self.psum(), self.psum()]
        for h in range(8):
            cs = slice((h % 4) * 128, (h % 4) * 128 + 128)
            kb.op("pe", lambda e, h=h: e.matmul(pu0.ap[:, h * 64:(h + 1) * 64], lhsT=P[:, h, :],
                                                rhs=self.bv.ap[:, h * 64:(h + 1) * 64], start=True, stop=True),
                  reads=(P_t, self.bv), writes=(pu0,))
            kb.op("pe", lambda e, h=h, cs=cs: e.matmul(pwt[h // 4].ap[:, cs],
                                                       lhsT=self.bk.ap[:, (h // 2) * 128:(h // 2) * 128 + 128],
                                                       rhs=P[:, h, :], start=True, stop=True),
                  reads=(P_t, self.bk), writes=(pwt[h // 4],))
        kb.op("act", lambda e: e.activation(out=self.u0.ap, in_=pu0.ap, func=AF.Copy), reads=(pu0,), writes=(self.u0,))
        for h in range(8):
            r0 = (h % 2) * 64
            cs = slice((h % 4) * 128, (h % 4) * 128 + 128)
            eng = "dve" if h % 2 == 0 else "act"
            if eng == "dve":
                kb.op("dve", lambda e, h=h, r0=r0, cs=cs: e.tensor_copy(out=self.wtb.ap[r0:r0 + 64, h, :],
                                                                        in_=pwt[h // 4].ap[r0:r0 + 64, cs]),
                      reads=(pwt[h // 4],), writes=(self.wtb,))
            else:
                kb.op("act", lambda e, h=h, r0=r0, cs=cs: e.activation(out=self.wtb.ap[r0:r0 + 64, h, :],
                                                                       in_=pwt[h // 4].ap[r0:r0 + 64, cs],
                                                                       func=AF.Copy),
                      reads=(pwt[h // 4],), writes=(self.wtb,))
        pws = [self.psum(), self.psum()]
        for h in range(8):
            r0 = (h % 2) * 64
            kb.op("pe", lambda e, h=h, r0=r0: e.matmul(pws[h % 2].ap[:, (h // 2) * 64:(h // 2) * 64 + 64],
                                                       lhsT=self.wtb.ap[r0:r0 + 64, h, :],
                                                       rhs=self.sgb.ap[r0:r0 + 64, h // 2, :], start=True, stop=True),
                  reads=(self.wtb, self.sgb), writes=(pws[h % 2],))
        u3 = self.u.ap.rearrange("p (h c) -> p h c", h=8)
        u03 = self.u0.ap.rearrange("p (h c) -> p h c", h=8)
        for par in range(2):
            kb.op("dve", lambda e, par=par: e.tensor_tensor(
                out=u3[:, par:8:2, :], in0=u03[:, par:8:2, :],
                in1=pws[par].ap[:, 0:256].rearrange("p (h c) -> p h c", h=4), op=ALU.subtract),
                reads=(self.u0, pws[par]), writes=(self.u,))
        po = self.psum()
        pqs = [self.psum(), self.psum()]
        for h in range(8):
            r0 = (h % 2) * 64
            qt, qap = qT(h)
            kb.op("pe", lambda e, h=h: e.matmul(po.ap[:, h * 64:(h + 1) * 64], lhsT=AT[:, h, :],
                                                rhs=self.u.ap[:, h * 64:(h + 1) * 64], start=True, stop=True),
                  reads=(at_t, self.u), writes=(po,))
            kb.op("pe", lambda e, h=h, r0=r0, qap=qap: e.matmul(pqs[h % 2].ap[:, (h // 2) * 64:(h // 2) * 64 + 64],
                                                                lhsT=qap, rhs=self.sgb.ap[r0:r0 + 64, h // 2, :],
                                                                start=True, stop=True),
                  reads=(qt, self.sgb), writes=(pqs[h % 2],))
        ob = self.ytmp
        ob3 = ob.ap.rearrange("p (h c) -> p h c", h=8)
        for par in range(2):
            kb.op("dve", lambda e, par=par: e.tensor_tensor(
                out=ob3[:, par:8:2, :], in0=pqs[par].ap[:, 0:256].rearrange("p (h c) -> p h c", h=4),
                in1=bcl(hv.ap[:, 5, par:8:2], 4, 64), op=ALU.mult),
                reads=(pqs[par], hv), writes=(ob,))
        kb.op("dve", lambda e: e.tensor_tensor(out=ob.ap, in0=ob.ap, in1=po.ap, op=ALU.add),
              reads=(ob, po), writes=(ob,))
        psu = self.psum()
        for h in range(8):
            kb.op("pe", lambda e, h=h: e.matmul(psu.ap[:, h * 64:(h + 1) * 64],
                                                lhsT=self.kw.ap[:, (h // 2) * 128:(h // 2) * 128 + 128],
                                                rhs=self.u.ap[:, h * 64:(h + 1) * 64], start=True, stop=True),
                  reads=(self.kw, self.u), writes=(psu,))
        for h in range(8):
            r0 = (h % 2) * 64
            kb.op("dve", lambda e, h=h, r0=r0: e.scalar_tensor_tensor(
                out=self.sg.ap[r0:r0 + 64, h // 2, :], in0=self.sg.ap[r0:r0 + 64, h // 2, :],
                scalar=hv.ap[r0:r0 + 64, 9, h:h + 1], in1=psu.ap[r0:r0 + 64, h * 64:(h + 1) * 64],
                op0=ALU.mult, op1=ALU.add), reads=(self.sg, hv, psu), writes=(self.sg,))
        kb.op("act", lambda e: e.activation(out=self.sgb.ap, in_=self.sg.ap, func=AF.Copy),
              reads=(self.sg,), writes=(self.sgb,))
        sq2 = raw["ybuf"][:, 1024:1536]
        kb.op("act", lambda e: e.activation(out=sq2, in_=ob.ap, func=AF.Square), reads=(ob,), writes=(dg_t,))
        kb.op("dve", lambda e: e.reduce_sum(out=hs.ap[:, 2, 0:8], in_=sq2.rearrange("p (h c) -> p h c", h=8),
                                            axis=AX.X), reads=(dg_t,), writes=(hs,))
        kb.op("act", lambda e: e.activation(out=hs.ap[:, 2, 8:16], in_=hs.ap[:, 2, 0:8], func=AF.Sqrt,
                                            bias=float(RMS_EPS), scale=1.0 / 64.0), reads=(hs,), writes=(hs,))
        kb.op("dve", lambda e: e.reciprocal(out=hs.ap[:, 3, 0:8], in_=hs.ap[:, 2, 8:16]), reads=(hs,), writes=(hs,))
        kb.op("dve", lambda e: e.tensor_tensor(out=ob.ap.rearrange("p (h c) -> p h c", h=8),
                                               in0=ob.ap.rearrange("p (h c) -> p h c", h=8),
                                               in1=bcl(hs.ap[:, 3, 0:8], 8, 64), op=ALU.mult),
              reads=(ob, hs), writes=(ob,))
        kb.op("pool", lambda e: e.tensor_tensor(out=ob.ap.rearrange("p (h c) -> p h c", h=8),
                                                in0=ob.ap.rearrange("p (h c) -> p h c", h=8),
                                                in1=bcm(self.bp.ap[:, BP_G_NW:BP_G_NW + 64], 8, 64), op=ALU.mult),
              reads=(ob, self.bp), writes=(ob,))
        kb.op("dve", lambda e: e.tensor_tensor(out=self.ybf.ap[:, 0:512], in0=ob.ap, in1=gt.ap[:, 0:512], op=ALU.mult),
              reads=(ob, gt), writes=(self.ybf,))

        srcs = [(self.ybf, self.ybf.ap[:, q * 128:(q + 1) * 128]) for q in range(8)]
        self.transposes_to(srcs, tuple(self.hT[q] for q in range(8)), self.hT_full[:, 0:8, tc])

    def body(self):
        kb = self.kb
        self.prologue()
        for sc in range(self.nsc):
            if sc > 0:
                self.load_x(sc)
            for t in range(NT):
                self.to_xT(t)
            for l in self.layers:
                self.ffn_ln(l, "pre", sc, l * 3 + 0)
                if l % 2 == 1:
                    self.ssd_mixer(sc, l * 3 + 1)
                else:
                    self.hyb_mixer(sc, l * 3 + 1)
                self.ffn_ln(l, "post", sc, l * 3 + 2)
            self.store_y(sc)
        kb.finish()


def prep_inputs(inp, layers):
    ws = make_wstream(inp, layers)
    lnp = np.zeros((DEPTH * 3, 2, D_MODEL), np.float32)
    for l in range(DEPTH):
        lnp[l * 3 + 0, 0] = inp["ln_pre_g"][l]
        lnp[l * 3 + 0, 1] = inp["ln_pre_b"][l]
        lnp[l * 3 + 1, 0] = inp["ln_mix_g"][l]
        lnp[l * 3 + 1, 1] = inp["ln_mix_b"][l]
        lnp[l * 3 + 2, 0] = inp["ln_post_g"][l]
        lnp[l * 3 + 2, 1] = inp["ln_post_b"][l]
    j = np.arange(128)[:, None]
    i = np.arange(128)[None, :]
    cst = np.zeros((128, 5, 128), np.float32)
    cst[:, C_ID, :] = np.eye(128)
    cst[:, C_ONES, :] = 1.0
    cst[:, C_MINC, :] = np.where(j <= i, 0.0, -1e30)
    cst[:, C_MSTR, :] = np.where(j < i, 0.0, -1e30)
    cst[:, C_TRI, :] = (j <= i)
    bpar = np.zeros((NBP,), np.float32)
    bpar[BP_SSD_DTB:BP_SSD_DTB + 32] = inp["ssd_dt_bias"][0]
    bpar[BP_SSD_ALOG:BP_SSD_ALOG + 32] = inp["ssd_a_log"][0]
    bpar[BP_SSD_D:BP_SSD_D + 32] = inp["ssd_d_skip"][0]
    bpar[BP_G_ALOG:BP_G_ALOG + 8] = inp["gdn_a_log"][0]
    bpar[BP_G_DTB:BP_G_DTB + 8] = inp["gdn_dt_bias"][0]
    bpar[BP_G_NW:BP_G_NW + 64] = inp["gdn_norm_w"][0]
    bpar[BP_M_IB:BP_M_IB + 4] = inp["mlstm_i_bias"][0]
    bpar[BP_M_FB:BP_M_FB + 4] = inp["mlstm_f_bias"][0]
    bpar[BP_H_SGN:BP_H_SGN + 24] = np.array([-1] * 8 + [1] * 8 + [1] * 4 + [-1] * 4, np.float32)
    bpar[BP_H_BIAS + 8:BP_H_BIAS + 16] = inp["gdn_dt_bias"][0]
    bpar[BP_H_BIAS + 16:BP_H_BIAS + 20] = inp["mlstm_i_bias"][0]
    bpar[BP_H_BIAS + 20:BP_H_BIAS + 24] = inp["mlstm_f_bias"][0]
    convw = np.zeros((128, 36, 5), np.float32)
    hw = inp["hyb_conv_w"][0]
    convw[:, 0:12, 0:4] = hw.reshape(4, 12, 128).transpose(2, 1, 0)
    sw = inp["ssd_conv_w"][0]
    convw[:, 12:36, 0:4] = sw.reshape(4, 24, 128).transpose(2, 1, 0)
    convw[:, 12:36, 4] = inp["ssd_conv_b"][0].reshape(24, 128).T
    colp = np.ascontiguousarray(inp["ssd_norm_w"][0].reshape(16, 128).T)
    return ws, lnp, dict(cst=cst, bpar=bpar, convw=convw, colp=colp)


def kernel(**inputs):
    inp = {k: np.asarray(v) for k, v in inputs.items()}
    layers = list(range(DEPTH))
    ws, lnp, small = prep_inputs(inp, layers)
    warr = ws.array()
    prog = Prog(SEQ, layers, ws.n, ws.index)
    nc = prog.build()
    x = inp["x"]
    in_maps = []
    for b in range(BATCH):
        m = {"x": np.ascontiguousarray(x[b]), "wsrc": warr, "lnp": lnp}
        m.update(small)
        in_maps.append(m)
    res = run_bass_kernel_spmd(nc, in_maps, core_ids=list(range(BATCH)))
    out = np.stack([res.results[b]["y"] for b in range(BATCH)], axis=0)
    return out.astype(np.float32)
```

```python
import math
from contextlib import ExitStack

import numpy as np
import concourse.bass as bass
import concourse.mybir as mybir
from concourse.bass_utils import run_bass_kernel_spmd

F32 = mybir.dt.float32
BF16 = mybir.dt.bfloat16
ALU = mybir.AluOpType
AF = mybir.ActivationFunctionType
AX = mybir.AxisListType

D_MODEL = 1024
BATCH = 4
SEQ = 8192
DEPTH = 2
DN_ALPHA = (2 * DEPTH) ** 0.25
LN_EPS = 1e-5
D_FF = 2816
NFC = D_FF // 128
TS = 512
NT = TS // 128
WBLK = 2048
NSLOT = 5
NDS = 12
NBP = 256
BP_SSD_DTB, BP_SSD_ALOG, BP_SSD_D = 0, 32, 64
BP_G_ALOG, BP_G_DTB, BP_G_NW, BP_M_IB, BP_M_FB = 96, 104, 112, 176, 180
C_ID, C_ONES, C_MINC, C_MSTR, C_TRI = 0, 1, 2, 3, 4
RMS_EPS = 1e-6
BP_H_SGN, BP_H_BIAS = 192, 216

ENGS = ("sp", "act", "dve", "pe", "pool")


class Tile:
    __slots__ = ("ap", "w", "rs", "name")

    def __init__(self, ap, name=""):
        self.ap = ap
        self.name = name
        self.w = None
        self.rs = {}


class Plan:
    def __init__(self):
        self.needed = set()
        self.cnt = None

    def finalize(self):
        per = {}
        for (p, s) in self.needed:
            per.setdefault(p, []).append(s)
        self.cnt = {}
        for p, lst in per.items():
            lst.sort()
            self.cnt[p] = {s: i + 1 for i, s in enumerate(lst)}


class KB:
    def __init__(self, nc, plan, sems, dsems, tiles):
        self.nc = nc
        self.plan = plan
        self.sems = sems
        self.dsems = dsems
        self.tiles = tiles
        self.cur = None
        self.eng = None

    def begin(self, cur, eng):
        self.cur = cur
        self.eng = eng
        self.seq = {e: 0 for e in ENGS}
        self.known = {e: {} for e in ENGS}
        self.ndma = 0
        self.ndma_q = [0, 0]
        self.psum_rr = 0
        self.wr_rr = 0
        self.misc = {}
        for t in self.tiles:
            t.w = None
            t.rs = {}

    def _need(self, E, P, ps, s_cur):
        if P == E:
            if E == "pe":
                return
            if ps < s_cur - 3:
                return
        kn = self.known[E]
        if kn.get(P, 0) >= ps:
            return
        kn[P] = ps
        if self.cur is None:
            self.plan.needed.add((P, ps))
        elif self.cur == E:
            if P[0] == "q":
                self.eng.wait_ge(self.dsems[int(P[1:])], 16 * ps)
            else:
                self.eng.wait_ge(self.sems[P], self.plan.cnt[P][ps])

    def _deps(self, E, reads, writes, s_cur, extra=()):
        for t in reads:
            if t.w is not None:
                self._need(E, t.w[0], t.w[1], s_cur)
        for t in writes:
            if t.w is not None:
                self._need(E, t.w[0], t.w[1], s_cur)
            for p, s in t.rs.items():
                self._need(E, p, s, s_cur)
        for (p, s) in extra:
            self._need(E, p, s, s_cur)

    def _mark(self, tok, reads, writes):
        for t in reads:
            if t.rs.get(tok[0], 0) < tok[1]:
                t.rs[tok[0]] = tok[1]
        for t in writes:
            t.w = tok
            t.rs = {}

    def op(self, E, fn, reads=(), writes=()):
        s = self.seq[E] + 1
        self.seq[E] = s
        self._deps(E, reads, writes, s)
        tok = (E, s)
        if self.cur == E:
            ins = fn(self.eng)
            if tok in self.plan.needed:
                ins.then_inc(self.sems[E], 1)
        self._mark(tok, reads, writes)
        return tok

    def dma(self, Q, out, in_, reads=(), writes=(), deps=()):
        half = NDS // 2
        qi = 0 if Q == "sp" else 1
        i = self.ndma_q[qi]
        self.ndma_q[qi] += 1
        self.ndma += 1
        j = qi * half + (i % half)
        ds = i // half + 1
        s = self.seq[Q] + 1
        self.seq[Q] = s
        extra = ((("q%d" % j), ds - 1),) if ds > 1 else ()
        extra = extra + tuple(deps)
        self._deps(Q, reads, writes, s, extra)
        tok = ("q%d" % j, ds)
        if self.cur == Q:
            self.eng.dma_start(out=out, in_=in_).then_inc(self.dsems[j], 16)
        self._mark(tok, reads, writes)
        return tok

    def wait_all_dma(self, E):
        half = NDS // 2
        s = self.seq[E] + 1
        for qi in range(2):
            n = self.ndma_q[qi]
            for jj in range(half):
                if n > jj:
                    ds = (n - 1 - jj) // half + 1
                    self._need(E, "q%d" % (qi * half + jj), ds, s)

    def finish(self):
        self.wait_all_dma("sp")


def _blocks_kc(w, cols_per_blk=256):
    K, C = w.shape
    nkc = K // 128
    assert nkc * cols_per_blk == WBLK
    nb = (C + cols_per_blk - 1) // cols_per_blk
    wp = np.zeros((K, nb * cols_per_blk), np.float32)
    wp[:, :C] = w
    wp = wp.reshape(nkc, 128, nb, cols_per_blk).transpose(2, 1, 0, 3)
    return np.ascontiguousarray(wp).reshape(nb, 128, WBLK)


def _blocks_rows(w, rows_per_blk=2):
    K, C = w.shape
    assert C == 1024 and rows_per_blk * C == WBLK
    nkc = K // 128
    nb = nkc // rows_per_blk
    wp = w.reshape(nb, rows_per_blk, 128, C).transpose(0, 2, 1, 3)
    return np.ascontiguousarray(wp).reshape(nb, 128, WBLK)


def _blocks_gu(wg, wu):
    g = wg.reshape(8, 128, NFC, 128).transpose(2, 1, 0, 3)
    u = wu.reshape(8, 128, NFC, 128).transpose(2, 1, 0, 3)
    gu = np.stack([g, u], axis=2)
    return np.ascontiguousarray(gu).reshape(NFC, 128, WBLK)


class WStream:
    def __init__(self):
        self.parts = []
        self.index = {}
        self.n = 0

    def add(self, name, blocks):
        self.index[name] = (self.n, blocks.shape[0])
        self.parts.append(blocks)
        self.n += blocks.shape[0]

    def array(self):
        return np.concatenate(self.parts, axis=0)


def make_wstream(inp, layers):
    ws = WStream()
    for l in layers:
        ws.add("pre_gu%d" % l, _blocks_gu(inp["ffn_pre_w_gate"][l], inp["ffn_pre_w_up"][l]))
        ws.add("pre_d%d" % l, _blocks_rows(inp["ffn_pre_w_down"][l]))
        ws.add("post_gu%d" % l, _blocks_gu(inp["ffn_post_w_gate"][l], inp["ffn_post_w_up"][l]))
        ws.add("post_d%d" % l, _blocks_rows(inp["ffn_post_w_down"][l]))
        if l % 2 == 1:
            w = inp["ssd_w_in"][0]
            ws.add("ssd_in", np.concatenate([_blocks_kc(w[:, 2048:5120]), _blocks_kc(w[:, 0:2048]),
                                             _blocks_kc(w[:, 5120:5152])], axis=0))
            ws.add("ssd_out", _blocks_rows(inp["ssd_w_out"][0]))
        else:
            ws.add("hyb_in", _blocks_kc(hyb_perm(inp["hyb_w_in"][0])))
            ws.add("hyb_out", _blocks_rows(inp["hyb_w_out"][0]))
    return ws


def hyb_perm(w):
    o = np.cumsum([0, 512, 512, 512, 512, 8, 8, 256, 256, 512, 512, 4, 4])
    gq, gk, gv, gz, gb, ga, mq, mk, mv, mo, mi, mf = [w[:, o[i]:o[i + 1]] for i in range(12)]
    small = np.zeros((w.shape[0], 256), np.float32)
    small[:, 0:8] = gb
    small[:, 8:16] = ga
    small[:, 16:20] = mi
    small[:, 20:24] = mf
    return np.concatenate([gq, gk, gv, mq, mk, gz, mo, mv, mk, small], axis=1)


class Prog:
    def __init__(self, ntok, layers, nblk, windex, debug_stop=None):
        self.ntok = ntok
        self.layers = layers
        self.nblk = nblk
        self.windex = windex
        self.nsc = ntok // TS
        self.debug_stop = debug_stop
        nc = bass.Bass("TRN2", target_bir_lowering=False)
        self.nc = nc
        self.x = nc.dram_tensor("x", [ntok, D_MODEL], F32, kind="ExternalInput").ap()
        self.wsrc = nc.dram_tensor("wsrc", [nblk, 128, WBLK], F32, kind="ExternalInput").ap()
        self.lnp_d = nc.dram_tensor("lnp", [DEPTH * 3, 2, D_MODEL], F32, kind="ExternalInput").ap()
        self.cst_d = nc.dram_tensor("cst", [128, 5, 128], F32, kind="ExternalInput").ap()
        self.bpar_d = nc.dram_tensor("bpar", [NBP], F32, kind="ExternalInput").ap()
        self.convw_d = nc.dram_tensor("convw", [128, 36, 5], F32, kind="ExternalInput").ap()
        self.colp_d = nc.dram_tensor("colp", [128, 16], F32, kind="ExternalInput").ap()
        self.y = nc.dram_tensor("y", [ntok, D_MODEL], F32, kind="ExternalOutput").ap()
        self.wbf = nc.dram_tensor("wbf", [nblk, 128, WBLK], BF16).ap()

    def build(self):
        nc = self.nc
        with ExitStack() as es:
            def sb(name, shape, dt):
                return es.enter_context(nc.sbuf_tensor(name, shape, dt))

            self.tiles = []

            def T(ap, name=""):
                t = Tile(ap, name)
                self.tiles.append(t)
                return t

            xtm = sb("xtm", [128, NT, D_MODEL], F32)
            self.xtm = [T(xtm[:, t, :], "xtm%d" % t) for t in range(NT)]
            xT = sb("xT", [128, 8, TS], BF16)
            self.xT_full = xT
            self.xT = [T(xT[:, :, t * 128:(t + 1) * 128], "xT%d" % t) for t in range(NT)]
            hT = sb("hT", [128, NFC, TS], BF16)
            self.hT_full = hT
            self.hT = [T(hT[:, j, :], "hT%d" % j) for j in range(NFC)]
            wring = sb("wring", [128, NSLOT, WBLK], BF16)
            self.wslot = [T(wring[:, s, :], "w%d" % s) for s in range(NSLOT)]
            lnp = sb("lnpb", [128, 1, 2, D_MODEL], F32)
            self.lnp = [T(lnp[:, 0, :, :], "lnp0")]
            cf = sb("cf", [128, 5, 128], F32)
            self.cf = T(cf[:], "cf")
            bp = sb("bp", [128, NBP], F32)
            self.bp = T(bp[:], "bp")
            cw = sb("cw", [128, 36, 5], F32)
            self.cw = T(cw[:], "cw")
            colp = sb("colp_sb", [128, 16], F32)
            self.colp = T(colp[:], "colp")
            aneg = sb("aneg", [128, 32], F32)
            self.aneg = T(aneg[:], "aneg")
            hist = sb("hist", [128, 36, 3], F32)
            self.hist_full = hist
            self.hist = [T(hist[:, c, :], "hist%d" % c) for c in range(36)]
            fm = sb("fm", [128, 24, TS], BF16)
            self.fm = [T(fm[:, c, :], "fm%d" % c) for c in range(24)]
            gates = sb("gates", [128, NT, 2048], BF16)
            self.gates = [T(gates[:, t, :], "gates%d" % t) for t in range(NT)]
            sm = sb("sm", [128, NT, 4, 32], F32)
            self.sm = [T(sm[:, t, :, :], "sm%d" % t) for t in range(NT)]
            sv = sb("sv", [128, 8, 32], F32)
            self.sv = T(sv[:], "sv")
            cst2 = sb("cstage", [128, 1, TS + 3], F32)
            self.cstage = [T(cst2[:, i, :], "cstage%d" % i) for i in range(1)]
            cacc = sb("cacc", [128, 2, TS], F32)
            self.cacc_full = cacc
            self.cacc = [T(cacc[:, i, :], "cacc%d" % i) for i in range(2)]
            self.gact = self.cacc
            xs_tm = sb("xs_tm", [128, 2048], BF16)
            self.xs_tm = T(xs_tm[:], "xs_tm")
            xdt = sb("xdt", [128, 2048], BF16)
            self.xdt = T(xdt[:], "xdt")
            xw = sb("xw", [128, 2048], BF16)
            self.xw = T(xw[:], "xw")
            xD = sb("xD", [128, 2048], BF16)
            self.xD = T(xD[:], "xD")
            b_tm = sb("b_tm", [128, 512], BF16)
            self.b_tm = T(b_tm[:], "b_tm")
            yb = sb("ybuf", [128, 2048], F32)
            self.ybuf = T(yb[:], "ybuf")
            ysq = sb("ysq", [128, 2048], F32)
            self.ysq_full = ysq
            self.ysq_h = [T(ysq[:, 0:1024], "ysq_a"), T(ysq[:, 1024:2048], "ysq_b")]
            self.wtmp = [(self.ybuf, yb[:, 0:1024]), (self.ybuf, yb[:, 1024:2048]),
                         (self.ysq_h[0], ysq[:, 0:1024]), (self.ysq_h[1], ysq[:, 1024:2048])]
            ybf = sb("ybf", [128, 2048], BF16)
            self.ybf = T(ybf[:], "ybf")
            self.xbf = [(self.ybf, ybf[:, 0:1024]), (self.ybf, ybf[:, 1024:2048])]
            ytmp = sb("ytmp", [128, 512], F32)
            self.ytmp = T(ytmp[:], "ytmp")
            sst = sb("sstate", [128, 4, 512], F32)
            self.sst = [T(sst[:, g, :], "sst%d" % g) for g in range(4)]
            sstb = sb("sstate_bf", [128, 4, 512], BF16)
            self.sstb = [T(sstb[:, g, :], "sstb%d" % g) for g in range(4)]
            m5 = sb("m_AT8", [128, 8, 128], BF16)
            self.m_AT8 = T(m5[:], "m_AT8")
            kqsb = sb("kq_sb", [128, 2, 128], F32)
            self.kq_sb = [T(kqsb[:, i, :], "kq_sb%d" % i) for i in range(2)]
            ss = sb("ss", [128, 4, 4], F32)
            self.ss = T(ss[:], "ss")
            self.raw = dict(xs_tm=xs_tm, xdt=xdt, xw=xw, xD=xD, ybuf=yb, ysq=ysq)
            qk_tm = sb("qk_tm", [128, 1024], BF16)
            self.qk_tm = T(qk_tm[:], "qk_tm")
            v_tm = sb("v_tm", [128, 512], BF16)
            self.v_tm = T(v_tm[:], "v_tm")
            bvk = sb("bvk", [128, 2, 512], F32)
            self.bv = T(bvk[:, 0, :], "bv")
            self.bk = T(bvk[:, 1, :], "bk")
            u0 = sb("u0sb", [128, 512], F32)
            self.u0 = T(u0[:], "u0sb")
            ukw = sb("ukw", [128, 2, 512], BF16)
            self.u = T(ukw[:, 0, :], "u")
            self.kw = T(ukw[:, 1, :], "kw")
            wtb = sb("wtb", [128, 8, 128], BF16)
            self.wtb = T(wtb[:], "wtb")
            hv = sb("hv", [128, 16, 8], F32)
            self.hv = T(hv[:], "hv")
            hs = sb("hs", [128, 4, 16], F32)
            self.hs = T(hs[:], "hs")
            hmul = sb("hmul", [128, 24], F32)
            self.hmul = T(hmul[:], "hmul")
            vext = sb("vext", [128, 4, 129], BF16)
            self.vext = T(vext[:], "vext")
            atm = sb("atm", [128, 4, 128], BF16)
            self.atm = T(atm[:], "atm")
            hraw = sb("hraw", [128, 4, 129], F32)
            self.hraw = T(hraw[:], "hraw")
            self.raw["hraw"] = hraw
            kwm = sb("kwm", [128, 256], BF16)
            self.kwm = T(kwm[:], "kwm")
            sg = sb("sg", [128, 4, 64], F32)
            self.sg = T(sg[:], "sg")
            sgb = sb("sgb", [128, 4, 64], BF16)
            self.sgb = T(sgb[:], "sgb")
            cn = sb("cn", [128, 2, 129], F32)
            self.cn = T(cn[:], "cn")
            cnb = sb("cnb", [128, 2, 129], BF16)
            self.cnb = T(cnb[:], "cnb")
            st = sb("st", [128, NT, 2, 6], F32)
            self.st = [T(st[:, t, :, :], "st%d" % t) for t in range(NT)]
            mv = sb("mv", [128, NT, 2], F32)
            self.mv = T(mv[:], "mv")
            rs = sb("rs", [128, 3, NT], F32)
            self.rs = T(rs[:], "rs")
            identb = sb("identb", [128, 128], BF16)
            self.identb = T(identb[:], "identb")
            self.ps = []
            for b in range(8):
                p = es.enter_context(nc.psum_tensor("ps%d" % b, [128, 512], F32))
                self.ps.append(T(p[:], "ps%d" % b))

            sems = {e: es.enter_context(nc.semaphore("s_" + e)) for e in ENGS}
            dsems = [es.enter_context(nc.semaphore("q%d" % j)) for j in range(NDS)]
            plan = Plan()
            kb = KB(nc, plan, sems, dsems, self.tiles)
            self.kb = kb
            kb.begin(None, None)
            self.body()
            plan.finalize()
            block = es.enter_context(nc.Block())

            def run(name):
                def f(e):
                    kb.begin(name, e)
                    self.body()
                return f

            block.sync(run("sp"))
            block.scalar(run("act"))
            block.vector(run("dve"))
            block.tensor(run("pe"))
            block.gpsimd(run("pool"))
        return nc

    def psum(self, n=8):
        kb = self.kb
        t = self.ps[kb.psum_rr % n]
        kb.psum_rr += 1
        return t

    def rot(self, lst, key):
        kb = self.kb
        i = kb.misc.get(key, 0)
        kb.misc[key] = i + 1
        return lst[i % len(lst)]

    def wload(self, blk):
        kb = self.kb
        slot = self.wslot[kb.wr_rr % NSLOT]
        kb.wr_rr += 1
        tok = self.wbf_toks.pop(blk, None)
        kb.dma("sp", slot.ap, self.wbf[blk], reads=(), writes=(slot,), deps=(tok,) if tok is not None else ())
        return slot

    def prologue(self):
        kb = self.kb
        kb.dma("pool", self.identb.ap, self.cst_d[:, 0, :], writes=(self.identb,))
        kb.dma("pool", self.cf.ap, self.cst_d, writes=(self.cf,))
        kb.dma("pool", self.bp.ap, self.bpar_d.partition_broadcast(128), writes=(self.bp,))
        kb.dma("pool", self.cw.ap, self.convw_d, writes=(self.cw,))
        kb.dma("pool", self.colp.ap, self.colp_d, writes=(self.colp,))
        self.load_x(0)
        self.wbf_toks = {}
        for b in range(self.nblk):
            self.wbf_toks[b] = kb.dma("pool", self.wbf[b], self.wsrc[b])
        self.setup_state()
        self.setup_hyb()

    def load_x(self, sc):
        kb = self.kb
        for t in range(NT):
            r0 = sc * TS + t * 128
            kb.dma("pool", self.xtm[t].ap, self.x[r0:r0 + 128, :], writes=(self.xtm[t],))

    def store_y(self, sc):
        kb = self.kb
        for t in range(NT):
            r0 = sc * TS + t * 128
            kb.dma("pool", self.y[r0:r0 + 128, :], self.xtm[t].ap, reads=(self.xtm[t],))

    def to_xT_cast(self, t):
        kb = self.kb
        xb, xb_ap = self.xbf[t % 2]
        src = self.xtm[t]
        kb.op("act", lambda e: e.activation(out=xb_ap, in_=src.ap, func=AF.Copy),
              reads=(src,), writes=(xb,))

    def to_xT_T(self, t):
        kb = self.kb
        xb, xb_ap = self.xbf[t % 2]
        p = self.psum()
        pb = p.ap.bitcast(BF16)
        for kc in range(8):
            kb.op("pe", lambda e, kc=kc: e.transpose(pb[:, kc * 128:(kc + 1) * 128],
                                                     xb_ap[:, kc * 128:(kc + 1) * 128],
                                                     self.identb.ap),
                  reads=(xb, self.identb), writes=(p,))
        dst = self.xT[t]
        kb.op("dve", lambda e: e.tensor_copy(out=dst.ap, in_=pb.rearrange("p (k c) -> p k c", k=8)),
              reads=(p,), writes=(dst,))

    def to_xT(self, t):
        self.to_xT_cast(t)
        self.to_xT_T(t)

    def down_ln(self, blk0, nkc, c, ln_idx):
        kb = self.kb
        lp = self.lnp[0]
        kb.dma("pool", lp.ap, self.lnp_d[ln_idx].partition_broadcast(128), writes=(lp,))
        for tp in range(NT // 2):
            banks = [self.psum(), self.psum(), self.psum(), self.psum()]
            for b in range(nkc // 2):
                wsl = self.wload(blk0 + b)
                wv = wsl.ap.rearrange("p (r c) -> p r c", r=2)
                for r in range(2):
                    kc = b * 2 + r
                    for ti in range(2):
                        t = tp * 2 + ti
                        for dh in range(2):
                            pt = banks[ti * 2 + dh]
                            kb.op("pe", lambda e, r=r, kc=kc, t=t, dh=dh, pt=pt, wv=wv: e.matmul(
                                pt.ap, lhsT=self.hT[kc].ap[:, t * 128:(t + 1) * 128],
                                rhs=wv[:, r, dh * 512:(dh + 1) * 512],
                                start=(kc == 0), stop=(kc == nkc - 1)),
                                reads=(wsl, self.hT[kc]), writes=(pt,))
            if tp > 0:
                self.to_xT_T(tp * 2 - 2)
                self.to_xT_T(tp * 2 - 1)
            for ti in range(2):
                self.resid_ln_tile(tp * 2 + ti, banks[ti * 2:ti * 2 + 2], c, lp)
        self.to_xT_T(NT - 2)
        self.to_xT_T(NT - 1)

    def resid_ln_tile(self, t, banks, c, lp):
        kb = self.kb
        eps = LN_EPS / (DN_ALPHA * DN_ALPHA)
        wt, wap = self.wtmp[t]
        x = self.xtm[t]
        rs = self.rs
        for dh in range(2):
            pt = banks[dh]
            sl = slice(dh * 512, (dh + 1) * 512)
            kb.op("dve", lambda e, pt=pt, sl=sl: e.scalar_tensor_tensor(
                out=wap[:, sl], in0=pt.ap, scalar=float(c), in1=x.ap[:, sl],
                op0=ALU.mult, op1=ALU.add), reads=(pt, x), writes=(wt,))
        stt = self.st[t]
        for dh in range(2):
            kb.op("dve", lambda e, dh=dh: e.bn_stats(out=stt.ap[:, dh, :], in_=wap[:, dh * 512:(dh + 1) * 512]),
                  reads=(wt,), writes=(stt,))
        kb.op("dve", lambda e: e.bn_aggr(out=self.mv.ap[:, t, :], in_=stt.ap), reads=(stt,), writes=(self.mv,))
        kb.op("act", lambda e: e.activation(out=rs.ap[:, 0, t:t + 1], in_=self.mv.ap[:, t, 1:2],
                                            func=AF.Sqrt, bias=float(eps), scale=1.0),
              reads=(self.mv,), writes=(rs,))
        kb.op("dve", lambda e: e.reciprocal(out=rs.ap[:, 1, t:t + 1], in_=rs.ap[:, 0, t:t + 1]),
              reads=(rs,), writes=(rs,))
        kb.op("dve", lambda e: e.scalar_tensor_tensor(out=rs.ap[:, 2, t:t + 1], in0=self.mv.ap[:, t, 0:1],
                                                      scalar=-1.0, in1=rs.ap[:, 1, t:t + 1],
                                                      op0=ALU.mult, op1=ALU.mult),
              reads=(self.mv, rs), writes=(rs,))
        kb.op("act", lambda e: e.activation(out=wap, in_=wap, func=AF.Identity, bias=rs.ap[:, 2, t:t + 1],
                                            scale=rs.ap[:, 1, t:t + 1]), reads=(wt, rs), writes=(wt,))
        kb.op("dve", lambda e: e.tensor_tensor(out=wap, in0=wap, in1=lp.ap[:, 0, :], op=ALU.mult),
              reads=(wt, lp), writes=(wt,))
        kb.op("dve", lambda e: e.tensor_tensor(out=x.ap, in0=wap, in1=lp.ap[:, 1, :], op=ALU.add),
              reads=(wt, lp), writes=(x,))
        self.to_xT_cast(t)

    def ffn_ln(self, l, which, sc, ln_idx):
        kb = self.kb
        gu0, ngu = self.windex["%s_gu%d" % (which, l)]
        d0, nd = self.windex["%s_d%d" % (which, l)]
        for j in range(NFC):
            wsl = self.wload(gu0 + j)
            wv = wsl.ap.rearrange("p (g k f) -> p g k f", g=2, k=8)
            pg = self.psum()
            pu = self.psum()
            for g, pt in ((0, pg), (1, pu)):
                for kc in range(8):
                    kb.op("pe", lambda e, g=g, kc=kc, pt=pt: e.matmul(
                        pt.ap, lhsT=wv[:, g, kc, :], rhs=self.xT_full[:, kc, :],
                        start=(kc == 0), stop=(kc == 7)),
                        reads=(wsl,) + tuple(self.xT), writes=(pt,))
            ga = self.gact[j % 2]
            kb.op("act", lambda e: e.activation(out=ga.ap, in_=pg.ap, func=AF.Silu),
                  reads=(pg,), writes=(ga,))
            h = self.hT[j]
            kb.op("dve", lambda e: e.tensor_tensor(out=h.ap, in0=ga.ap, in1=pu.ap, op=ALU.mult),
                  reads=(ga, pu), writes=(h,))
        self.down_ln(d0, NFC, 0.5 / DN_ALPHA, ln_idx)

    def resid_ln(self, banks, c, lp):
        kb = self.kb
        eps = LN_EPS / (DN_ALPHA * DN_ALPHA)
        for t in range(NT):
            wt, wap = self.wtmp[t]
            x = self.xtm[t]
            for dh in range(2):
                pt = banks[t * 2 + dh]
                sl = slice(dh * 512, (dh + 1) * 512)
                kb.op("dve", lambda e, pt=pt, sl=sl: e.scalar_tensor_tensor(
                    out=wap[:, sl], in0=pt.ap, scalar=float(c), in1=x.ap[:, sl],
                    op0=ALU.mult, op1=ALU.add), reads=(pt, x), writes=(wt,))
            stt = self.st[t]
            for dh in range(2):
                kb.op("dve", lambda e, dh=dh: e.bn_stats(out=stt.ap[:, dh, :],
                                                         in_=wap[:, dh * 512:(dh + 1) * 512]),
                      reads=(wt,), writes=(stt,))
            kb.op("dve", lambda e, t=t: e.bn_aggr(out=self.mv.ap[:, t, :], in_=stt.ap),
                  reads=(stt,), writes=(self.mv,))
        rs = self.rs
        kb.op("act", lambda e: e.activation(out=rs.ap[:, 0, :], in_=self.mv.ap[:, :, 1],
                                            func=AF.Sqrt, bias=float(eps), scale=1.0),
              reads=(self.mv,), writes=(rs,))
        kb.op("dve", lambda e: e.reciprocal(out=rs.ap[:, 1, :], in_=rs.ap[:, 0, :]),
              reads=(rs,), writes=(rs,))
        kb.op("dve", lambda e: e.scalar_tensor_tensor(out=rs.ap[:, 2, :], in0=self.mv.ap[:, :, 0],
                                                      scalar=-1.0, in1=rs.ap[:, 1, :],
                                                      op0=ALU.mult, op1=ALU.mult),
              reads=(self.mv, rs), writes=(rs,))
        for t in range(NT):
            wt, wap = self.wtmp[t]
            x = self.xtm[t]
            kb.op("act", lambda e, t=t, wap=wap: e.activation(out=wap, in_=wap, func=AF.Identity,
                                                              bias=rs.ap[:, 2, t:t + 1],
                                                              scale=rs.ap[:, 1, t:t + 1]),
                  reads=(wt, rs), writes=(wt,))
            kb.op("pool", lambda e, wap=wap: e.tensor_tensor(out=wap, in0=wap, in1=lp.ap[:, 0, :], op=ALU.mult),
                  reads=(wt, lp), writes=(wt,))
            kb.op("dve", lambda e, wap=wap, x=x: e.tensor_tensor(out=x.ap, in0=wap, in1=lp.ap[:, 1, :], op=ALU.add),
                  reads=(wt, lp), writes=(x,))
            self.to_xT(t)


    def setup_state(self):
        kb = self.kb
        for g in range(4):
            kb.op("pool", lambda e, g=g: e.memset(self.sst[g].ap, 0.0), writes=(self.sst[g],))
            kb.op("pool", lambda e, g=g: e.memset(self.sstb[g].ap, 0.0), writes=(self.sstb[g],))
        kb.op("pool", lambda e: e.memset(self.hist_full[:], 0.0), writes=tuple(self.hist))
        kb.op("act", lambda e: e.activation(out=self.aneg.ap, in_=self.bp.ap[:, BP_SSD_ALOG:BP_SSD_ALOG + 32],
                                            func=AF.Exp), reads=(self.bp,), writes=(self.aneg,))
        kb.op("dve", lambda e: e.tensor_scalar(out=self.aneg.ap, in0=self.aneg.ap, scalar1=-1.0, scalar2=None,
                                               op0=ALU.mult), reads=(self.aneg,), writes=(self.aneg,))

    def conv_silu(self, pt, ci, dst):
        kb = self.kb
        st = self.rot(self.cstage, "cstage")
        acc = self.rot(self.cacc, "cacc")
        hs = self.hist[ci]
        cw = self.cw
        kb.op("act", lambda e: e.activation(out=st.ap[:, 3:TS + 3], in_=pt.ap, func=AF.Copy),
              reads=(pt,), writes=(st,))
        kb.op("pool", lambda e: e.tensor_copy(out=st.ap[:, 0:3], in_=hs.ap), reads=(hs,), writes=(st,))
        kb.op("pool", lambda e: e.tensor_copy(out=hs.ap, in_=st.ap[:, TS:TS + 3]), reads=(st,), writes=(hs,))
        kb.op("dve", lambda e: e.tensor_scalar(out=acc.ap, in0=st.ap[:, 0:TS], scalar1=cw.ap[:, ci, 0:1],
                                               scalar2=None, op0=ALU.mult), reads=(st, cw), writes=(acc,))
        for k in range(1, 4):
            kb.op("dve", lambda e, k=k: e.scalar_tensor_tensor(
                out=acc.ap, in0=st.ap[:, k:k + TS], scalar=cw.ap[:, ci, k:k + 1], in1=acc.ap,
                op0=ALU.mult, op1=ALU.add), reads=(st, cw, acc), writes=(acc,))
        kb.op("act", lambda e: e.activation(out=dst.ap, in_=acc.ap, func=AF.Silu, bias=cw.ap[:, ci, 4:5],
                                            scale=1.0), reads=(acc, cw), writes=(dst,))

    def fm_proj(self, wsl, half, pt):
        kb = self.kb
        wv = wsl.ap.rearrange("p (k c) -> p k c", k=8)
        for kc in range(8):
            kb.op("pe", lambda e, kc=kc: e.matmul(pt.ap, lhsT=wv[:, kc, half * 128:(half + 1) * 128],
                                                  rhs=self.xT_full[:, kc, :], start=(kc == 0), stop=(kc == 7)),
                  reads=(wsl,) + tuple(self.xT), writes=(pt,))

    def tm_proj(self, wsl, t, pt, ncols=256):
        kb = self.kb
        wv = wsl.ap.rearrange("p (k c) -> p k c", k=8)
        for kc in range(8):
            kb.op("pe", lambda e, kc=kc: e.matmul(pt.ap[:, 0:ncols],
                                                  lhsT=self.xT_full[:, kc, t * 128:(t + 1) * 128],
                                                  rhs=wv[:, kc, 0:ncols], start=(kc == 0), stop=(kc == 7)),
                  reads=(wsl, self.xT[t]), writes=(pt,))

    def out_proj_ln(self, o0, nkc, ln_idx):
        self.down_ln(o0, nkc, 1.0 / DN_ALPHA, ln_idx)

    def ssd_mixer(self, sc, ln_idx):
        kb = self.kb
        w0, _ = self.windex["ssd_in"]
        o0, _ = self.windex["ssd_out"]
        wsl = None
        for cc in range(24):
            if cc % 2 == 0:
                wsl = self.wload(w0 + cc // 2)
            pt = self.psum(5)
            self.fm_proj(wsl, cc % 2, pt)
            self.conv_silu(pt, 12 + cc, self.fm[cc])
        for b in range(8):
            wsl = self.wload(w0 + 12 + b)
            for t in range(NT):
                pt = self.psum(5)
                self.tm_proj(wsl, t, pt)
                gt = self.gates[t]
                kb.op("act", lambda e, b=b, pt=pt, gt=gt: e.activation(
                    out=gt.ap[:, b * 256:(b + 1) * 256], in_=pt.ap[:, 0:256], func=AF.Silu),
                    reads=(pt,), writes=(gt,))
        wsl = self.wload(w0 + 20)
        for t in range(NT):
            pt = self.psum(5)
            self.tm_proj(wsl, t, pt, 32)
            sm = self.sm[t]
            kb.op("dve", lambda e, pt=pt, sm=sm: e.tensor_tensor(
                out=sm.ap[:, 0, :], in0=pt.ap[:, 0:32], in1=self.bp.ap[:, BP_SSD_DTB:BP_SSD_DTB + 32],
                op=ALU.add), reads=(pt, self.bp), writes=(sm,))
            kb.op("act", lambda e, sm=sm: e.activation(out=sm.ap[:, 1, :], in_=sm.ap[:, 0, :], func=AF.Exp),
                  reads=(sm,), writes=(sm,))
            kb.op("act", lambda e, sm=sm: e.activation(out=sm.ap[:, 2, :], in_=sm.ap[:, 1, :], func=AF.Ln,
                                                       bias=1.0, scale=1.0), reads=(sm,), writes=(sm,))
            kb.op("dve", lambda e, sm=sm: e.tensor_tensor(out=sm.ap[:, 3, :], in0=sm.ap[:, 2, :],
                                                          in1=self.aneg.ap, op=ALU.mult),
                  reads=(sm, self.aneg), writes=(sm,))
        for t in range(NT):
            self.ssd_chunk(t)
        self.out_proj_ln(o0, 16, ln_idx)

    def transposes_to(self, srcs, dst_tile, dst_ap, evac="dve", scale_ap=None):
        kb = self.kb
        p = self.psum(5)
        pb = p.ap.bitcast(BF16)
        n = len(srcs)
        for q, (tl, ap) in enumerate(srcs):
            kb.op("pe", lambda e, q=q, ap=ap: e.transpose(pb[:, q * 128:(q + 1) * 128], ap, self.identb.ap),
                  reads=(tl, self.identb), writes=(p,))
        if scale_ap is None:
            if evac == "act":
                kb.op("act", lambda e: e.activation(out=dst_ap, in_=pb[:, 0:n * 128], func=AF.Copy),
                      reads=(p,), writes=dst_tile)
            else:
                src = pb[:, 0:n * 128]
                if len(dst_ap.shape) == 3:
                    src = src.rearrange("p (k c) -> p k c", k=n)
                kb.op("dve", lambda e: e.tensor_copy(out=dst_ap, in_=src),
                      reads=(p,), writes=dst_tile)
        else:
            kb.op("dve", lambda e: e.tensor_tensor(out=dst_ap, in0=pb[:, 0:n * 128].rearrange("p (k c) -> p k c", k=n),
                                                   in1=scale_ap, op=ALU.mult),
                  reads=(p, self.colp), writes=dst_tile)

    def decay_block(self, row_ap, col_ap, mask_idx, n, dg, dg_t, tt, tt_t, col_t, nb=5):
        kb = self.kb
        cf = self.cf
        tt_w = tt_t if isinstance(tt_t, tuple) else (tt_t,)
        identf = cf.ap[:, C_ID, :]
        onesf = cf.ap[:, C_ONES, :]
        maskf = cf.ap[:, mask_idx if mask_idx is not None else 0, :]
        kb.op("pool", lambda e: e.tensor_tensor(out=dg[:, 0:n, :],
                                                in0=identf.unsqueeze(1).to_broadcast([128, n, 128]),
                                                in1=row_ap.unsqueeze(2).to_broadcast([128, n, 128]), op=ALU.mult),
              reads=(cf, col_t), writes=(dg_t,))
        for half in range((n + 3) // 4):
            pb = self.psum(nb)
            kb.op("pe", lambda e, half=half, pb=pb: e.matmul(
                pb.ap, lhsT=onesf, rhs=dg[:, half * 4:(half + 1) * 4, :], start=True, stop=True),
                reads=(cf, dg_t), writes=(pb,))
            kb.op("dve", lambda e, half=half, pb=pb: e.tensor_tensor(
                out=tt[:, half * 4:(half + 1) * 4, :], in0=pb.ap.rearrange("p (h c) -> p h c", h=4),
                in1=col_ap[:, half * 4:(half + 1) * 4].unsqueeze(2).to_broadcast([128, 4, 128]), op=ALU.subtract),
                reads=(pb, col_t), writes=tt_w)
        if mask_idx is None:
            kb.op("act", lambda e: e.activation(out=tt[:, 0:n, :], in_=tt[:, 0:n, :], func=AF.Relu, scale=-1.0),
                  reads=tt_w, writes=tt_w)
            kb.op("act", lambda e: e.activation(out=tt[:, 0:n, :], in_=tt[:, 0:n, :], func=AF.Exp, scale=-1.0),
                  reads=tt_w, writes=tt_w)
            return
        kb.op("pool", lambda e: e.tensor_tensor(out=tt[:, 0:n, :], in0=tt[:, 0:n, :],
                                                in1=maskf.unsqueeze(1).to_broadcast([128, n, 128]), op=ALU.add),
              reads=tt_w + (cf,), writes=tt_w)
        kb.op("act", lambda e: e.activation(out=tt[:, 0:n, :], in_=tt[:, 0:n, :], func=AF.Exp),
              reads=tt_w, writes=tt_w)

    def ssd_chunk(self, t):
        kb = self.kb
        tc = slice(t * 128, (t + 1) * 128)
        cf, sv, sm = self.cf, self.sv, self.sm[t]
        for half in range(2):
            srcs = [(self.fm[half * 8 + q], self.fm[half * 8 + q].ap[:, tc]) for q in range(8)]
            self.transposes_to(srcs, (self.xs_tm,), self.xs_tm.ap[:, half * 1024:(half + 1) * 1024],
                               evac="act" if half else "dve")
        srcs = [(self.fm[16 + g], self.fm[16 + g].ap[:, tc]) for g in range(4)]
        self.transposes_to(srcs, (self.b_tm,), self.b_tm.ap, evac="dve")
        p = self.psum(5)
        kb.op("pe", lambda e: e.matmul(p.ap[:, 0:32], lhsT=cf.ap[:, C_TRI, :], rhs=sm.ap[:, 3, :],
                                       start=True, stop=True), reads=(cf, sm), writes=(p,))
        kb.op("pe", lambda e: e.matmul(p.ap[:, 32:64], lhsT=cf.ap[:, C_ONES, :], rhs=sm.ap[:, 3, :],
                                       start=True, stop=True), reads=(cf, sm), writes=(p,))
        kb.op("dve", lambda e: e.tensor_copy(out=sv.ap[:, 0, :], in_=p.ap[:, 0:32]), reads=(p,), writes=(sv,))
        kb.op("act", lambda e: e.activation(out=sv.ap[:, 1, :], in_=p.ap[:, 0:32], func=AF.Exp),
              reads=(p,), writes=(sv,))
        kb.op("dve", lambda e: e.tensor_tensor(out=sv.ap[:, 2, :], in0=p.ap[:, 32:64], in1=sv.ap[:, 0, :],
                                               op=ALU.subtract), reads=(p, sv), writes=(sv,))
        kb.op("act", lambda e: e.activation(out=sv.ap[:, 3, :], in_=sv.ap[:, 2, :], func=AF.Exp),
              reads=(sv,), writes=(sv,))
        kb.op("dve", lambda e: e.tensor_tensor(out=sv.ap[:, 4, :], in0=sv.ap[:, 3, :], in1=sm.ap[:, 2, :],
                                               op=ALU.mult), reads=(sv, sm), writes=(sv,))
        kb.op("act", lambda e: e.activation(out=sv.ap[:, 5, :], in_=p.ap[:, 32:64], func=AF.Exp),
              reads=(p,), writes=(sv,))
        xs3 = self.xs_tm.ap.rearrange("p (h c) -> p h c", h=32)

        def bc32(ap2d):
            return ap2d.unsqueeze(2).to_broadcast([128, 32, 64])

        kb.op("dve", lambda e: e.tensor_tensor(out=self.xdt.ap.rearrange("p (h c) -> p h c", h=32), in0=xs3,
                                               in1=bc32(sm.ap[:, 2, :]), op=ALU.mult),
              reads=(self.xs_tm, sm), writes=(self.xdt,))
        kb.op("pool", lambda e: e.tensor_tensor(out=self.xw.ap.rearrange("p (h c) -> p h c", h=32), in0=xs3,
                                                in1=bc32(sv.ap[:, 4, :]), op=ALU.mult),
              reads=(self.xs_tm, sv), writes=(self.xw,))
        kb.op("dve", lambda e: e.tensor_tensor(out=self.xD.ap.rearrange("p (h c) -> p h c", h=32), in0=xs3,
                                               in1=bc32(self.bp.ap[:, BP_SSD_D:BP_SSD_D + 32]), op=ALU.mult),
              reads=(self.xs_tm, self.bp), writes=(self.xD,))
        YD, QS, SU = self.ps[5], self.ps[6], self.ps[7]
        raw = self.raw
        dg8 = raw["ysq"][:, 0:1024].rearrange("p (h c) -> p h c", h=8)
        TT = [(self.ysq_h[1], raw["ysq"][:, 1024:2048].rearrange("p (h c) -> p h c", h=8)),
              (tuple(self.cacc), self.cacc_full[:].rearrange("p a c -> p (a c)").rearrange("p (h c) -> p h c", h=8))]
        hraw_bf = self.raw["hraw"][:].rearrange("p a c -> p (a c)")[:, 0:512].bitcast(BF16).rearrange("p (h c) -> p h c", h=8)
        ATB = [(self.m_AT8, self.m_AT8.ap), (self.hraw, hraw_bf)]
        for g in range(4):
            bt, ct = self.fm[16 + g], self.fm[20 + g]
            tt_tile, tt8 = TT[g % 2]
            tt_tiles = tt_tile if isinstance(tt_tile, tuple) else (tt_tile,)
            AT8t, AT8 = ATB[g % 2]
            pkq = self.psum(5)
            kb.op("pe", lambda e, bt=bt, ct=ct, pkq=pkq: e.matmul(pkq.ap[:, 0:128], lhsT=bt.ap[:, tc], rhs=ct.ap[:, tc],
                                                                 start=True, stop=True),
                  reads=(bt, ct), writes=(pkq,))
            kqs = self.kq_sb[g % 2]
            kb.op("dve", lambda e, pkq=pkq, kqs=kqs: e.tensor_tensor(out=kqs.ap, in0=pkq.ap[:, 0:128],
                                                                    in1=cf.ap[:, C_TRI, :], op=ALU.mult),
                  reads=(pkq, cf), writes=(kqs,))
            gcg = sv.ap[:, 0, g * 8:(g + 1) * 8]
            self.decay_block(gcg, gcg, None, 8, dg8, self.ysq_h[0], tt8, tt_tile, sv)
            kb.op("pool", lambda e, kqs=kqs, tt8=tt8, AT8=AT8: e.tensor_tensor(
                out=AT8, in0=tt8, in1=kqs.ap.unsqueeze(1).to_broadcast([128, 8, 128]), op=ALU.mult),
                reads=tt_tiles + (kqs,), writes=(AT8t,))
            for r in range(8):
                h = g * 8 + r
                kb.op("pe", lambda e, r=r, h=h, AT8=AT8: e.matmul(YD.ap[:, r * 64:(r + 1) * 64], lhsT=AT8[:, r, :],
                                                         rhs=self.xdt.ap[:, h * 64:(h + 1) * 64],
                                                         start=True, stop=False),
                      reads=(AT8t, self.xdt), writes=(YD,))
                kb.op("pe", lambda e, r=r, h=h: e.matmul(YD.ap[:, r * 64:(r + 1) * 64], lhsT=self.identb.ap,
                                                         rhs=self.xD.ap[:, h * 64:(h + 1) * 64],
                                                         start=False, stop=True),
                      reads=(self.identb, self.xD), writes=(YD,))
            sb_, s_ = self.sstb[g], self.sst[g]
            kb.op("pe", lambda e, ct=ct, sb_=sb_: e.matmul(QS.ap, lhsT=ct.ap[:, tc], rhs=sb_.ap, start=True, stop=True),
                  reads=(ct, sb_), writes=(QS,))
            kb.op("pe", lambda e, g=g: e.matmul(SU.ap, lhsT=self.b_tm.ap[:, g * 128:(g + 1) * 128],
                                                rhs=self.xw.ap[:, g * 512:(g + 1) * 512], start=True, stop=True),
                  reads=(self.b_tm, self.xw), writes=(SU,))

            def bc8(ap2d):
                return ap2d.unsqueeze(2).to_broadcast([128, 8, 64])

            yt = self.ytmp
            kb.op("dve", lambda e, g=g: e.tensor_tensor(out=yt.ap.rearrange("p (h c) -> p h c", h=8),
                                                        in0=QS.ap.rearrange("p (h c) -> p h c", h=8),
                                                        in1=bc8(sv.ap[:, 1, g * 8:(g + 1) * 8]), op=ALU.mult),
                  reads=(QS, sv), writes=(yt,))
            kb.op("dve", lambda e, g=g: e.tensor_tensor(out=self.ybuf.ap[:, g * 512:(g + 1) * 512], in0=yt.ap,
                                                        in1=YD.ap, op=ALU.add),
                  reads=(yt, YD), writes=(self.ybuf,))
            kb.op("dve", lambda e, g=g, s_=s_: e.tensor_tensor(out=s_.ap.rearrange("p (h c) -> p h c", h=8),
                                                               in0=s_.ap.rearrange("p (h c) -> p h c", h=8),
                                                               in1=bc8(sv.ap[:, 5, g * 8:(g + 1) * 8]), op=ALU.mult),
                  reads=(s_, sv), writes=(s_,))
            kb.op("dve", lambda e, s_=s_: e.tensor_tensor(out=s_.ap, in0=s_.ap, in1=SU.ap, op=ALU.add),
                  reads=(s_, SU), writes=(s_,))
            kb.op("act", lambda e, s_=s_, sb_=sb_: e.activation(out=sb_.ap, in_=s_.ap, func=AF.Copy),
                  reads=(s_,), writes=(sb_,))
        yb, gt, ss = self.ybuf, self.gates[t], self.ss
        kb.op("dve", lambda e: e.tensor_tensor(out=yb.ap, in0=yb.ap, in1=gt.ap, op=ALU.mult),
              reads=(yb, gt), writes=(yb,))
        kb.op("act", lambda e: e.activation(out=self.ysq_full[:], in_=yb.ap, func=AF.Square),
              reads=(yb,), writes=tuple(self.ysq_h))
        kb.op("dve", lambda e: e.reduce_sum(out=ss.ap[:, 0, :], in_=self.ysq_full[:].rearrange("p (g c) -> p g c", g=4),
                                            axis=AX.X), reads=tuple(self.ysq_h), writes=(ss,))
        kb.op("act", lambda e: e.activation(out=ss.ap[:, 1, :], in_=ss.ap[:, 0, :], func=AF.Sqrt,
                                            bias=float(RMS_EPS), scale=1.0 / 512.0), reads=(ss,), writes=(ss,))
        kb.op("dve", lambda e: e.reciprocal(out=ss.ap[:, 2, :], in_=ss.ap[:, 1, :]), reads=(ss,), writes=(ss,))
        for g in range(4):
            kb.op("dve", lambda e, g=g: e.tensor_scalar(out=self.ybf.ap[:, g * 512:(g + 1) * 512],
                                                        in0=yb.ap[:, g * 512:(g + 1) * 512],
                                                        scalar1=ss.ap[:, 2, g:g + 1], scalar2=None, op0=ALU.mult),
                  reads=(yb, ss), writes=(self.ybf,))
        for half in range(2):
            srcs = [(self.ybf, self.ybf.ap[:, (half * 8 + q) * 128:(half * 8 + q + 1) * 128]) for q in range(8)]
            dst_tiles = tuple(self.hT[half * 8 + q] for q in range(8))
            dst_ap = self.hT_full[:, half * 8:(half + 1) * 8, tc]
            sc_ap = self.colp.ap[:, half * 8:(half + 1) * 8].unsqueeze(2).to_broadcast([128, 8, 128])
            self.transposes_to(srcs, dst_tiles, dst_ap, scale_ap=sc_ap)

    def setup_hyb(self):
        kb = self.kb
        for tl in (self.sg, self.sgb, self.cn, self.cnb):
            kb.op("pool", lambda e, tl=tl: e.memset(tl.ap, 0.0), writes=(tl,))
        kb.op("pool", lambda e: e.memset(self.vext.ap, 1.0), writes=(self.vext,))
        hm = self.hmul
        kb.op("pool", lambda e: e.memset(hm.ap, -1.0), writes=(hm,))
        kb.op("pool", lambda e: e.memset(hm.ap[:, 16:20], 0.0), writes=(hm,))
        kb.op("act", lambda e: e.activation(out=hm.ap[:, 8:16], in_=self.bp.ap[:, BP_G_ALOG:BP_G_ALOG + 8],
                                            func=AF.Exp), reads=(self.bp, hm), writes=(hm,))
        kb.op("dve", lambda e: e.tensor_scalar(out=hm.ap[:, 8:16], in0=hm.ap[:, 8:16], scalar1=-1.0, scalar2=None,
                                               op0=ALU.mult), reads=(hm,), writes=(hm,))

    def hyb_mixer(self, sc, ln_idx):
        kb = self.kb
        w0, _ = self.windex["hyb_in"]
        o0, _ = self.windex["hyb_out"]
        wsl = None
        for cc in range(16):
            if cc % 2 == 0:
                wsl = self.wload(w0 + cc // 2)
            pt = self.psum()
            self.fm_proj(wsl, cc % 2, pt)
            if cc < 12:
                self.conv_silu(pt, cc, self.fm[cc])
            else:
                dst = self.fm[cc]
                scl = 0.125 if cc >= 14 else 1.0
                kb.op("act", lambda e, pt=pt, dst=dst, scl=scl: e.activation(out=dst.ap, in_=pt.ap, func=AF.Copy,
                                                                             scale=scl),
                      reads=(pt,), writes=(dst,))
        for b in range(7):
            wsl = self.wload(w0 + 8 + b)
            for t in range(NT):
                pt = self.psum()
                self.tm_proj(wsl, t, pt)
                gt = self.gates[t]
                if b < 2:
                    fn, scl = AF.Silu, 1.0
                elif b < 4:
                    fn, scl = AF.Sigmoid, 1.0
                elif b < 6:
                    fn, scl = AF.Copy, 1.0
                else:
                    fn, scl = AF.Copy, 0.125
                kb.op("act", lambda e, b=b, pt=pt, gt=gt, fn=fn, scl=scl: e.activation(
                    out=gt.ap[:, b * 256:(b + 1) * 256], in_=pt.ap[:, 0:256], func=fn, scale=scl),
                    reads=(pt,), writes=(gt,))
        wsl = self.wload(w0 + 15)
        bp = self.bp
        for t in range(NT):
            pt = self.psum()
            self.tm_proj(wsl, t, pt, 32)
            sm = self.sm[t]
            kb.op("dve", lambda e, pt=pt, sm=sm: e.tensor_tensor(
                out=sm.ap[:, 0, 0:24], in0=pt.ap[:, 0:24], in1=bp.ap[:, BP_H_BIAS:BP_H_BIAS + 24], op=ALU.add),
                reads=(pt, bp), writes=(sm,))
            kb.op("dve", lambda e, sm=sm: e.tensor_tensor(
                out=sm.ap[:, 0, 0:24], in0=sm.ap[:, 0, 0:24], in1=bp.ap[:, BP_H_SGN:BP_H_SGN + 24], op=ALU.mult),
                reads=(sm, bp), writes=(sm,))
            kb.op("act", lambda e, sm=sm: e.activation(out=sm.ap[:, 1, 0:24], in_=sm.ap[:, 0, 0:24], func=AF.Exp),
                  reads=(sm,), writes=(sm,))
            kb.op("act", lambda e, sm=sm: e.activation(out=sm.ap[:, 2, 0:24], in_=sm.ap[:, 1, 0:24], func=AF.Ln,
                                                       bias=1.0, scale=1.0), reads=(sm,), writes=(sm,))
            kb.op("dve", lambda e, sm=sm: e.tensor_tensor(out=sm.ap[:, 3, 0:24], in0=sm.ap[:, 2, 0:24],
                                                          in1=self.hmul.ap, op=ALU.mult),
                  reads=(sm, self.hmul), writes=(sm,))
            kb.op("dve", lambda e, sm=sm: e.tensor_copy(out=sm.ap[:, 3, 16:20], in_=sm.ap[:, 0, 16:20]),
                  reads=(sm,), writes=(sm,))
        for t in range(NT):
            self.hyb_chunk(t)
        self.out_proj_ln(o0, 8, ln_idx)

    def hyb_chunk(self, t):
        kb = self.kb
        tc = slice(t * 128, (t + 1) * 128)
        cf, hv, hs, sm, gt = self.cf, self.hv, self.hs, self.sm[t], self.gates[t]
        raw = self.raw
        A_xs, A_xdt, A_xw, A_xD, A_yb = (self.xs_tm, self.xdt, self.xw, self.xD, self.ybuf)

        def v8(h):
            return h[:].bitcast(F32).rearrange("p (h c) -> p h c", h=8)

        Y = [(A_xs, v8(raw["xs_tm"])), (A_xdt, v8(raw["xdt"]))]
        X = [(A_xw, v8(raw["xw"])), (A_xD, v8(raw["xD"]))]
        P_t, P = A_yb, raw["ybuf"][:, 0:1024].rearrange("p (h c) -> p h c", h=8)
        dg_t, dg = A_yb, raw["ybuf"][:, 1024:2048].rearrange("p (h c) -> p h c", h=8)
        tt_t, tt = self.ysq_h[0], raw["ysq"][:, 0:1024].rearrange("p (h c) -> p h c", h=8)
        at_t, AT = self.ysq_h[1], raw["ysq"][:, 1024:1536].bitcast(BF16).rearrange("p (h c) -> p h c", h=8)
        identf = cf.ap[:, C_ID, :]
        onesf = cf.ap[:, C_ONES, :]

        def bcl(ap2d, n, c):
            return ap2d.unsqueeze(2).to_broadcast([128, n, c])

        def bcm(ap2d, n, c):
            return ap2d.unsqueeze(1).to_broadcast([128, n, c])

        srcs = [(self.fm[q], self.fm[q].ap[:, tc]) for q in range(8)]
        self.transposes_to(srcs, (self.qk_tm,), self.qk_tm.ap, evac="dve")
        srcs = [(self.fm[8 + q], self.fm[8 + q].ap[:, tc]) for q in range(4)]
        self.transposes_to(srcs, (self.v_tm,), self.v_tm.ap, evac="act")
        sqv = raw["ybuf"][:, 1024:2048]
        kb.op("act", lambda e: e.activation(out=sqv, in_=self.qk_tm.ap, func=AF.Square),
              reads=(self.qk_tm,), writes=(dg_t,))
        kb.op("dve", lambda e: e.reduce_sum(out=hs.ap[:, 0, :], in_=sqv.rearrange("p (h c) -> p h c", h=16),
                                            axis=AX.X), reads=(dg_t,), writes=(hs,))
        kb.op("act", lambda e: e.activation(out=hs.ap[:, 1, :], in_=hs.ap[:, 0, :], func=AF.Ln, bias=1e-6,
                                            scale=1.0), reads=(hs,), writes=(hs,))
        p = self.psum()
        kb.op("pe", lambda e: e.matmul(p.ap[:, 0:24], lhsT=cf.ap[:, C_TRI, :], rhs=sm.ap[:, 3, 0:24],
                                       start=True, stop=True), reads=(cf, sm), writes=(p,))
        kb.op("pe", lambda e: e.matmul(p.ap[:, 32:56], lhsT=onesf, rhs=sm.ap[:, 3, 0:24],
                                       start=True, stop=True), reads=(cf, sm), writes=(p,))
        H = lambda i, n=8: hv.ap[:, i, 0:n]

        def dv(fn, reads, writes=(hv,)):
            kb.op("dve", fn, reads=reads, writes=writes)

        def ac(fn, reads, writes=(hv,)):
            kb.op("act", fn, reads=reads, writes=writes)

        dv(lambda e: e.tensor_copy(out=H(0), in_=p.ap[:, 8:16]), (p,))
        dv(lambda e: e.tensor_scalar(out=H(1), in0=hs.ap[:, 1, 8:16], scalar1=-0.5, scalar2=None,
                                     op0=ALU.mult), (hs,))
        dv(lambda e: e.tensor_scalar(out=H(2), in0=hs.ap[:, 1, 0:8], scalar1=-0.5, scalar2=-math.log(8.0),
                                     op0=ALU.mult, op1=ALU.add), (hs,))
        dv(lambda e: e.tensor_tensor(out=H(2), in0=H(2), in1=H(0), op=ALU.add), (hv,))
        dv(lambda e: e.tensor_tensor(out=H(3), in0=H(0), in1=H(1), op=ALU.subtract), (hv,))
        dv(lambda e: e.tensor_tensor(out=H(4), in0=H(0), in1=H(1), op=ALU.add), (hv,))
        dv(lambda e: e.tensor_tensor(out=H(4), in0=H(4), in1=sm.ap[:, 3, 0:8], op=ALU.add), (hv, sm))
        ac(lambda e: e.activation(out=H(5), in_=H(2), func=AF.Exp), (hv,))
        dv(lambda e: e.tensor_tensor(out=H(6), in0=p.ap[:, 40:48], in1=H(3), op=ALU.subtract), (p, hv))
        ac(lambda e: e.activation(out=H(6), in_=H(6), func=AF.Exp), (hv,))
        ac(lambda e: e.activation(out=H(7), in_=H(4), func=AF.Exp), (hv,))
        ac(lambda e: e.activation(out=H(8), in_=sm.ap[:, 3, 0:8], func=AF.Exp), (sm,))
        ac(lambda e: e.activation(out=H(9), in_=p.ap[:, 40:48], func=AF.Exp), (p,))
        dv(lambda e: e.tensor_copy(out=H(10, 4), in_=p.ap[:, 20:24]), (p,))
        dv(lambda e: e.tensor_tensor(out=H(11, 4), in0=H(10, 4), in1=sm.ap[:, 3, 16:20], op=ALU.subtract),
           (hv, sm))
        ac(lambda e: e.activation(out=H(12, 4), in_=H(10, 4), func=AF.Exp), (hv,))
        dv(lambda e: e.tensor_tensor(out=H(13, 4), in0=p.ap[:, 52:56], in1=H(11, 4), op=ALU.subtract), (p, hv))
        ac(lambda e: e.activation(out=H(13, 4), in_=H(13, 4), func=AF.Exp), (hv,))
        ac(lambda e: e.activation(out=H(14, 4), in_=p.ap[:, 52:56], func=AF.Exp), (p,))

        def kT(h):
            return self.fm[4 + h // 2], self.fm[4 + h // 2].ap[(h % 2) * 64:(h % 2) * 64 + 64, tc]

        def qT(h):
            return self.fm[h // 2], self.fm[h // 2].ap[(h % 2) * 64:(h % 2) * 64 + 64, tc]

        def decay(row_ap, col_ap, mask_idx, n):
            self.decay_block(row_ap, col_ap, mask_idx, n, dg, dg_t, tt, tt_t, hv, nb=8)

        pkk = [self.psum(), self.psum()]
        pkq = [self.psum(), self.psum()]
        for h in range(8):
            kt, kap = kT(h)
            qt, qap = qT(h)
            cs = slice((h // 2) * 128, (h // 2) * 128 + 128)
            kb.op("pe", lambda e, h=h, kap=kap, cs=cs: e.matmul(pkk[h % 2].ap[:, cs], lhsT=kap, rhs=kap,
                                                               start=True, stop=True),
                  reads=(kt,), writes=(pkk[h % 2],))
            kb.op("pe", lambda e, h=h, kap=kap, qap=qap, cs=cs: e.matmul(pkq[h % 2].ap[:, cs], lhsT=kap, rhs=qap,
                                                                        start=True, stop=True),
                  reads=(kt, qt), writes=(pkq[h % 2],))
        decay(H(4), H(3), C_MSTR, 8)
        Y0t, Y0 = Y[0]
        for par in range(2):
            kb.op("dve", lambda e, par=par: e.scalar_tensor_tensor(
                out=Y0[:, par:8:2, :], in0=tt[:, par:8:2, :], scalar=-1.0,
                in1=pkk[par].ap.rearrange("p (h c) -> p h c", h=4), op0=ALU.mult, op1=ALU.mult),
                reads=(tt_t, pkk[par]), writes=(Y0t,))
        decay(H(2), H(3), C_MINC, 8)
        for par in range(2):
            kb.op("dve", lambda e, par=par: e.tensor_tensor(
                out=AT[:, par:8:2, :], in0=tt[:, par:8:2, :],
                in1=pkq[par].ap.rearrange("p (h c) -> p h c", h=4), op=ALU.mult),
                reads=(tt_t, pkq[par]), writes=(at_t,))
        X0t, X0 = X[0]
        px = [self.psum(), self.psum()]
        for h in range(8):
            cs = slice((h % 4) * 128, (h % 4) * 128 + 128)
            kb.op("pe", lambda e, h=h, cs=cs: e.transpose(px[h // 4].ap[:, cs], Y0[:, h, :], identf),
                  reads=(Y0t, cf), writes=(px[h // 4],))
        kb.op("act", lambda e: e.activation(out=X0[:, 0:4, :], in_=px[0].ap.rearrange("p (h c) -> p h c", h=4),
                                            func=AF.Copy), reads=(px[0],), writes=(X0t,))
        kb.op("dve", lambda e: e.tensor_copy(out=X0[:, 4:8, :], in_=px[1].ap.rearrange("p (h c) -> p h c", h=4)),
              reads=(px[1],), writes=(X0t,))
        kb.op("pool", lambda e: e.tensor_tensor(out=P, in0=Y0, in1=bcm(identf, 8, 128), op=ALU.add),
              reads=(Y0t, cf), writes=(P_t,))
        dg_m = self.cacc[0].ap.rearrange("p (h c) -> p h c", h=4)
        tt_m = self.cacc[1].ap.rearrange("p (h c) -> p h c", h=4)
        mstate = {}

        def mlstm_part1():
            def mqT(m):
                return self.fm[12 + m // 2], self.fm[12 + m // 2].ap[(m % 2) * 64:(m % 2) * 64 + 64, tc]

            def mkT(m):
                return self.fm[14 + m // 2], self.fm[14 + m // 2].ap[(m % 2) * 64:(m % 2) * 64 + 64, tc]

            vext = self.vext
            kb.op("pool", lambda e: e.tensor_copy(out=vext.ap[:, :, 0:128],
                                                  in_=gt.ap[:, 1024:1536].rearrange("p (h c) -> p h c", h=4)),
                  reads=(gt,), writes=(vext,))
            kb.op("pool", lambda e: e.tensor_tensor(out=self.kwm.ap.rearrange("p (h c) -> p h c", h=4),
                                                    in0=gt.ap[:, 1536:1792].rearrange("p (h c) -> p h c", h=4),
                                                    in1=bcl(H(13, 4), 4, 64), op=ALU.mult),
                  reads=(gt, hv), writes=(self.kwm,))
            pmk = [self.psum(), self.psum()]
            for m in range(4):
                kt, kap = mkT(m)
                qt, qap = mqT(m)
                kb.op("pe", lambda e, m=m, kap=kap, qap=qap: e.matmul(pmk[m % 2].ap[:, (m // 2) * 128:(m // 2) * 128 + 128],
                                                                     lhsT=kap, rhs=qap, start=True, stop=True),
                      reads=(kt, qt), writes=(pmk[m % 2],))
            self.decay_block(H(10, 4), H(11, 4), C_MINC, 4, dg_m, self.cacc[0], tt_m, self.cacc[1], hv, nb=8)
            for par in range(2):
                kb.op("dve", lambda e, par=par: e.tensor_tensor(
                    out=self.atm.ap[:, par:4:2, :], in0=tt_m[:, par:4:2, :],
                    in1=pmk[par].ap[:, 0:256].rearrange("p (h c) -> p h c", h=2), op=ALU.mult),
                    reads=(self.cacc[1], pmk[par]), writes=(self.atm,))
            mstate["vext"] = vext
            mstate["mqT"] = mqT

        def mlstm_part2():
            vext = mstate["vext"]
            mqT = mstate["mqT"]
            pn = [self.psum(), self.psum()]
            pq = [self.psum(), self.psum()]
            for m in range(4):
                r0 = (m % 2) * 64
                cs = slice((m % 2) * 256, (m % 2) * 256 + 129)
                cq = slice((m // 2) * 256, (m // 2) * 256 + 129)
                qt, qap = mqT(m)
                kb.op("pe", lambda e, m=m, cs=cs: e.matmul(pn[m // 2].ap[:, cs], lhsT=self.atm.ap[:, m, :],
                                                           rhs=vext.ap[:, m, :], start=True, stop=True),
                      reads=(self.atm, vext), writes=(pn[m // 2],))
                kb.op("pe", lambda e, m=m, cq=cq, r0=r0, qap=qap: e.matmul(pq[m % 2].ap[:, cq], lhsT=qap,
                                                                           rhs=self.cnb.ap[r0:r0 + 64, m // 2, :],
                                                                           start=True, stop=True),
                      reads=(qt, self.cnb), writes=(pq[m % 2],))
            hr = self.hraw
            for par in range(2):
                kb.op("dve", lambda e, par=par: e.tensor_tensor(
                    out=hr.ap[:, par:4:2, :], in0=pq[par].ap.rearrange("p (h c) -> p h c", h=2)[:, :, 0:129],
                    in1=bcl(hv.ap[:, 12, par:4:2], 2, 129), op=ALU.mult), reads=(pq[par], hv), writes=(hr,))
            for b in range(2):
                kb.op("dve", lambda e, b=b: e.tensor_tensor(
                    out=hr.ap[:, 2 * b:2 * b + 2, :], in0=hr.ap[:, 2 * b:2 * b + 2, :],
                    in1=pn[b].ap.rearrange("p (h c) -> p h c", h=2)[:, :, 0:129], op=ALU.add),
                    reads=(hr, pn[b]), writes=(hr,))
            kb.op("act", lambda e: e.activation(out=hs.ap[:, 3, 8:12], in_=hr.ap[:, :, 128], func=AF.Abs),
                  reads=(hr,), writes=(hs,))
            kb.op("dve", lambda e: e.tensor_scalar(out=hs.ap[:, 3, 8:12], in0=hs.ap[:, 3, 8:12], scalar1=1.0,
                                                   scalar2=None, op0=ALU.max), reads=(hs,), writes=(hs,))
            kb.op("dve", lambda e: e.reciprocal(out=hs.ap[:, 3, 12:16], in_=hs.ap[:, 3, 8:12]), reads=(hs,), writes=(hs,))
            kb.op("dve", lambda e: e.tensor_tensor(out=hr.ap[:, :, 0:128], in0=hr.ap[:, :, 0:128],
                                                   in1=bcl(hs.ap[:, 3, 12:16], 4, 128), op=ALU.mult),
                  reads=(hr, hs), writes=(hr,))
            kb.op("dve", lambda e: e.tensor_tensor(out=self.ybf.ap[:, 512:1024].rearrange("p (h c) -> p h c", h=4),
                                                   in0=hr.ap[:, :, 0:128],
                                                   in1=gt.ap[:, 512:1024].rearrange("p (h c) -> p h c", h=4),
                                                   op=ALU.mult), reads=(hr, gt), writes=(self.ybf,))

        def mlstm_part3():
            vext = mstate["vext"]
            psm = [self.psum(), self.psum()]
            for m in range(4):
                cs = slice((m % 2) * 256, (m % 2) * 256 + 129)
                kb.op("pe", lambda e, m=m, cs=cs: e.matmul(psm[m // 2].ap[:, cs],
                                                           lhsT=self.kwm.ap[:, (m // 2) * 128:(m // 2) * 128 + 128],
                                                           rhs=vext.ap[:, m, :], start=True, stop=True),
                      reads=(self.kwm, vext), writes=(psm[m // 2],))
            for m in range(4):
                r0 = (m % 2) * 64
                cs = slice((m % 2) * 256, (m % 2) * 256 + 129)
                kb.op("dve", lambda e, m=m, r0=r0, cs=cs: e.scalar_tensor_tensor(
                    out=self.cn.ap[r0:r0 + 64, m // 2, :], in0=self.cn.ap[r0:r0 + 64, m // 2, :],
                    scalar=hv.ap[r0:r0 + 64, 14, m:m + 1], in1=psm[m // 2].ap[r0:r0 + 64, cs],
                    op0=ALU.mult, op1=ALU.add), reads=(self.cn, hv, psm[m // 2]), writes=(self.cn,))
            kb.op("act", lambda e: e.activation(out=self.cnb.ap, in_=self.cn.ap, func=AF.Copy),
                  reads=(self.cn,), writes=(self.cnb,))

        for k in range(1, 7):
            if k == 2:
                mlstm_part1()
            elif k == 4:
                mlstm_part2()
            elif k == 6:
                mlstm_part3()
            (Ypt, Yp), (Xpt, Xp) = Y[(k - 1) % 2], X[(k - 1) % 2]
            (Ynt, Yn), (Xnt, Xn) = Y[k % 2], X[k % 2]
            px = [self.psum(), self.psum()]
            for h in range(8):
                cs = slice((h % 4) * 128, (h % 4) * 128 + 128)
                kb.op("pe", lambda e, h=h, cs=cs, Yp=Yp, Xp=Xp, px=px: e.matmul(
                    px[h // 4].ap[:, cs], lhsT=Yp[:, h, :], rhs=Xp[:, h, :], start=True, stop=True),
                    reads=(Ypt, Xpt), writes=(px[h // 4],))
            kb.op("act", lambda e, Xn=Xn, px=px: e.activation(out=Xn[:, 0:4, :],
                                                              in_=px[0].ap.rearrange("p (h c) -> p h c", h=4),
                                                              func=AF.Copy), reads=(px[0],), writes=(Xnt,))
            kb.op("dve", lambda e, Xn=Xn, px=px: e.tensor_copy(out=Xn[:, 4:8, :],
                                                               in_=px[1].ap.rearrange("p (h c) -> p h c", h=4)),
                  reads=(px[1],), writes=(Xnt,))
            if k < 6:
                py = [self.psum(), self.psum()]
                for h in range(8):
                    cs = slice((h % 4) * 128, (h % 4) * 128 + 128)
                    kb.op("pe", lambda e, h=h, cs=cs, Yp=Yp, Xp=Xp, py=py: e.matmul(
                        py[h // 4].ap[:, cs], lhsT=Xp[:, h, :], rhs=Yp[:, h, :], start=True, stop=True),
                        reads=(Ypt, Xpt), writes=(py[h // 4],))
                kb.op("act", lambda e, Yn=Yn, py=py: e.activation(out=Yn[:, 0:4, :],
                                                                  in_=py[0].ap.rearrange("p (h c) -> p h c", h=4),
                                                                  func=AF.Copy), reads=(py[0],), writes=(Ynt,))
                kb.op("dve", lambda e, Yn=Yn, py=py: e.tensor_copy(out=Yn[:, 4:8, :],
                                                                   in_=py[1].ap.rearrange("p (h c) -> p h c", h=4)),
                      reads=(py[1],), writes=(Ynt,))
            pp = [self.psum(), self.psum()]
            for h in range(8):
                cs = slice((h % 4) * 128, (h % 4) * 128 + 128)
                kb.op("pe", lambda e, h=h, cs=cs, Xn=Xn, pp=pp: e.matmul(
                    pp[h // 4].ap[:, cs], lhsT=Xn[:, h, :], rhs=P[:, h, :], start=True, stop=True),
                    reads=(Xnt, P_t), writes=(pp[h // 4],))
            for half in range(2):
                kb.op("dve", lambda e, half=half, pp=pp: e.tensor_tensor(
                    out=P[:, half * 4:(half + 1) * 4, :], in0=P[:, half * 4:(half + 1) * 4, :],
                    in1=pp[half].ap.rearrange("p (h c) -> p h c", h=4), op=ALU.add),
                    reads=(P_t, pp[half]), writes=(P_t,))
        k_tm = self.qk_tm.ap[:, 512:1024]
        kb.op("dve", lambda e: e.tensor_tensor(out=self.bv.ap.rearrange("p (h c) -> p h c", h=8),
                                               in0=self.v_tm.ap.rearrange("p (h c) -> p h c", h=8),
                                               in1=bcl(H(8), 8, 64), op=ALU.mult),
              reads=(self.v_tm, hv), writes=(self.bv,))
        kb.op("dve", lambda e: e.tensor_tensor(out=self.bk.ap.rearrange("p (h c) -> p h c", h=8),
                                               in0=k_tm.rearrange("p (h c) -> p h c", h=8),
                                               in1=bcl(H(7), 8, 64), op=ALU.mult),
              reads=(self.qk_tm, hv), writes=(self.bk,))
        kb.op("pool", lambda e: e.tensor_tensor(out=self.kw.ap.rearrange("p (h c) -> p h c", h=8),
                                                in0=k_tm.rearrange("p (h c) -> p h c", h=8),
                                                in1=bcl(H(6), 8, 64), op=ALU.mult),
              reads=(self.qk_tm, hv), writes=(self.kw,))
        pu0 = self.psum()
        pwt = [self.psum(), self.psum()]
        for h in range(8):
            cs = slice((h % 4) * 128, (h % 4) * 128 + 128)
            kb.op("pe", lambda e, h=h: e.matmul(pu0.ap[:, h * 64:(h + 1) * 64], lhsT=P[:, h, :],
                                                rhs=self.bv.ap[:, h * 64:(h + 1) * 64], start=True, stop=True),
                  reads=(P_t, self.bv), writes=(pu0,))
            kb.op("pe", lambda e, h=h, cs=cs: e.matmul(pwt[h // 4].ap[:, cs],
                                                       lhsT=self.bk.ap[:, (h // 2) * 128:(h // 2) * 128 + 128],
                                                       rhs=P[:, h, :], start=True, stop=True),
                  reads=(P_t, self.bk), writes=(pwt[h // 4],))
        kb.op("act", lambda e: e.activation(out=self.u0.ap, in_=pu0.ap, func=AF.Copy), reads=(pu0,), writes=(self.u0,))
        for h in range(8):
            r0 = (h % 2) * 64
            cs = slice((h % 4) * 128, (h % 4) * 128 + 128)
            eng = "dve" if h % 2 == 0 else "act"
            if eng == "dve":
                kb.op("dve", lambda e, h=h, r0=r0, cs=cs: e.tensor_copy(out=self.wtb.ap[r0:r0 + 64, h, :],
                                                                        in_=pwt[h // 4].ap[r0:r0 + 64, cs]),
                      reads=(pwt[h // 4],), writes=(self.wtb,))
            else:
                kb.op("act", lambda e, h=h, r0=r0, cs=cs: e.activation(out=self.wtb.ap[r0:r0 + 64, h, :],
                                                                       in_=pwt[h // 4].ap[r0:r0 + 64, cs],
                                                                       func=AF.Copy),
                      reads=(pwt[h // 4],), writes=(self.wtb,))
        pws = [self.psum(), self.psum()]
        for h in range(8):
            r0 = (h % 2) * 64
            kb.op("pe", lambda e, h=h, r0=r0: e.matmul(pws[h % 2].ap[:, (h // 2) * 64:(h // 2) * 64 + 64],
                                                       lhsT=self.wtb.ap[r0:r0 + 64, h, :],
                                                       rhs=self.sgb.ap[r0:r0 + 64, h // 2, :], start=True, stop=True),
                  reads=(self.wtb, self.sgb), writes=(pws[h % 2],))
        u3 = self.u.ap.rearrange("p (h c) -> p h c", h=8)
        u03 = self.u0.ap.rearrange("p (h c) -> p h c", h=8)
        for par in range(2):
            kb.op("dve", lambda e, par=par: e.tensor_tensor(
                out=u3[:, par:8:2, :], in0=u03[:, par:8:2, :],
                in1=pws[par].ap[:, 0:256].rearrange("p (h c) -> p h c", h=4), op=ALU.subtract),
                reads=(self.u0, pws[par]), writes=(self.u,))
        po = self.psum()
        pqs = [self.psum(), self.psum()]
        for h in range(8):
            r0 = (h % 2) * 64
            qt, qap = qT(h)
            kb.op("pe", lambda e, h=h: e.matmul(po.ap[:, h * 64:(h + 1) * 64], lhsT=AT[:, h, :],
                                                rhs=self.u.ap[:, h * 64:(h + 1) * 64], start=True, stop=True),
                  reads=(at_t, self.u), writes=(po,))
            kb.op("pe", lambda e, h=h, r0=r0, qap=qap: e.matmul(pqs[h % 2].ap[:, (h // 2) * 64:(h // 2) * 64 + 64],
                                                                lhsT=qap, rhs=self.sgb.ap[r0:r0 + 64, h // 2, :],
                                                                start=True, stop=True),
                  reads=(qt, self.sgb), writes=(pqs[h % 2],))
        ob = self.ytmp
        ob3 = ob.ap.rearrange("p (h c) -> p h c", h=8)
        for par in range(2):
            kb.op("dve", lambda e, par=par: e.tensor_tensor(
                out=ob3[:, par:8:2, :], in0=pqs[par].ap[:, 0:256].rearrange("p (h c) -> p h c", h=4),
                in1=bcl(hv.ap[:, 5, par:8:2], 4, 64), op=ALU.mult),
                reads=(pqs[par], hv), writes=(ob,))
        kb.op("dve", lambda e: e.tensor_tensor(out=ob.ap, in0=ob.ap, in1=po.ap, op=ALU.add),
              reads=(ob, po), writes=(ob,))
        psu = self.psum()
        for h in range(8):
            kb.op("pe", lambda e, h=h: e.matmul(psu.ap[:, h * 64:(h + 1) * 64],
                                                lhsT=self.kw.ap[:, (h // 2) * 128:(h // 2) * 128 + 128],
                                                rhs=self.u.ap[:, h * 64:(h + 1) * 64], start=True, stop=True),
                  reads=(self.kw, self.u), writes=(psu,))
        for h in range(8):
            r0 = (h % 2) * 64
            kb.op("dve", lambda e, h=h, r0=r0: e.scalar_tensor_tensor(
                out=self.sg.ap[r0:r0 + 64, h // 2, :], in0=self.sg.ap[r0:r0 + 64, h // 2, :],
                scalar=hv.ap[r0:r0 + 64, 9, h:h + 1], in1=psu.ap[r0:r0 + 64, h * 64:(h + 1) * 64],
                op0=ALU.mult, op1=ALU.add), reads=(self.sg, hv, psu), writes=(self.sg,))
        kb.op("act", lambda e: e.activation(out=self.sgb.ap, in_=self.sg.ap, func=AF.Copy),
              reads=(self.sg,), writes=(self.sgb,))
        sq2 = raw["ybuf"][:, 1024:1536]
        kb.op("act", lambda e: e.activation(out=sq2, in_=ob.ap, func=AF.Square), reads=(ob,), writes=(dg_t,))
        kb.op("dve", lambda e: e.reduce_sum(out=hs.ap[:, 2, 0:8], in_=sq2.rearrange("p (h c) -> p h c", h=8),
                                            axis=AX.X), reads=(dg_t,), writes=(hs,))
        kb.op("act", lambda e: e.activation(out=hs.ap[:, 2, 8:16], in_=hs.ap[:, 2, 0:8], func=AF.Sqrt,
                                            bias=float(RMS_EPS), scale=1.0 / 64.0), reads=(hs,), writes=(hs,))
        kb.op("dve", lambda e: e.reciprocal(out=hs.ap[:, 3, 0:8], in_=hs.ap[:, 2, 8:16]), reads=(hs,), writes=(hs,))
        kb.op("dve", lambda e: e.tensor_tensor(out=ob.ap.rearrange("p (h c) -> p h c", h=8),
                                               in0=ob.ap.rearrange("p (h c) -> p h c", h=8),
                                               in1=bcl(hs.ap[:, 3, 0:8], 8, 64), op=ALU.mult),
              reads=(ob, hs), writes=(ob,))
        kb.op("pool", lambda e: e.tensor_tensor(out=ob.ap.rearrange("p (h c) -> p h c", h=8),
                                                in0=ob.ap.rearrange("p (h c) -> p h c", h=8),
                                                in1=bcm(self.bp.ap[:, BP_G_NW:BP_G_NW + 64], 8, 64), op=ALU.mult),
              reads=(ob, self.bp), writes=(ob,))
        kb.op("dve", lambda e: e.tensor_tensor(out=self.ybf.ap[:, 0:512], in0=ob.ap, in1=gt.ap[:, 0:512], op=ALU.mult),
              reads=(ob, gt), writes=(self.ybf,))

        srcs = [(self.ybf, self.ybf.ap[:, q * 128:(q + 1) * 128]) for q in range(8)]
        self.transposes_to(srcs, tuple(self.hT[q] for q in range(8)), self.hT_full[:, 0:8, tc])

    def body(self):
        kb = self.kb
        self.prologue()
        for sc in range(self.nsc):
            if sc > 0:
                self.load_x(sc)
            for t in range(NT):
                self.to_xT(t)
            for l in self.layers:
                self.ffn_ln(l, "pre", sc, l * 3 + 0)
                if l % 2 == 1:
                    self.ssd_mixer(sc, l * 3 + 1)
                else:
                    self.hyb_mixer(sc, l * 3 + 1)
                self.ffn_ln(l, "post", sc, l * 3 + 2)
            self.store_y(sc)
        kb.finish()


def prep_inputs(inp, layers):
    ws = make_wstream(inp, layers)
    lnp = np.zeros((DEPTH * 3, 2, D_MODEL), np.float32)
    for l in range(DEPTH):
        lnp[l * 3 + 0, 0] = inp["ln_pre_g"][l]
        lnp[l * 3 + 0, 1] = inp["ln_pre_b"][l]
        lnp[l * 3 + 1, 0] = inp["ln_mix_g"][l]
        lnp[l * 3 + 1, 1] = inp["ln_mix_b"][l]
        lnp[l * 3 + 2, 0] = inp["ln_post_g"][l]
        lnp[l * 3 + 2, 1] = inp["ln_post_b"][l]
    j = np.arange(128)[:, None]
    i = np.arange(128)[None, :]
    cst = np.zeros((128, 5, 128), np.float32)
    cst[:, C_ID, :] = np.eye(128)
    cst[:, C_ONES, :] = 1.0
    cst[:, C_MINC, :] = np.where(j <= i, 0.0, -1e30)
    cst[:, C_MSTR, :] = np.where(j < i, 0.0, -1e30)
    cst[:, C_TRI, :] = (j <= i)
    bpar = np.zeros((NBP,), np.float32)
    bpar[BP_SSD_DTB:BP_SSD_DTB + 32] = inp["ssd_dt_bias"][0]
    bpar[BP_SSD_ALOG:BP_SSD_ALOG + 32] = inp["ssd_a_log"][0]
    bpar[BP_SSD_D:BP_SSD_D + 32] = inp["ssd_d_skip"][0]
    bpar[BP_G_ALOG:BP_G_ALOG + 8] = inp["gdn_a_log"][0]
    bpar[BP_G_DTB:BP_G_DTB + 8] = inp["gdn_dt_bias"][0]
    bpar[BP_G_NW:BP_G_NW + 64] = inp["gdn_norm_w"][0]
    bpar[BP_M_IB:BP_M_IB + 4] = inp["mlstm_i_bias"][0]
    bpar[BP_M_FB:BP_M_FB + 4] = inp["mlstm_f_bias"][0]
    bpar[BP_H_SGN:BP_H_SGN + 24] = np.array([-1] * 8 + [1] * 8 + [1] * 4 + [-1] * 4, np.float32)
    bpar[BP_H_BIAS + 8:BP_H_BIAS + 16] = inp["gdn_dt_bias"][0]
    bpar[BP_H_BIAS + 16:BP_H_BIAS + 20] = inp["mlstm_i_bias"][0]
    bpar[BP_H_BIAS + 20:BP_H_BIAS + 24] = inp["mlstm_f_bias"][0]
    convw = np.zeros((128, 36, 5), np.float32)
    hw = inp["hyb_conv_w"][0]
    convw[:, 0:12, 0:4] = hw.reshape(4, 12, 128).transpose(2, 1, 0)
    sw = inp["ssd_conv_w"][0]
    convw[:, 12:36, 0:4] = sw.reshape(4, 24, 128).transpose(2, 1, 0)
    convw[:, 12:36, 4] = inp["ssd_conv_b"][0].reshape(24, 128).T
    colp = np.ascontiguousarray(inp["ssd_norm_w"][0].reshape(16, 128).T)
    return ws, lnp, dict(cst=cst, bpar=bpar, convw=convw, colp=colp)


def kernel(**inputs):
    inp = {k: np.asarray(v) for k, v in inputs.items()}
    layers = list(range(DEPTH))
    ws, lnp, small = prep_inputs(inp, layers)
    warr = ws.array()
    prog = Prog(SEQ, layers, ws.n, ws.index)
    nc = prog.build()
    x = inp["x"]
    in_maps = []
    for b in range(BATCH):
        m = {"x": np.ascontiguousarray(x[b]), "wsrc": warr, "lnp": lnp}
        m.update(small)
        in_maps.append(m)
    res = run_bass_kernel_spmd(nc, in_maps, core_ids=list(range(BATCH)))
    out = np.stack([res.results[b]["y"] for b in range(BATCH)], axis=0)
    return out.astype(np.float32)
```

```python
import math
from contextlib import ExitStack

import numpy as np
import concourse.bass as bass
import concourse.mybir as mybir
from concourse.bass_utils import run_bass_kernel_spmd

F32 = mybir.dt.float32
BF16 = mybir.dt.bfloat16
ALU = mybir.AluOpType
AF = mybir.ActivationFunctionType
AX = mybir.AxisListType

D_MODEL = 1024
BATCH = 4
SEQ = 8192
DEPTH = 2
DN_ALPHA = (2 * DEPTH) ** 0.25
LN_EPS = 1e-5
D_FF = 2816
NFC = D_FF // 128
TS = 512
NT = TS // 128
WBLK = 2048
NSLOT = 5
NDS = 12
NBP = 256
BP_SSD_DTB, BP_SSD_ALOG, BP_SSD_D = 0, 32, 64
BP_G_ALOG, BP_G_DTB, BP_G_NW, BP_M_IB, BP_M_FB = 96, 104, 112, 176, 180
C_ID, C_ONES, C_MINC, C_MSTR, C_TRI = 0, 1, 2, 3, 4
RMS_EPS = 1e-6
BP_H_SGN, BP_H_BIAS = 192, 216

ENGS = ("sp", "act", "dve", "pe", "pool")


class Tile:
    __slots__ = ("ap", "w", "rs", "name")

    def __init__(self, ap, name=""):
        self.ap = ap
        self.name = name
        self.w = None
        self.rs = {}


class Plan:
    def __init__(self):
        self.needed = set()
        self.cnt = None

    def finalize(self):
        per = {}
        for (p, s) in self.needed:
            per.setdefault(p, []).append(s)
        self.cnt = {}
        for p, lst in per.items():
            lst.sort()
            self.cnt[p] = {s: i + 1 for i, s in enumerate(lst)}


class KB:
    def __init__(self, nc, plan, sems, dsems, tiles):
        self.nc = nc
        self.plan = plan
        self.sems = sems
        self.dsems = dsems
        self.tiles = tiles
        self.cur = None
        self.eng = None

    def begin(self, cur, eng):
        self.cur = cur
        self.eng = eng
        self.seq = {e: 0 for e in ENGS}
        self.known = {e: {} for e in ENGS}
        self.ndma = 0
        self.ndma_q = [0, 0]
        self.psum_rr = 0
        self.wr_rr = 0
        self.misc = {}
        for t in self.tiles:
            t.w = None
            t.rs = {}

    def _need(self, E, P, ps, s_cur):
        if P == E:
            if E == "pe":
                return
            if ps < s_cur - 3:
                return
        kn = self.known[E]
        if kn.get(P, 0) >= ps:
            return
        kn[P] = ps
        if self.cur is None:
            self.plan.needed.add((P, ps))
        elif self.cur == E:
            if P[0] == "q":
                self.eng.wait_ge(self.dsems[int(P[1:])], 16 * ps)
            else:
                self.eng.wait_ge(self.sems[P], self.plan.cnt[P][ps])

    def _deps(self, E, reads, writes, s_cur, extra=()):
        for t in reads:
            if t.w is not None:
                self._need(E, t.w[0], t.w[1], s_cur)
        for t in writes:
            if t.w is not None:
                self._need(E, t.w[0], t.w[1], s_cur)
            for p, s in t.rs.items():
                self._need(E, p, s, s_cur)
        for (p, s) in extra:
            self._need(E, p, s, s_cur)

    def _mark(self, tok, reads, writes):
        for t in reads:
            if t.rs.get(tok[0], 0) < tok[1]:
                t.rs[tok[0]] = tok[1]
        for t in writes:
            t.w = tok
            t.rs = {}

    def op(self, E, fn, reads=(), writes=()):
        s = self.seq[E] + 1
        self.seq[E] = s
        self._deps(E, reads, writes, s)
        tok = (E, s)
        if self.cur == E:
            ins = fn(self.eng)
            if tok in self.plan.needed:
                ins.then_inc(self.sems[E], 1)
        self._mark(tok, reads, writes)
        return tok

    def dma(self, Q, out, in_, reads=(), writes=(), deps=()):
        half = NDS // 2
        qi = 0 if Q == "sp" else 1
        i = self.ndma_q[qi]
        self.ndma_q[qi] += 1
        self.ndma += 1
        j = qi * half + (i % half)
        ds = i // half + 1
        s = self.seq[Q] + 1
        self.seq[Q] = s
        extra = ((("q%d" % j), ds - 1),) if ds > 1 else ()
        extra = extra + tuple(deps)
        self._deps(Q, reads, writes, s, extra)
        tok = ("q%d" % j, ds)
        if self.cur == Q:
            self.eng.dma_start(out=out, in_=in_).then_inc(self.dsems[j], 16)
        self._mark(tok, reads, writes)
        return tok

    def wait_all_dma(self, E):
        half = NDS // 2
        s = self.seq[E] + 1
        for qi in range(2):
            n = self.ndma_q[qi]
            for jj in range(half):
                if n > jj:
                    ds = (n - 1 - jj) // half + 1
                    self._need(E, "q%d" % (qi * half + jj), ds, s)

    def finish(self):
        self.wait_all_dma("sp")


def _blocks_kc(w, cols_per_blk=256):
    K, C = w.shape
    nkc = K // 128
    assert nkc * cols_per_blk == WBLK
    nb = (C + cols_per_blk - 1) // cols_per_blk
    wp = np.zeros((K, nb * cols_per_blk), np.float32)
    wp[:, :C] = w
    wp = wp.reshape(nkc, 128, nb, cols_per_blk).transpose(2, 1, 0, 3)
    return np.ascontiguousarray(wp).reshape(nb, 128, WBLK)


def _blocks_rows(w, rows_per_blk=2):
    K, C = w.shape
    assert C == 1024 and rows_per_blk * C == WBLK
    nkc = K // 128
    nb = nkc // rows_per_blk
    wp = w.reshape(nb, rows_per_blk, 128, C).transpose(0, 2, 1, 3)
    return np.ascontiguousarray(wp).reshape(nb, 128, WBLK)


def _blocks_gu(wg, wu):
    g = wg.reshape(8, 128, NFC, 128).transpose(2, 1, 0, 3)
    u = wu.reshape(8, 128, NFC, 128).transpose(2, 1, 0, 3)
    gu = np.stack([g, u], axis=2)
    return np.ascontiguousarray(gu).reshape(NFC, 128, WBLK)


class WStream:
    def __init__(self):
        self.parts = []
        self.index = {}
        self.n = 0

    def add(self, name, blocks):
        self.index[name] = (self.n, blocks.shape[0])
        self.parts.append(blocks)
        self.n += blocks.shape[0]

    def array(self):
        return np.concatenate(self.parts, axis=0)


def make_wstream(inp, layers):
    ws = WStream()
    for l in layers:
        ws.add("pre_gu%d" % l, _blocks_gu(inp["ffn_pre_w_gate"][l], inp["ffn_pre_w_up"][l]))
        ws.add("pre_d%d" % l, _blocks_rows(inp["ffn_pre_w_down"][l]))
        ws.add("post_gu%d" % l, _blocks_gu(inp["ffn_post_w_gate"][l], inp["ffn_post_w_up"][l]))
        ws.add("post_d%d" % l, _blocks_rows(inp["ffn_post_w_down"][l]))
        if l % 2 == 1:
            w = inp["ssd_w_in"][0]
            ws.add("ssd_in", np.concatenate([_blocks_kc(w[:, 2048:5120]), _blocks_kc(w[:, 0:2048]),
                                             _blocks_kc(w[:, 5120:5152])], axis=0))
            ws.add("ssd_out", _blocks_rows(inp["ssd_w_out"][0]))
        else:
            ws.add("hyb_in", _blocks_kc(hyb_perm(inp["hyb_w_in"][0])))
            ws.add("hyb_out", _blocks_rows(inp["hyb_w_out"][0]))
    return ws


def hyb_perm(w):
    o = np.cumsum([0, 512, 512, 512, 512, 8, 8, 256, 256, 512, 512, 4, 4])
    gq, gk, gv, gz, gb, ga, mq, mk, mv, mo, mi, mf = [w[:, o[i]:o[i + 1]] for i in range(12)]
    small = np.zeros((w.shape[0], 256), np.float32)
    small[:, 0:8] = gb
    small[:, 8:16] = ga
    small[:, 16:20] = mi
    small[:, 20:24] = mf
    return np.concatenate([gq, gk, gv, mq, mk, gz, mo, mv, mk, small], axis=1)


class Prog:
    def __init__(self, ntok, layers, nblk, windex, debug_stop=None):
        self.ntok = ntok
        self.layers = layers
        self.nblk = nblk
        self.windex = windex
        self.nsc = ntok // TS
        self.debug_stop = debug_stop
        nc = bass.Bass("TRN2", target_bir_lowering=False)
        self.nc = nc
        self.x = nc.dram_tensor("x", [ntok, D_MODEL], F32, kind="ExternalInput").ap()
        self.wsrc = nc.dram_tensor("wsrc", [nblk, 128, WBLK], F32, kind="ExternalInput").ap()
        self.lnp_d = nc.dram_tensor("lnp", [DEPTH * 3, 2, D_MODEL], F32, kind="ExternalInput").ap()
        self.cst_d = nc.dram_tensor("cst", [128, 5, 128], F32, kind="ExternalInput").ap()
        self.bpar_d = nc.dram_tensor("bpar", [NBP], F32, kind="ExternalInput").ap()
        self.convw_d = nc.dram_tensor("convw", [128, 36, 5], F32, kind="ExternalInput").ap()
        self.colp_d = nc.dram_tensor("colp", [128, 16], F32, kind="ExternalInput").ap()
        self.y = nc.dram_tensor("y", [ntok, D_MODEL], F32, kind="ExternalOutput").ap()
        self.wbf = nc.dram_tensor("wbf", [nblk, 128, WBLK], BF16).ap()

    def build(self):
        nc = self.nc
        with ExitStack() as es:
            def sb(name, shape, dt):
                return es.enter_context(nc.sbuf_tensor(name, shape, dt))

            self.tiles = []

            def T(ap, name=""):
                t = Tile(ap, name)
                self.tiles.append(t)
                return t

            xtm = sb("xtm", [128, NT, D_MODEL], F32)
            self.xtm = [T(xtm[:, t, :], "xtm%d" % t) for t in range(NT)]
            xT = sb("xT", [128, 8, TS], BF16)
            self.xT_full = xT
            self.xT = [T(xT[:, :, t * 128:(t + 1) * 128], "xT%d" % t) for t in range(NT)]
            hT = sb("hT", [128, NFC, TS], BF16)
            self.hT_full = hT
            self.hT = [T(hT[:, j, :], "hT%d" % j) for j in range(NFC)]
            wring = sb("wring", [128, NSLOT, WBLK], BF16)
            self.wslot = [T(wring[:, s, :], "w%d" % s) for s in range(NSLOT)]
            lnp = sb("lnpb", [128, 1, 2, D_MODEL], F32)
            self.lnp = [T(lnp[:, 0, :, :], "lnp0")]
            cf = sb("cf", [128, 5, 128], F32)
            self.cf = T(cf[:], "cf")
            bp = sb("bp", [128, NBP], F32)
            self.bp = T(bp[:], "bp")
            cw = sb("cw", [128, 36, 5], F32)
            self.cw = T(cw[:], "cw")
            colp = sb("colp_sb", [128, 16], F32)
            self.colp = T(colp[:], "colp")
            aneg = sb("aneg", [128, 32], F32)
            self.aneg = T(aneg[:], "aneg")
            hist = sb("hist", [128, 36, 3], F32)
            self.hist_full = hist
            self.hist = [T(hist[:, c, :], "hist%d" % c) for c in range(36)]
            fm = sb("fm", [128, 24, TS], BF16)
            self.fm = [T(fm[:, c, :], "fm%d" % c) for c in range(24)]
            gates = sb("gates", [128, NT, 2048], BF16)
            self.gates = [T(gates[:, t, :], "gates%d" % t) for t in range(NT)]
            sm = sb("sm", [128, NT, 4, 32], F32)
            self.sm = [T(sm[:, t, :, :], "sm%d" % t) for t in range(NT)]
            sv = sb("sv", [128, 8, 32], F32)
            self.sv = T(sv[:], "sv")
            cst2 = sb("cstage", [128, 1, TS + 3], F32)
            self.cstage = [T(cst2[:, i, :], "cstage%d" % i) for i in range(1)]
            cacc = sb("cacc", [128, 2, TS], F32)
            self.cacc_full = cacc
            self.cacc = [T(cacc[:, i, :], "cacc%d" % i) for i in range(2)]
            self.gact = self.cacc
            xs_tm = sb("xs_tm", [128, 2048], BF16)
            self.xs_tm = T(xs_tm[:], "xs_tm")
            xdt = sb("xdt", [128, 2048], BF16)
            self.xdt = T(xdt[:], "xdt")
            xw = sb("xw", [128, 2048], BF16)
            self.xw = T(xw[:], "xw")
            xD = sb("xD", [128, 2048], BF16)
            self.xD = T(xD[:], "xD")
            b_tm = sb("b_tm", [128, 512], BF16)
            self.b_tm = T(b_tm[:], "b_tm")
            yb = sb("ybuf", [128, 2048], F32)
            self.ybuf = T(yb[:], "ybuf")
            ysq = sb("ysq", [128, 2048], F32)
            self.ysq_full = ysq
            self.ysq_h = [T(ysq[:, 0:1024], "ysq_a"), T(ysq[:, 1024:2048], "ysq_b")]
            self.wtmp = [(self.ybuf, yb[:, 0:1024]), (self.ybuf, yb[:, 1024:2048]),
                         (self.ysq_h[0], ysq[:, 0:1024]), (self.ysq_h[1], ysq[:, 1024:2048])]
            ybf = sb("ybf", [128, 2048], BF16)
            self.ybf = T(ybf[:], "ybf")
            self.xbf = [(self.ybf, ybf[:, 0:1024]), (self.ybf, ybf[:, 1024:2048])]
            ytmp = sb("ytmp", [128, 512], F32)
            self.ytmp = T(ytmp[:], "ytmp")
            sst = sb("sstate", [128, 4, 512], F32)
            self.sst = [T(sst[:, g, :], "sst%d" % g) for g in range(4)]
            sstb = sb("sstate_bf", [128, 4, 512], BF16)
            self.sstb = [T(sstb[:, g, :], "sstb%d" % g) for g in range(4)]
            m5 = sb("m_AT8", [128, 8, 128], BF16)
            self.m_AT8 = T(m5[:], "m_AT8")
            kqsb = sb("kq_sb", [128, 2, 128], F32)
            self.kq_sb = [T(kqsb[:, i, :], "kq_sb%d" % i) for i in range(2)]
            ss = sb("ss", [128, 4, 4], F32)
            self.ss = T(ss[:], "ss")
            self.raw = dict(xs_tm=xs_tm, xdt=xdt, xw=xw, xD=xD, ybuf=yb, ysq=ysq)
            mask4 = sb("mask4", [128, 4, 128], F32)
            self.mask4 = T(mask4[:], "mask4")
            qk_tm = sb("qk_tm", [128, 1024], BF16)
            self.qk_tm = T(qk_tm[:], "qk_tm")
            v_tm = sb("v_tm", [128, 512], BF16)
            self.v_tm = T(v_tm[:], "v_tm")
            bvk = sb("bvk", [128, 2, 512], F32)
            self.bv = T(bvk[:, 0, :], "bv")
            self.bk = T(bvk[:, 1, :], "bk")
            u0 = sb("u0sb", [128, 512], F32)
            self.u0 = T(u0[:], "u0sb")
            ukw = sb("ukw", [128, 2, 512], BF16)
            self.u = T(ukw[:, 0, :], "u")
            self.kw = T(ukw[:, 1, :], "kw")
            wtb = sb("wtb", [128, 8, 128], BF16)
            self.wtb = T(wtb[:], "wtb")
            hv = sb("hv", [128, 16, 8], F32)
            self.hv = T(hv[:], "hv")
            hs = sb("hs", [128, 4, 16], F32)
            self.hs = T(hs[:], "hs")
            hmul = sb("hmul", [128, 24], F32)
            self.hmul = T(hmul[:], "hmul")
            vext = sb("vext", [128, 4, 129], BF16)
            self.vext = T(vext[:], "vext")
            atm = sb("atm", [128, 4, 128], BF16)
            self.atm = T(atm[:], "atm")
            hraw = sb("hraw", [128, 4, 129], F32)
            self.hraw = T(hraw[:], "hraw")
            self.raw["hraw"] = hraw
            kwm = sb("kwm", [128, 256], BF16)
            self.kwm = T(kwm[:], "kwm")
            sg = sb("sg", [128, 4, 64], F32)
            self.sg = T(sg[:], "sg")
            sgb = sb("sgb", [128, 4, 64], BF16)
            self.sgb = T(sgb[:], "sgb")
            cn = sb("cn", [128, 2, 129], F32)
            self.cn = T(cn[:], "cn")
            cnb = sb("cnb", [128, 2, 129], BF16)
            self.cnb = T(cnb[:], "cnb")
            st = sb("st", [128, NT, 2, 6], F32)
            self.st = [T(st[:, t, :, :], "st%d" % t) for t in range(NT)]
            mv = sb("mv", [128, NT, 2], F32)
            self.mv = T(mv[:], "mv")
            rs = sb("rs", [128, 3, NT], F32)
            self.rs = T(rs[:], "rs")
            identb = sb("identb", [128, 128], BF16)
            self.identb = T(identb[:], "identb")
            self.ps = []
            for b in range(8):
                p = es.enter_context(nc.psum_tensor("ps%d" % b, [128, 512], F32))
                self.ps.append(T(p[:], "ps%d" % b))

            sems = {e: es.enter_context(nc.semaphore("s_" + e)) for e in ENGS}
            dsems = [es.enter_context(nc.semaphore("q%d" % j)) for j in range(NDS)]
            plan = Plan()
            kb = KB(nc, plan, sems, dsems, self.tiles)
            self.kb = kb
            kb.begin(None, None)
            self.body()
            plan.finalize()
            block = es.enter_context(nc.Block())

            def run(name):
                def f(e):
                    kb.begin(name, e)
                    self.body()
                return f

            block.sync(run("sp"))
            block.scalar(run("act"))
            block.vector(run("dve"))
            block.tensor(run("pe"))
            block.gpsimd(run("pool"))
        return nc

    def psum(self, n=8):
        kb = self.kb
        t = self.ps[kb.psum_rr % n]
        kb.psum_rr += 1
        return t

    def rot(self, lst, key):
        kb = self.kb
        i = kb.misc.get(key, 0)
        kb.misc[key] = i + 1
        return lst[i % len(lst)]

    def wload(self, blk):
        kb = self.kb
        slot = self.wslot[kb.wr_rr % NSLOT]
        kb.wr_rr += 1
        tok = self.wbf_toks.pop(blk, None)
        kb.dma("sp", slot.ap, self.wbf[blk], reads=(), writes=(slot,), deps=(tok,) if tok is not None else ())
        return slot

    def prologue(self):
        kb = self.kb
        kb.dma("pool", self.identb.ap, self.cst_d[:, 0, :], writes=(self.identb,))
        kb.dma("pool", self.cf.ap, self.cst_d, writes=(self.cf,))
        kb.dma("pool", self.bp.ap, self.bpar_d.partition_broadcast(128), writes=(self.bp,))
        kb.dma("pool", self.cw.ap, self.convw_d, writes=(self.cw,))
        kb.dma("pool", self.colp.ap, self.colp_d, writes=(self.colp,))
        for q in range(4):
            kb.dma("pool", self.mask4.ap[:, q, :], self.cst_d[:, C_MINC, :], writes=(self.mask4,))
        self.load_x(0)
        self.wbf_toks = {}
        for b in range(self.nblk):
            self.wbf_toks[b] = kb.dma("pool", self.wbf[b], self.wsrc[b])
        self.setup_state()
        self.setup_hyb()

    def load_x(self, sc):
        kb = self.kb
        for t in range(NT):
            r0 = sc * TS + t * 128
            kb.dma("pool", self.xtm[t].ap, self.x[r0:r0 + 128, :], writes=(self.xtm[t],))

    def store_y(self, sc):
        kb = self.kb
        for t in range(NT):
            r0 = sc * TS + t * 128
            kb.dma("pool", self.y[r0:r0 + 128, :], self.xtm[t].ap, reads=(self.xtm[t],))

    def to_xT_cast(self, t):
        kb = self.kb
        xb, xb_ap = self.xbf[t % 2]
        src = self.xtm[t]
        kb.op("act", lambda e: e.activation(out=xb_ap, in_=src.ap, func=AF.Copy),
              reads=(src,), writes=(xb,))

    def to_xT_T(self, t):
        kb = self.kb
        xb, xb_ap = self.xbf[t % 2]
        p = self.psum()
        pb = p.ap.bitcast(BF16)
        for kc in range(8):
            kb.op("pe", lambda e, kc=kc: e.transpose(pb[:, kc * 128:(kc + 1) * 128],
                                                     xb_ap[:, kc * 128:(kc + 1) * 128],
                                                     self.identb.ap),
                  reads=(xb, self.identb), writes=(p,))
        dst = self.xT[t]
        kb.op("dve", lambda e: e.tensor_copy(out=dst.ap, in_=pb.rearrange("p (k c) -> p k c", k=8)),
              reads=(p,), writes=(dst,))

    def to_xT(self, t):
        self.to_xT_cast(t)
        self.to_xT_T(t)

    def down_ln(self, blk0, nkc, c, ln_idx):
        kb = self.kb
        lp = self.lnp[0]
        kb.dma("pool", lp.ap, self.lnp_d[ln_idx].partition_broadcast(128), writes=(lp,))
        for tp in range(NT // 2):
            banks = [self.psum(), self.psum(), self.psum(), self.psum()]
            for b in range(nkc // 2):
                wsl = self.wload(blk0 + b)
                wv = wsl.ap.rearrange("p (r c) -> p r c", r=2)
                for r in range(2):
                    kc = b * 2 + r
                    for ti in range(2):
                        t = tp * 2 + ti
                        for dh in range(2):
                            pt = banks[ti * 2 + dh]
                            kb.op("pe", lambda e, r=r, kc=kc, t=t, dh=dh, pt=pt, wv=wv: e.matmul(
                                pt.ap, lhsT=self.hT[kc].ap[:, t * 128:(t + 1) * 128],
                                rhs=wv[:, r, dh * 512:(dh + 1) * 512],
                                start=(kc == 0), stop=(kc == nkc - 1)),
                                reads=(wsl, self.hT[kc]), writes=(pt,))
            if tp > 0:
                self.to_xT_T(tp * 2 - 2)
                self.to_xT_T(tp * 2 - 1)
            for ti in range(2):
                self.resid_ln_tile(tp * 2 + ti, banks[ti * 2:ti * 2 + 2], c, lp)
        self.to_xT_T(NT - 2)
        self.to_xT_T(NT - 1)

    def resid_ln_tile(self, t, banks, c, lp):
        kb = self.kb
        eps = LN_EPS / (DN_ALPHA * DN_ALPHA)
        wt, wap = self.wtmp[t]
        x = self.xtm[t]
        rs = self.rs
        for dh in range(2):
            pt = banks[dh]
            sl = slice(dh * 512, (dh + 1) * 512)
            kb.op("dve", lambda e, pt=pt, sl=sl: e.scalar_tensor_tensor(
                out=wap[:, sl], in0=pt.ap, scalar=float(c), in1=x.ap[:, sl],
                op0=ALU.mult, op1=ALU.add), reads=(pt, x), writes=(wt,))
        stt = self.st[t]
        for dh in range(2):
            kb.op("dve", lambda e, dh=dh: e.bn_stats(out=stt.ap[:, dh, :], in_=wap[:, dh * 512:(dh + 1) * 512]),
                  reads=(wt,), writes=(stt,))
        kb.op("dve", lambda e: e.bn_aggr(out=self.mv.ap[:, t, :], in_=stt.ap), reads=(stt,), writes=(self.mv,))
        kb.op("act", lambda e: e.activation(out=rs.ap[:, 0, t:t + 1], in_=self.mv.ap[:, t, 1:2],
                                            func=AF.Sqrt, bias=float(eps), scale=1.0),
              reads=(self.mv,), writes=(rs,))
        kb.op("dve", lambda e: e.reciprocal(out=rs.ap[:, 1, t:t + 1], in_=rs.ap[:, 0, t:t + 1]),
              reads=(rs,), writes=(rs,))
        kb.op("dve", lambda e: e.scalar_tensor_tensor(out=rs.ap[:, 2, t:t + 1], in0=self.mv.ap[:, t, 0:1],
                                                      scalar=-1.0, in1=rs.ap[:, 1, t:t + 1],
                                                      op0=ALU.mult, op1=ALU.mult),
              reads=(self.mv, rs), writes=(rs,))
        kb.op("act", lambda e: e.activation(out=wap, in_=wap, func=AF.Identity, bias=rs.ap[:, 2, t:t + 1],
                                            scale=rs.ap[:, 1, t:t + 1]), reads=(wt, rs), writes=(wt,))
        kb.op("dve", lambda e: e.tensor_tensor(out=wap, in0=wap, in1=lp.ap[:, 0, :], op=ALU.mult),
              reads=(wt, lp), writes=(wt,))
        kb.op("dve", lambda e: e.tensor_tensor(out=x.ap, in0=wap, in1=lp.ap[:, 1, :], op=ALU.add),
              reads=(wt, lp), writes=(x,))
        self.to_xT_cast(t)

    def ffn_ln(self, l, which, sc, ln_idx):
        kb = self.kb
        gu0, ngu = self.windex["%s_gu%d" % (which, l)]
        d0, nd = self.windex["%s_d%d" % (which, l)]
        for j in range(NFC):
            wsl = self.wload(gu0 + j)
            wv = wsl.ap.rearrange("p (g k f) -> p g k f", g=2, k=8)
            pg = self.psum()
            pu = self.psum()
            for g, pt in ((0, pg), (1, pu)):
                for kc in range(8):
                    kb.op("pe", lambda e, g=g, kc=kc, pt=pt: e.matmul(
                        pt.ap, lhsT=wv[:, g, kc, :], rhs=self.xT_full[:, kc, :],
                        start=(kc == 0), stop=(kc == 7)),
                        reads=(wsl,) + tuple(self.xT), writes=(pt,))
            ga = self.gact[j % 2]
            kb.op("act", lambda e: e.activation(out=ga.ap, in_=pg.ap, func=AF.Silu),
                  reads=(pg,), writes=(ga,))
            h = self.hT[j]
            kb.op("dve", lambda e: e.tensor_tensor(out=h.ap, in0=ga.ap, in1=pu.ap, op=ALU.mult),
                  reads=(ga, pu), writes=(h,))
        self.down_ln(d0, NFC, 0.5 / DN_ALPHA, ln_idx)

    def resid_ln(self, banks, c, lp):
        kb = self.kb
        eps = LN_EPS / (DN_ALPHA * DN_ALPHA)
        for t in range(NT):
            wt, wap = self.wtmp[t]
            x = self.xtm[t]
            for dh in range(2):
                pt = banks[t * 2 + dh]
                sl = slice(dh * 512, (dh + 1) * 512)
                kb.op("dve", lambda e, pt=pt, sl=sl: e.scalar_tensor_tensor(
                    out=wap[:, sl], in0=pt.ap, scalar=float(c), in1=x.ap[:, sl],
                    op0=ALU.mult, op1=ALU.add), reads=(pt, x), writes=(wt,))
            stt = self.st[t]
            for dh in range(2):
                kb.op("dve", lambda e, dh=dh: e.bn_stats(out=stt.ap[:, dh, :],
                                                         in_=wap[:, dh * 512:(dh + 1) * 512]),
                      reads=(wt,), writes=(stt,))
            kb.op("dve", lambda e, t=t: e.bn_aggr(out=self.mv.ap[:, t, :], in_=stt.ap),
                  reads=(stt,), writes=(self.mv,))
        rs = self.rs
        kb.op("act", lambda e: e.activation(out=rs.ap[:, 0, :], in_=self.mv.ap[:, :, 1],
                                            func=AF.Sqrt, bias=float(eps), scale=1.0),
              reads=(self.mv,), writes=(rs,))
        kb.op("dve", lambda e: e.reciprocal(out=rs.ap[:, 1, :], in_=rs.ap[:, 0, :]),
              reads=(rs,), writes=(rs,))
        kb.op("dve", lambda e: e.scalar_tensor_tensor(out=rs.ap[:, 2, :], in0=self.mv.ap[:, :, 0],
                                                      scalar=-1.0, in1=rs.ap[:, 1, :],
                                                      op0=ALU.mult, op1=ALU.mult),
              reads=(self.mv, rs), writes=(rs,))
        for t in range(NT):
            wt, wap = self.wtmp[t]
            x = self.xtm[t]
            kb.op("act", lambda e, t=t, wap=wap: e.activation(out=wap, in_=wap, func=AF.Identity,
                                                              bias=rs.ap[:, 2, t:t + 1],
                                                              scale=rs.ap[:, 1, t:t + 1]),
                  reads=(wt, rs), writes=(wt,))
            kb.op("pool", lambda e, wap=wap: e.tensor_tensor(out=wap, in0=wap, in1=lp.ap[:, 0, :], op=ALU.mult),
                  reads=(wt, lp), writes=(wt,))
            kb.op("dve", lambda e, wap=wap, x=x: e.tensor_tensor(out=x.ap, in0=wap, in1=lp.ap[:, 1, :], op=ALU.add),
                  reads=(wt, lp), writes=(x,))
            self.to_xT(t)


    def setup_state(self):
        kb = self.kb
        for g in range(4):
            kb.op("pool", lambda e, g=g: e.memset(self.sst[g].ap, 0.0), writes=(self.sst[g],))
            kb.op("pool", lambda e, g=g: e.memset(self.sstb[g].ap, 0.0), writes=(self.sstb[g],))
        kb.op("pool", lambda e: e.memset(self.hist_full[:], 0.0), writes=tuple(self.hist))
        kb.op("act", lambda e: e.activation(out=self.aneg.ap, in_=self.bp.ap[:, BP_SSD_ALOG:BP_SSD_ALOG + 32],
                                            func=AF.Exp), reads=(self.bp,), writes=(self.aneg,))
        kb.op("dve", lambda e: e.tensor_scalar(out=self.aneg.ap, in0=self.aneg.ap, scalar1=-1.0, scalar2=None,
                                               op0=ALU.mult), reads=(self.aneg,), writes=(self.aneg,))

    def conv_silu(self, pt, ci, dst):
        kb = self.kb
        st = self.rot(self.cstage, "cstage")
        acc = self.rot(self.cacc, "cacc")
        hs = self.hist[ci]
        cw = self.cw
        kb.op("act", lambda e: e.activation(out=st.ap[:, 3:TS + 3], in_=pt.ap, func=AF.Copy),
              reads=(pt,), writes=(st,))
        kb.op("pool", lambda e: e.tensor_copy(out=st.ap[:, 0:3], in_=hs.ap), reads=(hs,), writes=(st,))
        kb.op("pool", lambda e: e.tensor_copy(out=hs.ap, in_=st.ap[:, TS:TS + 3]), reads=(st,), writes=(hs,))
        kb.op("dve", lambda e: e.tensor_scalar(out=acc.ap, in0=st.ap[:, 0:TS], scalar1=cw.ap[:, ci, 0:1],
                                               scalar2=None, op0=ALU.mult), reads=(st, cw), writes=(acc,))
        for k in range(1, 4):
            kb.op("dve", lambda e, k=k: e.scalar_tensor_tensor(
                out=acc.ap, in0=st.ap[:, k:k + TS], scalar=cw.ap[:, ci, k:k + 1], in1=acc.ap,
                op0=ALU.mult, op1=ALU.add), reads=(st, cw, acc), writes=(acc,))
        kb.op("act", lambda e: e.activation(out=dst.ap, in_=acc.ap, func=AF.Silu, bias=cw.ap[:, ci, 4:5],
                                            scale=1.0), reads=(acc, cw), writes=(dst,))

    def fm_proj(self, wsl, half, pt):
        kb = self.kb
        wv = wsl.ap.rearrange("p (k c) -> p k c", k=8)
        for kc in range(8):
            kb.op("pe", lambda e, kc=kc: e.matmul(pt.ap, lhsT=wv[:, kc, half * 128:(half + 1) * 128],
                                                  rhs=self.xT_full[:, kc, :], start=(kc == 0), stop=(kc == 7)),
                  reads=(wsl,) + tuple(self.xT), writes=(pt,))

    def tm_proj(self, wsl, t, pt, ncols=256):
        kb = self.kb
        wv = wsl.ap.rearrange("p (k c) -> p k c", k=8)
        for kc in range(8):
            kb.op("pe", lambda e, kc=kc: e.matmul(pt.ap[:, 0:ncols],
                                                  lhsT=self.xT_full[:, kc, t * 128:(t + 1) * 128],
                                                  rhs=wv[:, kc, 0:ncols], start=(kc == 0), stop=(kc == 7)),
                  reads=(wsl, self.xT[t]), writes=(pt,))

    def out_proj_ln(self, o0, nkc, ln_idx):
        self.down_ln(o0, nkc, 1.0 / DN_ALPHA, ln_idx)

    def ssd_mixer(self, sc, ln_idx):
        kb = self.kb
        w0, _ = self.windex["ssd_in"]
        o0, _ = self.windex["ssd_out"]
        wsl = None
        for cc in range(24):
            if cc % 2 == 0:
                wsl = self.wload(w0 + cc // 2)
            pt = self.psum(5)
            self.fm_proj(wsl, cc % 2, pt)
            self.conv_silu(pt, 12 + cc, self.fm[cc])
        for b in range(8):
            wsl = self.wload(w0 + 12 + b)
            for t in range(NT):
                pt = self.psum(5)
                self.tm_proj(wsl, t, pt)
                gt = self.gates[t]
                kb.op("act", lambda e, b=b, pt=pt, gt=gt: e.activation(
                    out=gt.ap[:, b * 256:(b + 1) * 256], in_=pt.ap[:, 0:256], func=AF.Silu),
                    reads=(pt,), writes=(gt,))
        wsl = self.wload(w0 + 20)
        for t in range(NT):
            pt = self.psum(5)
            self.tm_proj(wsl, t, pt, 32)
            sm = self.sm[t]
            kb.op("dve", lambda e, pt=pt, sm=sm: e.tensor_tensor(
                out=sm.ap[:, 0, :], in0=pt.ap[:, 0:32], in1=self.bp.ap[:, BP_SSD_DTB:BP_SSD_DTB + 32],
                op=ALU.add), reads=(pt, self.bp), writes=(sm,))
            kb.op("act", lambda e, sm=sm: e.activation(out=sm.ap[:, 1, :], in_=sm.ap[:, 0, :], func=AF.Exp),
                  reads=(sm,), writes=(sm,))
            kb.op("act", lambda e, sm=sm: e.activation(out=sm.ap[:, 2, :], in_=sm.ap[:, 1, :], func=AF.Ln,
                                                       bias=1.0, scale=1.0), reads=(sm,), writes=(sm,))
            kb.op("dve", lambda e, sm=sm: e.tensor_tensor(out=sm.ap[:, 3, :], in0=sm.ap[:, 2, :],
                                                          in1=self.aneg.ap, op=ALU.mult),
                  reads=(sm, self.aneg), writes=(sm,))
        for t in range(NT):
            self.ssd_chunk(t)
        self.out_proj_ln(o0, 16, ln_idx)

    def transposes_to(self, srcs, dst_tile, dst_ap, evac="dve", scale_ap=None):
        kb = self.kb
        p = self.psum(5)
        pb = p.ap.bitcast(BF16)
        n = len(srcs)
        for q, (tl, ap) in enumerate(srcs):
            kb.op("pe", lambda e, q=q, ap=ap: e.transpose(pb[:, q * 128:(q + 1) * 128], ap, self.identb.ap),
                  reads=(tl, self.identb), writes=(p,))
        if scale_ap is None:
            if evac == "act":
                kb.op("act", lambda e: e.activation(out=dst_ap, in_=pb[:, 0:n * 128], func=AF.Copy),
                      reads=(p,), writes=dst_tile)
            else:
                src = pb[:, 0:n * 128]
                if len(dst_ap.shape) == 3:
                    src = src.rearrange("p (k c) -> p k c", k=n)
                kb.op("dve", lambda e: e.tensor_copy(out=dst_ap, in_=src),
                      reads=(p,), writes=dst_tile)
        else:
            kb.op("dve", lambda e: e.tensor_tensor(out=dst_ap, in0=pb[:, 0:n * 128].rearrange("p (k c) -> p k c", k=n),
                                                   in1=scale_ap, op=ALU.mult),
                  reads=(p, self.colp), writes=dst_tile)

    def decay_block(self, row_ap, col_ap, mask_idx, n, dg, dg_t, tt, tt_t, col_t, nb=5):
        kb = self.kb
        cf = self.cf
        tt_w = tt_t if isinstance(tt_t, tuple) else (tt_t,)
        identf = cf.ap[:, C_ID, :]
        onesf = cf.ap[:, C_ONES, :]
        maskf = cf.ap[:, mask_idx if mask_idx is not None else 0, :]
        kb.op("pool", lambda e: e.tensor_tensor(out=dg[:, 0:n, :],
                                                in0=identf.unsqueeze(1).to_broadcast([128, n, 128]),
                                                in1=row_ap.unsqueeze(2).to_broadcast([128, n, 128]), op=ALU.mult),
              reads=(cf, col_t), writes=(dg_t,))
        for half in range((n + 3) // 4):
            pb = self.psum(nb)
            kb.op("pe", lambda e, half=half, pb=pb: e.matmul(
                pb.ap, lhsT=onesf, rhs=dg[:, half * 4:(half + 1) * 4, :], start=True, stop=True),
                reads=(cf, dg_t), writes=(pb,))
            kb.op("dve", lambda e, half=half, pb=pb: e.tensor_tensor(
                out=tt[:, half * 4:(half + 1) * 4, :], in0=pb.ap.rearrange("p (h c) -> p h c", h=4),
                in1=col_ap[:, half * 4:(half + 1) * 4].unsqueeze(2).to_broadcast([128, 4, 128]), op=ALU.subtract),
                reads=(pb, col_t), writes=tt_w)
        if mask_idx is None:
            kb.op("act", lambda e: e.activation(out=tt[:, 0:n, :], in_=tt[:, 0:n, :], func=AF.Relu, scale=-1.0),
                  reads=tt_w, writes=tt_w)
            kb.op("act", lambda e: e.activation(out=tt[:, 0:n, :], in_=tt[:, 0:n, :], func=AF.Exp, scale=-1.0),
                  reads=tt_w, writes=tt_w)
            return
        if mask_idx == C_MINC:
            for half in range((n + 3) // 4):
                kb.op("dve", lambda e, half=half: e.tensor_tensor(
                    out=tt[:, half * 4:(half + 1) * 4, :], in0=tt[:, half * 4:(half + 1) * 4, :],
                    in1=self.mask4.ap, op=ALU.add), reads=tt_w + (self.mask4,), writes=tt_w)
        else:
            kb.op("pool", lambda e: e.tensor_tensor(out=tt[:, 0:n, :], in0=tt[:, 0:n, :],
                                                    in1=maskf.unsqueeze(1).to_broadcast([128, n, 128]), op=ALU.add),
                  reads=tt_w + (cf,), writes=tt_w)
        kb.op("act", lambda e: e.activation(out=tt[:, 0:n, :], in_=tt[:, 0:n, :], func=AF.Exp),
              reads=tt_w, writes=tt_w)

    def ssd_chunk(self, t):
        kb = self.kb
        tc = slice(t * 128, (t + 1) * 128)
        cf, sv, sm = self.cf, self.sv, self.sm[t]
        for half in range(2):
            srcs = [(self.fm[half * 8 + q], self.fm[half * 8 + q].ap[:, tc]) for q in range(8)]
            self.transposes_to(srcs, (self.xs_tm,), self.xs_tm.ap[:, half * 1024:(half + 1) * 1024],
                               evac="act" if half else "dve")
        srcs = [(self.fm[16 + g], self.fm[16 + g].ap[:, tc]) for g in range(4)]
        self.transposes_to(srcs, (self.b_tm,), self.b_tm.ap, evac="dve")
        p = self.psum(5)
        kb.op("pe", lambda e: e.matmul(p.ap[:, 0:32], lhsT=cf.ap[:, C_TRI, :], rhs=sm.ap[:, 3, :],
                                       start=True, stop=True), reads=(cf, sm), writes=(p,))
        kb.op("pe", lambda e: e.matmul(p.ap[:, 32:64], lhsT=cf.ap[:, C_ONES, :], rhs=sm.ap[:, 3, :],
                                       start=True, stop=True), reads=(cf, sm), writes=(p,))
        kb.op("dve", lambda e: e.tensor_copy(out=sv.ap[:, 0, :], in_=p.ap[:, 0:32]), reads=(p,), writes=(sv,))
        kb.op("act", lambda e: e.activation(out=sv.ap[:, 1, :], in_=p.ap[:, 0:32], func=AF.Exp),
              reads=(p,), writes=(sv,))
        kb.op("dve", lambda e: e.tensor_tensor(out=sv.ap[:, 2, :], in0=p.ap[:, 32:64], in1=sv.ap[:, 0, :],
                                               op=ALU.subtract), reads=(p, sv), writes=(sv,))
        kb.op("act", lambda e: e.activation(out=sv.ap[:, 3, :], in_=sv.ap[:, 2, :], func=AF.Exp),
              reads=(sv,), writes=(sv,))
        kb.op("dve", lambda e: e.tensor_tensor(out=sv.ap[:, 4, :], in0=sv.ap[:, 3, :], in1=sm.ap[:, 2, :],
                                               op=ALU.mult), reads=(sv, sm), writes=(sv,))
        kb.op("act", lambda e: e.activation(out=sv.ap[:, 5, :], in_=p.ap[:, 32:64], func=AF.Exp),
              reads=(p,), writes=(sv,))
        xs3 = self.xs_tm.ap.rearrange("p (h c) -> p h c", h=32)

        def bc32(ap2d):
            return ap2d.unsqueeze(2).to_broadcast([128, 32, 64])

        kb.op("dve", lambda e: e.tensor_tensor(out=self.xdt.ap.rearrange("p (h c) -> p h c", h=32), in0=xs3,
                                               in1=bc32(sm.ap[:, 2, :]), op=ALU.mult),
              reads=(self.xs_tm, sm), writes=(self.xdt,))
        kb.op("pool", lambda e: e.tensor_tensor(out=self.xw.ap.rearrange("p (h c) -> p h c", h=32), in0=xs3,
                                                in1=bc32(sv.ap[:, 4, :]), op=ALU.mult),
              reads=(self.xs_tm, sv), writes=(self.xw,))
        kb.op("dve", lambda e: e.tensor_tensor(out=self.xD.ap.rearrange("p (h c) -> p h c", h=32), in0=xs3,
                                               in1=bc32(self.bp.ap[:, BP_SSD_D:BP_SSD_D + 32]), op=ALU.mult),
              reads=(self.xs_tm, self.bp), writes=(self.xD,))
        YD, QS, SU = self.ps[5], self.ps[6], self.ps[7]
        raw = self.raw
        dg8 = raw["ysq"][:, 0:1024].rearrange("p (h c) -> p h c", h=8)
        TT = [(self.ysq_h[1], raw["ysq"][:, 1024:2048].rearrange("p (h c) -> p h c", h=8)),
              (tuple(self.cacc), self.cacc_full[:].rearrange("p a c -> p (a c)").rearrange("p (h c) -> p h c", h=8))]
        hraw_bf = self.raw["hraw"][:].rearrange("p a c -> p (a c)")[:, 0:512].bitcast(BF16).rearrange("p (h c) -> p h c", h=8)
        ATB = [(self.m_AT8, self.m_AT8.ap), (self.hraw, hraw_bf)]
        for g in range(4):
            bt, ct = self.fm[16 + g], self.fm[20 + g]
            tt_tile, tt8 = TT[g % 2]
            tt_tiles = tt_tile if isinstance(tt_tile, tuple) else (tt_tile,)
            AT8t, AT8 = ATB[g % 2]
            pkq = self.psum(5)
            kb.op("pe", lambda e, bt=bt, ct=ct, pkq=pkq: e.matmul(pkq.ap[:, 0:128], lhsT=bt.ap[:, tc], rhs=ct.ap[:, tc],
                                                                 start=True, stop=True),
                  reads=(bt, ct), writes=(pkq,))
            kqs = self.kq_sb[g % 2]
            kb.op("dve", lambda e, pkq=pkq, kqs=kqs: e.tensor_tensor(out=kqs.ap, in0=pkq.ap[:, 0:128],
                                                                    in1=cf.ap[:, C_TRI, :], op=ALU.mult),
                  reads=(pkq, cf), writes=(kqs,))
            gcg = sv.ap[:, 0, g * 8:(g + 1) * 8]
            self.decay_block(gcg, gcg, None, 8, dg8, self.ysq_h[0], tt8, tt_tile, sv)
            kb.op("pool", lambda e, kqs=kqs, tt8=tt8, AT8=AT8: e.tensor_tensor(
                out=AT8, in0=tt8, in1=kqs.ap.unsqueeze(1).to_broadcast([128, 8, 128]), op=ALU.mult),
                reads=tt_tiles + (kqs,), writes=(AT8t,))
            for r in range(8):
                h = g * 8 + r
                kb.op("pe", lambda e, r=r, h=h, AT8=AT8: e.matmul(YD.ap[:, r * 64:(r + 1) * 64], lhsT=AT8[:, r, :],
                                                         rhs=self.xdt.ap[:, h * 64:(h + 1) * 64],
                                                         start=True, stop=False),
                      reads=(AT8t, self.xdt), writes=(YD,))
                kb.op("pe", lambda e, r=r, h=h: e.matmul(YD.ap[:, r * 64:(r + 1) * 64], lhsT=self.identb.ap,
                                                         rhs=self.xD.ap[:, h * 64:(h + 1) * 64],
                                                         start=False, stop=True),
                      reads=(self.identb, self.xD), writes=(YD,))
            sb_, s_ = self.sstb[g], self.sst[g]
            kb.op("pe", lambda e, ct=ct, sb_=sb_: e.matmul(QS.ap, lhsT=ct.ap[:, tc], rhs=sb_.ap, start=True, stop=True),
                  reads=(ct, sb_), writes=(QS,))
            kb.op("pe", lambda e, g=g: e.matmul(SU.ap, lhsT=self.b_tm.ap[:, g * 128:(g + 1) * 128],
                                                rhs=self.xw.ap[:, g * 512:(g + 1) * 512], start=True, stop=True),
                  reads=(self.b_tm, self.xw), writes=(SU,))

            def bc8(ap2d):
                return ap2d.unsqueeze(2).to_broadcast([128, 8, 64])

            yt = self.ytmp
            kb.op("dve", lambda e, g=g: e.tensor_tensor(out=yt.ap.rearrange("p (h c) -> p h c", h=8),
                                                        in0=QS.ap.rearrange("p (h c) -> p h c", h=8),
                                                        in1=bc8(sv.ap[:, 1, g * 8:(g + 1) * 8]), op=ALU.mult),
                  reads=(QS, sv), writes=(yt,))
            kb.op("dve", lambda e, g=g: e.tensor_tensor(out=self.ybuf.ap[:, g * 512:(g + 1) * 512], in0=yt.ap,
                                                        in1=YD.ap, op=ALU.add),
                  reads=(yt, YD), writes=(self.ybuf,))
            kb.op("dve", lambda e, g=g, s_=s_: e.tensor_tensor(out=s_.ap.rearrange("p (h c) -> p h c", h=8),
                                                               in0=s_.ap.rearrange("p (h c) -> p h c", h=8),
                                                               in1=bc8(sv.ap[:, 5, g * 8:(g + 1) * 8]), op=ALU.mult),
                  reads=(s_, sv), writes=(s_,))
            kb.op("dve", lambda e, s_=s_: e.tensor_tensor(out=s_.ap, in0=s_.ap, in1=SU.ap, op=ALU.add),
                  reads=(s_, SU), writes=(s_,))
            kb.op("act", lambda e, s_=s_, sb_=sb_: e.activation(out=sb_.ap, in_=s_.ap, func=AF.Copy),
                  reads=(s_,), writes=(sb_,))
        yb, gt, ss = self.ybuf, self.gates[t], self.ss
        kb.op("dve", lambda e: e.tensor_tensor(out=yb.ap, in0=yb.ap, in1=gt.ap, op=ALU.mult),
              reads=(yb, gt), writes=(yb,))
        kb.op("act", lambda e: e.activation(out=self.ysq_full[:], in_=yb.ap, func=AF.Square),
              reads=(yb,), writes=tuple(self.ysq_h))
        kb.op("dve", lambda e: e.reduce_sum(out=ss.ap[:, 0, :], in_=self.ysq_full[:].rearrange("p (g c) -> p g c", g=4),
                                            axis=AX.X), reads=tuple(self.ysq_h), writes=(ss,))
        kb.op("act", lambda e: e.activation(out=ss.ap[:, 1, :], in_=ss.ap[:, 0, :], func=AF.Sqrt,
                                            bias=float(RMS_EPS), scale=1.0 / 512.0), reads=(ss,), writes=(ss,))
        kb.op("dve", lambda e: e.reciprocal(out=ss.ap[:, 2, :], in_=ss.ap[:, 1, :]), reads=(ss,), writes=(ss,))
        for g in range(4):
            kb.op("dve", lambda e, g=g: e.tensor_scalar(out=self.ybf.ap[:, g * 512:(g + 1) * 512],
                                                        in0=yb.ap[:, g * 512:(g + 1) * 512],
                                                        scalar1=ss.ap[:, 2, g:g + 1], scalar2=None, op0=ALU.mult),
                  reads=(yb, ss), writes=(self.ybf,))
        for half in range(2):
            srcs = [(self.ybf, self.ybf.ap[:, (half * 8 + q) * 128:(half * 8 + q + 1) * 128]) for q in range(8)]
            dst_tiles = tuple(self.hT[half * 8 + q] for q in range(8))
            dst_ap = self.hT_full[:, half * 8:(half + 1) * 8, tc]
            sc_ap = self.colp.ap[:, half * 8:(half + 1) * 8].unsqueeze(2).to_broadcast([128, 8, 128])
            self.transposes_to(srcs, dst_tiles, dst_ap, scale_ap=sc_ap)

    def setup_hyb(self):
        kb = self.kb
        for tl in (self.sg, self.sgb, self.cn, self.cnb):
            kb.op("pool", lambda e, tl=tl: e.memset(tl.ap, 0.0), writes=(tl,))
        kb.op("pool", lambda e: e.memset(self.vext.ap, 1.0), writes=(self.vext,))
        hm = self.hmul
        kb.op("pool", lambda e: e.memset(hm.ap, -1.0), writes=(hm,))
        kb.op("pool", lambda e: e.memset(hm.ap[:, 16:20], 0.0), writes=(hm,))
        kb.op("act", lambda e: e.activation(out=hm.ap[:, 8:16], in_=self.bp.ap[:, BP_G_ALOG:BP_G_ALOG + 8],
                                            func=AF.Exp), reads=(self.bp, hm), writes=(hm,))
        kb.op("dve", lambda e: e.tensor_scalar(out=hm.ap[:, 8:16], in0=hm.ap[:, 8:16], scalar1=-1.0, scalar2=None,
                                               op0=ALU.mult), reads=(hm,), writes=(hm,))

    def hyb_mixer(self, sc, ln_idx):
        kb = self.kb
        w0, _ = self.windex["hyb_in"]
        o0, _ = self.windex["hyb_out"]
        wsl = None
        for cc in range(16):
            if cc % 2 == 0:
                wsl = self.wload(w0 + cc // 2)
            pt = self.psum()
            self.fm_proj(wsl, cc % 2, pt)
            if cc < 12:
                self.conv_silu(pt, cc, self.fm[cc])
            else:
                dst = self.fm[cc]
                scl = 0.125 if cc >= 14 else 1.0
                kb.op("act", lambda e, pt=pt, dst=dst, scl=scl: e.activation(out=dst.ap, in_=pt.ap, func=AF.Copy,
                                                                             scale=scl),
                      reads=(pt,), writes=(dst,))
        for b in range(7):
            wsl = self.wload(w0 + 8 + b)
            for t in range(NT):
                pt = self.psum()
                self.tm_proj(wsl, t, pt)
                gt = self.gates[t]
                if b < 2:
                    fn, scl = AF.Silu, 1.0
                elif b < 4:
                    fn, scl = AF.Sigmoid, 1.0
                elif b < 6:
                    fn, scl = AF.Copy, 1.0
                else:
                    fn, scl = AF.Copy, 0.125
                kb.op("act", lambda e, b=b, pt=pt, gt=gt, fn=fn, scl=scl: e.activation(
                    out=gt.ap[:, b * 256:(b + 1) * 256], in_=pt.ap[:, 0:256], func=fn, scale=scl),
                    reads=(pt,), writes=(gt,))
        wsl = self.wload(w0 + 15)
        bp = self.bp
        for t in range(NT):
            pt = self.psum()
            self.tm_proj(wsl, t, pt, 32)
            sm = self.sm[t]
            kb.op("dve", lambda e, pt=pt, sm=sm: e.tensor_tensor(
                out=sm.ap[:, 0, 0:24], in0=pt.ap[:, 0:24], in1=bp.ap[:, BP_H_BIAS:BP_H_BIAS + 24], op=ALU.add),
                reads=(pt, bp), writes=(sm,))
            kb.op("dve", lambda e, sm=sm: e.tensor_tensor(
                out=sm.ap[:, 0, 0:24], in0=sm.ap[:, 0, 0:24], in1=bp.ap[:, BP_H_SGN:BP_H_SGN + 24], op=ALU.mult),
                reads=(sm, bp), writes=(sm,))
            kb.op("act", lambda e, sm=sm: e.activation(out=sm.ap[:, 1, 0:24], in_=sm.ap[:, 0, 0:24], func=AF.Exp),
                  reads=(sm,), writes=(sm,))
            kb.op("act", lambda e, sm=sm: e.activation(out=sm.ap[:, 2, 0:24], in_=sm.ap[:, 1, 0:24], func=AF.Ln,
                                                       bias=1.0, scale=1.0), reads=(sm,), writes=(sm,))
            kb.op("dve", lambda e, sm=sm: e.tensor_tensor(out=sm.ap[:, 3, 0:24], in0=sm.ap[:, 2, 0:24],
                                                          in1=self.hmul.ap, op=ALU.mult),
                  reads=(sm, self.hmul), writes=(sm,))
            kb.op("dve", lambda e, sm=sm: e.tensor_copy(out=sm.ap[:, 3, 16:20], in_=sm.ap[:, 0, 16:20]),
                  reads=(sm,), writes=(sm,))
        for t in range(NT):
            self.hyb_chunk(t)
        self.out_proj_ln(o0, 8, ln_idx)

    def hyb_chunk(self, t):
        kb = self.kb
        tc = slice(t * 128, (t + 1) * 128)
        cf, hv, hs, sm, gt = self.cf, self.hv, self.hs, self.sm[t], self.gates[t]
        raw = self.raw
        A_xs, A_xdt, A_xw, A_xD, A_yb = (self.xs_tm, self.xdt, self.xw, self.xD, self.ybuf)

        def v8(h):
            return h[:].bitcast(F32).rearrange("p (h c) -> p h c", h=8)

        Y = [(A_xs, v8(raw["xs_tm"])), (A_xdt, v8(raw["xdt"]))]
        X = [(A_xw, v8(raw["xw"])), (A_xD, v8(raw["xD"]))]
        P_t, P = A_yb, raw["ybuf"][:, 0:1024].rearrange("p (h c) -> p h c", h=8)
        dg_t, dg = A_yb, raw["ybuf"][:, 1024:2048].rearrange("p (h c) -> p h c", h=8)
        tt_t, tt = self.ysq_h[0], raw["ysq"][:, 0:1024].rearrange("p (h c) -> p h c", h=8)
        at_t, AT = self.ysq_h[1], raw["ysq"][:, 1024:1536].bitcast(BF16).rearrange("p (h c) -> p h c", h=8)
        identf = cf.ap[:, C_ID, :]
        onesf = cf.ap[:, C_ONES, :]

        def bcl(ap2d, n, c):
            return ap2d.unsqueeze(2).to_broadcast([128, n, c])

        def bcm(ap2d, n, c):
            return ap2d.unsqueeze(1).to_broadcast([128, n, c])

        srcs = [(self.fm[q], self.fm[q].ap[:, tc]) for q in range(8)]
        self.transposes_to(srcs, (self.qk_tm,), self.qk_tm.ap, evac="dve")
        srcs = [(self.fm[8 + q], self.fm[8 + q].ap[:, tc]) for q in range(4)]
        self.transposes_to(srcs, (self.v_tm,), self.v_tm.ap, evac="act")
        sqv = raw["ybuf"][:, 1024:2048]
        kb.op("act", lambda e: e.activation(out=sqv, in_=self.qk_tm.ap, func=AF.Square),
              reads=(self.qk_tm,), writes=(dg_t,))
        kb.op("dve", lambda e: e.reduce_sum(out=hs.ap[:, 0, :], in_=sqv.rearrange("p (h c) -> p h c", h=16),
                                            axis=AX.X), reads=(dg_t,), writes=(hs,))
        kb.op("act", lambda e: e.activation(out=hs.ap[:, 1, :], in_=hs.ap[:, 0, :], func=AF.Ln, bias=1e-6,
                                            scale=1.0), reads=(hs,), writes=(hs,))
        p = self.psum()
        kb.op("pe", lambda e: e.matmul(p.ap[:, 0:24], lhsT=cf.ap[:, C_TRI, :], rhs=sm.ap[:, 3, 0:24],
                                       start=True, stop=True), reads=(cf, sm), writes=(p,))
        kb.op("pe", lambda e: e.matmul(p.ap[:, 32:56], lhsT=onesf, rhs=sm.ap[:, 3, 0:24],
                                       start=True, stop=True), reads=(cf, sm), writes=(p,))
        H = lambda i, n=8: hv.ap[:, i, 0:n]

        def dv(fn, reads, writes=(hv,)):
            kb.op("dve", fn, reads=reads, writes=writes)

        def ac(fn, reads, writes=(hv,)):
            kb.op("act", fn, reads=reads, writes=writes)

        dv(lambda e: e.tensor_copy(out=H(0), in_=p.ap[:, 8:16]), (p,))
        dv(lambda e: e.tensor_scalar(out=H(1), in0=hs.ap[:, 1, 8:16], scalar1=-0.5, scalar2=None,
                                     op0=ALU.mult), (hs,))
        dv(lambda e: e.tensor_scalar(out=H(2), in0=hs.ap[:, 1, 0:8], scalar1=-0.5, scalar2=-math.log(8.0),
                                     op0=ALU.mult, op1=ALU.add), (hs,))
        dv(lambda e: e.tensor_tensor(out=H(2), in0=H(2), in1=H(0), op=ALU.add), (hv,))
        dv(lambda e: e.tensor_tensor(out=H(3), in0=H(0), in1=H(1), op=ALU.subtract), (hv,))
        dv(lambda e: e.tensor_tensor(out=H(4), in0=H(0), in1=H(1), op=ALU.add), (hv,))
        dv(lambda e: e.tensor_tensor(out=H(4), in0=H(4), in1=sm.ap[:, 3, 0:8], op=ALU.add), (hv, sm))
        ac(lambda e: e.activation(out=H(5), in_=H(2), func=AF.Exp), (hv,))
        dv(lambda e: e.tensor_tensor(out=H(6), in0=p.ap[:, 40:48], in1=H(3), op=ALU.subtract), (p, hv))
        ac(lambda e: e.activation(out=H(6), in_=H(6), func=AF.Exp), (hv,))
        ac(lambda e: e.activation(out=H(7), in_=H(4), func=AF.Exp), (hv,))
        ac(lambda e: e.activation(out=H(8), in_=sm.ap[:, 3, 0:8], func=AF.Exp), (sm,))
        ac(lambda e: e.activation(out=H(9), in_=p.ap[:, 40:48], func=AF.Exp), (p,))
        dv(lambda e: e.tensor_copy(out=H(10, 4), in_=p.ap[:, 20:24]), (p,))
        dv(lambda e: e.tensor_tensor(out=H(11, 4), in0=H(10, 4), in1=sm.ap[:, 3, 16:20], op=ALU.subtract),
           (hv, sm))
        ac(lambda e: e.activation(out=H(12, 4), in_=H(10, 4), func=AF.Exp), (hv,))
        dv(lambda e: e.tensor_tensor(out=H(13, 4), in0=p.ap[:, 52:56], in1=H(11, 4), op=ALU.subtract), (p, hv))
        ac(lambda e: e.activation(out=H(13, 4), in_=H(13, 4), func=AF.Exp), (hv,))
        ac(lambda e: e.activation(out=H(14, 4), in_=p.ap[:, 52:56], func=AF.Exp), (p,))

        def kT(h):
            return self.fm[4 + h // 2], self.fm[4 + h // 2].ap[(h % 2) * 64:(h % 2) * 64 + 64, tc]

        def qT(h):
            return self.fm[h // 2], self.fm[h // 2].ap[(h % 2) * 64:(h % 2) * 64 + 64, tc]

        def decay(row_ap, col_ap, mask_idx, n):
            self.decay_block(row_ap, col_ap, mask_idx, n, dg, dg_t, tt, tt_t, hv, nb=8)

        pkk = [self.psum(), self.psum()]
        pkq = [self.psum(), self.psum()]
        for h in range(8):
            kt, kap = kT(h)
            qt, qap = qT(h)
            cs = slice((h // 2) * 128, (h // 2) * 128 + 128)
            kb.op("pe", lambda e, h=h, kap=kap, cs=cs: e.matmul(pkk[h % 2].ap[:, cs], lhsT=kap, rhs=kap,
                                                               start=True, stop=True),
                  reads=(kt,), writes=(pkk[h % 2],))
            kb.op("pe", lambda e, h=h, kap=kap, qap=qap, cs=cs: e.matmul(pkq[h % 2].ap[:, cs], lhsT=kap, rhs=qap,
                                                                        start=True, stop=True),
                  reads=(kt, qt), writes=(pkq[h % 2],))
        decay(H(4), H(3), C_MSTR, 8)
        Y0t, Y0 = Y[0]
        for par in range(2):
            kb.op("dve", lambda e, par=par: e.scalar_tensor_tensor(
                out=Y0[:, par:8:2, :], in0=tt[:, par:8:2, :], scalar=-1.0,
                in1=pkk[par].ap.rearrange("p (h c) -> p h c", h=4), op0=ALU.mult, op1=ALU.mult),
                reads=(tt_t, pkk[par]), writes=(Y0t,))
        decay(H(2), H(3), C_MINC, 8)
        for par in range(2):
            kb.op("dve", lambda e, par=par: e.tensor_tensor(
                out=AT[:, par:8:2, :], in0=tt[:, par:8:2, :],
                in1=pkq[par].ap.rearrange("p (h c) -> p h c", h=4), op=ALU.mult),
                reads=(tt_t, pkq[par]), writes=(at_t,))
        X0t, X0 = X[0]
        px = [self.psum(), self.psum()]
        for h in range(8):
            cs = slice((h % 4) * 128, (h % 4) * 128 + 128)
            kb.op("pe", lambda e, h=h, cs=cs: e.transpose(px[h // 4].ap[:, cs], Y0[:, h, :], identf),
                  reads=(Y0t, cf), writes=(px[h // 4],))
        kb.op("act", lambda e: e.activation(out=X0[:, 0:4, :], in_=px[0].ap.rearrange("p (h c) -> p h c", h=4),
                                            func=AF.Copy), reads=(px[0],), writes=(X0t,))
        kb.op("dve", lambda e: e.tensor_copy(out=X0[:, 4:8, :], in_=px[1].ap.rearrange("p (h c) -> p h c", h=4)),
              reads=(px[1],), writes=(X0t,))
        kb.op("pool", lambda e: e.tensor_tensor(out=P, in0=Y0, in1=bcm(identf, 8, 128), op=ALU.add),
              reads=(Y0t, cf), writes=(P_t,))
        dg_m = self.cacc[0].ap.rearrange("p (h c) -> p h c", h=4)
        tt_m = self.cacc[1].ap.rearrange("p (h c) -> p h c", h=4)
        mstate = {}

        def mlstm_part1():
            def mqT(m):
                return self.fm[12 + m // 2], self.fm[12 + m // 2].ap[(m % 2) * 64:(m % 2) * 64 + 64, tc]

            def mkT(m):
                return self.fm[14 + m // 2], self.fm[14 + m // 2].ap[(m % 2) * 64:(m % 2) * 64 + 64, tc]

            vext = self.vext
            kb.op("pool", lambda e: e.tensor_copy(out=vext.ap[:, :, 0:128],
                                                  in_=gt.ap[:, 1024:1536].rearrange("p (h c) -> p h c", h=4)),
                  reads=(gt,), writes=(vext,))
            kb.op("pool", lambda e: e.tensor_tensor(out=self.kwm.ap.rearrange("p (h c) -> p h c", h=4),
                                                    in0=gt.ap[:, 1536:1792].rearrange("p (h c) -> p h c", h=4),
                                                    in1=bcl(H(13, 4), 4, 64), op=ALU.mult),
                  reads=(gt, hv), writes=(self.kwm,))
            pmk = [self.psum(), self.psum()]
            for m in range(4):
                kt, kap = mkT(m)
                qt, qap = mqT(m)
                kb.op("pe", lambda e, m=m, kap=kap, qap=qap: e.matmul(pmk[m % 2].ap[:, (m // 2) * 128:(m // 2) * 128 + 128],
                                                                     lhsT=kap, rhs=qap, start=True, stop=True),
                      reads=(kt, qt), writes=(pmk[m % 2],))
            self.decay_block(H(10, 4), H(11, 4), C_MINC, 4, dg_m, self.cacc[0], tt_m, self.cacc[1], hv, nb=8)
            for par in range(2):
                kb.op("dve", lambda e, par=par: e.tensor_tensor(
                    out=self.atm.ap[:, par:4:2, :], in0=tt_m[:, par:4:2, :],
                    in1=pmk[par].ap[:, 0:256].rearrange("p (h c) -> p h c", h=2), op=ALU.mult),
                    reads=(self.cacc[1], pmk[par]), writes=(self.atm,))
            mstate["vext"] = vext
            mstate["mqT"] = mqT

        def mlstm_part2():
            vext = mstate["vext"]
            mqT = mstate["mqT"]
            pn = [self.psum(), self.psum()]
            pq = [self.psum(), self.psum()]
            for m in range(4):
                r0 = (m % 2) * 64
                cs = slice((m % 2) * 256, (m % 2) * 256 + 129)
                cq = slice((m // 2) * 256, (m // 2) * 256 + 129)
                qt, qap = mqT(m)
                kb.op("pe", lambda e, m=m, cs=cs: e.matmul(pn[m // 2].ap[:, cs], lhsT=self.atm.ap[:, m, :],
                                                           rhs=vext.ap[:, m, :], start=True, stop=True),
                      reads=(self.atm, vext), writes=(pn[m // 2],))
                kb.op("pe", lambda e, m=m, cq=cq, r0=r0, qap=qap: e.matmul(pq[m % 2].ap[:, cq], lhsT=qap,
                                                                           rhs=self.cnb.ap[r0:r0 + 64, m // 2, :],
                                                                           start=True, stop=True),
                      reads=(qt, self.cnb), writes=(pq[m % 2],))
            hr = self.hraw
            for par in range(2):
                kb.op("dve", lambda e, par=par: e.tensor_tensor(
                    out=hr.ap[:, par:4:2, :], in0=pq[par].ap.rearrange("p (h c) -> p h c", h=2)[:, :, 0:129],
                    in1=bcl(hv.ap[:, 12, par:4:2], 2, 129), op=ALU.mult), reads=(pq[par], hv), writes=(hr,))
            for b in range(2):
                kb.op("dve", lambda e, b=b: e.tensor_tensor(
                    out=hr.ap[:, 2 * b:2 * b + 2, :], in0=hr.ap[:, 2 * b:2 * b + 2, :],
                    in1=pn[b].ap.rearrange("p (h c) -> p h c", h=2)[:, :, 0:129], op=ALU.add),
                    reads=(hr, pn[b]), writes=(hr,))
            kb.op("act", lambda e: e.activation(out=hs.ap[:, 3, 8:12], in_=hr.ap[:, :, 128], func=AF.Abs),
                  reads=(hr,), writes=(hs,))
            kb.op("dve", lambda e: e.tensor_scalar(out=hs.ap[:, 3, 8:12], in0=hs.ap[:, 3, 8:12], scalar1=1.0,
                                                   scalar2=None, op0=ALU.max), reads=(hs,), writes=(hs,))
            kb.op("dve", lambda e: e.reciprocal(out=hs.ap[:, 3, 12:16], in_=hs.ap[:, 3, 8:12]), reads=(hs,), writes=(hs,))
            kb.op("dve", lambda e: e.tensor_tensor(out=hr.ap[:, :, 0:128], in0=hr.ap[:, :, 0:128],
                                                   in1=bcl(hs.ap[:, 3, 12:16], 4, 128), op=ALU.mult),
                  reads=(hr, hs), writes=(hr,))
            kb.op("dve", lambda e: e.tensor_tensor(out=self.ybf.ap[:, 512:1024].rearrange("p (h c) -> p h c", h=4),
                                                   in0=hr.ap[:, :, 0:128],
                                                   in1=gt.ap[:, 512:1024].rearrange("p (h c) -> p h c", h=4),
                                                   op=ALU.mult), reads=(hr, gt), writes=(self.ybf,))

        def mlstm_part3():
            vext = mstate["vext"]
            psm = [self.psum(), self.psum()]
            for m in range(4):
                cs = slice((m % 2) * 256, (m % 2) * 256 + 129)
                kb.op("pe", lambda e, m=m, cs=cs: e.matmul(psm[m // 2].ap[:, cs],
                                                           lhsT=self.kwm.ap[:, (m // 2) * 128:(m // 2) * 128 + 128],
                                                           rhs=vext.ap[:, m, :], start=True, stop=True),
                      reads=(self.kwm, vext), writes=(psm[m // 2],))
            for m in range(4):
                r0 = (m % 2) * 64
                cs = slice((m % 2) * 256, (m % 2) * 256 + 129)
                kb.op("dve", lambda e, m=m, r0=r0, cs=cs: e.scalar_tensor_tensor(
                    out=self.cn.ap[r0:r0 + 64, m // 2, :], in0=self.cn.ap[r0:r0 + 64, m // 2, :],
                    scalar=hv.ap[r0:r0 + 64, 14, m:m + 1], in1=psm[m // 2].ap[r0:r0 + 64, cs],
                    op0=ALU.mult, op1=ALU.add), reads=(self.cn, hv, psm[m // 2]), writes=(self.cn,))
            kb.op("act", lambda e: e.activation(out=self.cnb.ap, in_=self.cn.ap, func=AF.Copy),
                  reads=(self.cn,), writes=(self.cnb,))

        for k in range(1, 7):
            if k == 2:
                mlstm_part1()
            elif k == 4:
                mlstm_part2()
            elif k == 6:
                mlstm_part3()
            (Ypt, Yp), (Xpt, Xp) = Y[(k - 1) % 2], X[(k - 1) % 2]
            (Ynt, Yn), (Xnt, Xn) = Y[k % 2], X[k % 2]
            px = [self.psum(), self.psum()]
            for h in range(8):
                cs = slice((h % 4) * 128, (h % 4) * 128 + 128)
                kb.op("pe", lambda e, h=h, cs=cs, Yp=Yp, Xp=Xp, px=px: e.matmul(
                    px[h // 4].ap[:, cs], lhsT=Yp[:, h, :], rhs=Xp[:, h, :], start=True, stop=True),
                    reads=(Ypt, Xpt), writes=(px[h // 4],))
            kb.op("act", lambda e, Xn=Xn, px=px: e.activation(out=Xn[:, 0:4, :],
                                                              in_=px[0].ap.rearrange("p (h c) -> p h c", h=4),
                                                              func=AF.Copy), reads=(px[0],), writes=(Xnt,))
            kb.op("dve", lambda e, Xn=Xn, px=px: e.tensor_copy(out=Xn[:, 4:8, :],
                                                               in_=px[1].ap.rearrange("p (h c) -> p h c", h=4)),
                  reads=(px[1],), writes=(Xnt,))
            if k < 6:
                py = [self.psum(), self.psum()]
                for h in range(8):
                    cs = slice((h % 4) * 128, (h % 4) * 128 + 128)
                    kb.op("pe", lambda e, h=h, cs=cs, Yp=Yp, Xp=Xp, py=py: e.matmul(
                        py[h // 4].ap[:, cs], lhsT=Xp[:, h, :], rhs=Yp[:, h, :], start=True, stop=True),
                        reads=(Ypt, Xpt), writes=(py[h // 4],))
                kb.op("act", lambda e, Yn=Yn, py=py: e.activation(out=Yn[:, 0:4, :],
                                                                  in_=py[0].ap.rearrange("p (h c) -> p h c", h=4),
                                                                  func=AF.Copy), reads=(py[0],), writes=(Ynt,))
                kb.op("dve", lambda e, Yn=Yn, py=py: e.tensor_copy(out=Yn[:, 4:8, :],
                                                                   in_=py[1].ap.rearrange("p (h c) -> p h c", h=4)),
                      reads=(py[1],), writes=(Ynt,))
            pp = [self.psum(), self.psum()]
            for h in range(8):
                cs = slice((h % 4) * 128, (h % 4) * 128 + 128)
                kb.op("pe", lambda e, h=h, cs=cs, Xn=Xn, pp=pp: e.matmul(
                    pp[h // 4].ap[:, cs], lhsT=Xn[:, h, :], rhs=P[:, h, :], start=True, stop=True),
                    reads=(Xnt, P_t), writes=(pp[h // 4],))
            for half in range(2):
                kb.op("dve", lambda e, half=half, pp=pp: e.tensor_tensor(
                    out=P[:, half * 4:(half + 1) * 4, :], in0=P[:, half * 4:(half + 1) * 4, :],
                    in1=pp[half].ap.rearrange("p (h c) -> p h c", h=4), op=ALU.add),
                    reads=(P_t, pp[half]), writes=(P_t,))
        k_tm = self.qk_tm.ap[:, 512:1024]
        kb.op("dve", lambda e: e.tensor_tensor(out=self.bv.ap.rearrange("p (h c) -> p h c", h=8),
                                               in0=self.v_tm.ap.rearrange("p (h c) -> p h c", h=8),
                                               in1=bcl(H(8), 8, 64), op=ALU.mult),
              reads=(self.v_tm, hv), writes=(self.bv,))
        kb.op("dve", lambda e: e.tensor_tensor(out=self.bk.ap.rearrange("p (h c) -> p h c", h=8),
                                               in0=k_tm.rearrange("p (h c) -> p h c", h=8),
                                               in1=bcl(H(7), 8, 64), op=ALU.mult),
              reads=(self.qk_tm, hv), writes=(self.bk,))
        kb.op("pool", lambda e: e.tensor_tensor(out=self.kw.ap.rearrange("p (h c) -> p h c", h=8),
                                                in0=k_tm.rearrange("p (h c) -> p h c", h=8),
                                                in1=bcl(H(6), 8, 64), op=ALU.mult),
              reads=(self.qk_tm, hv), writes=(self.kw,))
        pu0 = self.psum()
        pwt = [self.psum(), self.psum()]
        for h in range(8):
            cs = slice((h % 4) * 128, (h % 4) * 128 + 128)
            kb.op("pe", lambda e, h=h: e.matmul(pu0.ap[:, h * 64:(h + 1) * 64], lhsT=P[:, h, :],
                                                rhs=self.bv.ap[:, h * 64:(h + 1) * 64], start=True, stop=True),
                  reads=(P_t, self.bv), writes=(pu0,))
            kb.op("pe", lambda e, h=h, cs=cs: e.matmul(pwt[h // 4].ap[:, cs],
                                                       lhsT=self.bk.ap[:, (h // 2) * 128:(h // 2) * 128 + 128],
                                                       rhs=P[:, h, :], start=True, stop=True),
                  reads=(P_t, self.bk), writes=(pwt[h // 4],))
        kb.op("act", lambda e: e.activation(out=self.u0.ap, in_=pu0.ap, func=AF.Copy), reads=(pu0,), writes=(self.u0,))
        for h in range(8):
            r0 = (h % 2) * 64
            cs = slice((h % 4) * 128, (h % 4) * 128 + 128)
            eng = "dve" if h % 2 == 0 else "act"
            if eng == "dve":
                kb.op("dve", lambda e, h=h, r0=r0, cs=cs: e.tensor_copy(out=self.wtb.ap[r0:r0 + 64, h, :],
                                                                        in_=pwt[h // 4].ap[r0:r0 + 64, cs]),
                      reads=(pwt[h // 4],), writes=(self.wtb,))
            else:
                kb.op("act", lambda e, h=h, r0=r0, cs=cs: e.activation(out=self.wtb.ap[r0:r0 + 64, h, :],
                                                                       in_=pwt[h // 4].ap[r0:r0 + 64, cs],
                                                                       func=AF.Copy),
                      reads=(pwt[h // 4],), writes=(self.wtb,))
        pws = [self.psum(), self.psum()]
        for h in range(8):
            r0 = (h % 2) * 64
            kb.op("pe", lambda e, h=h, r0=r0: e.matmul(pws[h % 2].ap[:, (h // 2) * 64:(h // 2) * 64 + 64],
                                                       lhsT=self.wtb.ap[r0:r0 + 64, h, :],
                                                       rhs=self.sgb.ap[r0:r0 + 64, h // 2, :], start=True, stop=True),
                  reads=(self.wtb, self.sgb), writes=(pws[h % 2],))
        u3 = self.u.ap.rearrange("p (h c) -> p h c", h=8)
        u03 = self.u0.ap.rearrange("p (h c) -> p h c", h=8)
        for par in range(2):
            kb.op("dve", lambda e, par=par: e.tensor_tensor(
                out=u3[:, par:8:2, :], in0=u03[:, par:8:2, :],
                in1=pws[par].ap[:, 0:256].rearrange("p (h c) -> p h c", h=4), op=ALU.subtract),
                reads=(self.u0, pws[par]), writes=(self.u,))
        po = self.psum()
        pqs = [self.psum(), self.psum()]
        for h in range(8):
            r0 = (h % 2) * 64
            qt, qap = qT(h)
            kb.op("pe", lambda e, h=h: e.matmul(po.ap[:, h * 64:(h + 1) * 64], lhsT=AT[:, h, :],
                                                rhs=self.u.ap[:, h * 64:(h + 1) * 64], start=True, stop=True),
                  reads=(at_t, self.u), writes=(po,))
            kb.op("pe", lambda e, h=h, r0=r0, qap=qap: e.matmul(pqs[h % 2].ap[:, (h // 2) * 64:(h // 2) * 64 + 64],
                                                                lhsT=qap, rhs=self.sgb.ap[r0:r0 + 64, h // 2, :],
                                                                start=True, stop=True),
                  reads=(qt, self.sgb), writes=(pqs[h % 2],))
        ob = self.ytmp
        ob3 = ob.ap.rearrange("p (h c) -> p h c", h=8)
        for par in range(2):
            kb.op("dve", lambda e, par=par: e.tensor_tensor(
                out=ob3[:, par:8:2, :], in0=pqs[par].ap[:, 0:256].rearrange("p (h c) -> p h c", h=4),
                in1=bcl(hv.ap[:, 5, par:8:2], 4, 64), op=ALU.mult),
                reads=(pqs[par], hv), writes=(ob,))
        kb.op("dve", lambda e: e.tensor_tensor(out=ob.ap, in0=ob.ap, in1=po.ap, op=ALU.add),
              reads=(ob, po), writes=(ob,))
        psu = self.psum()
        for h in range(8):
            kb.op("pe", lambda e, h=h: e.matmul(psu.ap[:, h * 64:(h + 1) * 64],
                                                lhsT=self.kw.ap[:, (h // 2) * 128:(h // 2) * 128 + 128],
                                                rhs=self.u.ap[:, h * 64:(h + 1) * 64], start=True, stop=True),
                  reads=(self.kw, self.u), writes=(psu,))
        for h in range(8):
            r0 = (h % 2) * 64
            kb.op("dve", lambda e, h=h, r0=r0: e.scalar_tensor_tensor(
                out=self.sg.ap[r0:r0 + 64, h // 2, :], in0=self.sg.ap[r0:r0 + 64, h // 2, :],
                scalar=hv.ap[r0:r0 + 64, 9, h:h + 1], in1=psu.ap[r0:r0 + 64, h * 64:(h + 1) * 64],
                op0=ALU.mult, op1=ALU.add), reads=(self.sg, hv, psu), writes=(self.sg,))
        kb.op("act", lambda e: e.activation(out=self.sgb.ap, in_=self.sg.ap, func=AF.Copy),
              reads=(self.sg,), writes=(self.sgb,))
        sq2 = raw["ybuf"][:, 1024:1536]
        kb.op("act", lambda e: e.activation(out=sq2, in_=ob.ap, func=AF.Square), reads=(ob,), writes=(dg_t,))
        kb.op("dve", lambda e: e.reduce_sum(out=hs.ap[:, 2, 0:8], in_=sq2.rearrange("p (h c) -> p h c", h=8),
                                            axis=AX.X), reads=(dg_t,), writes=(hs,))
        kb.op("act", lambda e: e.activation(out=hs.ap[:, 2, 8:16], in_=hs.ap[:, 2, 0:8], func=AF.Sqrt,
                                            bias=float(RMS_EPS), scale=1.0 / 64.0), reads=(hs,), writes=(hs,))
        kb.op("dve", lambda e: e.reciprocal(out=hs.ap[:, 3, 0:8], in_=hs.ap[:, 2, 8:16]), reads=(hs,), writes=(hs,))
        kb.op("dve", lambda e: e.tensor_tensor(out=ob.ap.rearrange("p (h c) -> p h c", h=8),
                                               in0=ob.ap.rearrange("p (h c) -> p h c", h=8),
                                               in1=bcl(hs.ap[:, 3, 0:8], 8, 64), op=ALU.mult),
              reads=(ob, hs), writes=(ob,))
        kb.op("pool", lambda e: e.tensor_tensor(out=ob.ap.rearrange("p (h c) -> p h c", h=8),
                                                in0=ob.ap.rearrange("p (h c) -> p h c", h=8),
                                                in1=bcm(self.bp.ap[:, BP_G_NW:BP_G_NW + 64], 8, 64), op=ALU.mult),
              reads=(ob, self.bp), writes=(ob,))
        kb.op("dve", lambda e: e.tensor_tensor(out=self.ybf.ap[:, 0:512], in0=ob.ap, in1=gt.ap[:, 0:512], op=ALU.mult),
              reads=(ob, gt), writes=(self.ybf,))

        srcs = [(self.ybf, self.ybf.ap[:, q * 128:(q + 1) * 128]) for q in range(8)]
        self.transposes_to(srcs, tuple(self.hT[q] for q in range(8)), self.hT_full[:, 0:8, tc])

    def body(self):
        kb = self.kb
        self.prologue()
        for sc in range(self.nsc):
            if sc > 0:
                self.load_x(sc)
            for t in range(NT):
                self.to_xT(t)
            for l in self.layers:
                self.ffn_ln(l, "pre", sc, l * 3 + 0)
                if l % 2 == 1:
                    self.ssd_mixer(sc, l * 3 + 1)
                else:
                    self.hyb_mixer(sc, l * 3 + 1)
                self.ffn_ln(l, "post", sc, l * 3 + 2)
            self.store_y(sc)
        kb.finish()


def prep_inputs(inp, layers):
    ws = make_wstream(inp, layers)
    lnp = np.zeros((DEPTH * 3, 2, D_MODEL), np.float32)
    for l in range(DEPTH):
        lnp[l * 3 + 0, 0] = inp["ln_pre_g"][l]
        lnp[l * 3 + 0, 1] = inp["ln_pre_b"][l]
        lnp[l * 3 + 1, 0] = inp["ln_mix_g"][l]
        lnp[l * 3 + 1, 1] = inp["ln_mix_b"][l]
        lnp[l * 3 + 2, 0] = inp["ln_post_g"][l]
        lnp[l * 3 + 2, 1] = inp["ln_post_b"][l]
    j = np.arange(128)[:, None]
    i = np.arange(128)[None, :]
    cst = np.zeros((128, 5, 128), np.float32)
    cst[:, C_ID, :] = np.eye(128)
    cst[:, C_ONES, :] = 1.0
    cst[:, C_MINC, :] = np.where(j <= i, 0.0, -1e30)
    cst[:, C_MSTR, :] = np.where(j < i, 0.0, -1e30)
    cst[:, C_TRI, :] = (j <= i)
    bpar = np.zeros((NBP,), np.float32)
    bpar[BP_SSD_DTB:BP_SSD_DTB + 32] = inp["ssd_dt_bias"][0]
    bpar[BP_SSD_ALOG:BP_SSD_ALOG + 32] = inp["ssd_a_log"][0]
    bpar[BP_SSD_D:BP_SSD_D + 32] = inp["ssd_d_skip"][0]
    bpar[BP_G_ALOG:BP_G_ALOG + 8] = inp["gdn_a_log"][0]
    bpar[BP_G_DTB:BP_G_DTB + 8] = inp["gdn_dt_bias"][0]
    bpar[BP_G_NW:BP_G_NW + 64] = inp["gdn_norm_w"][0]
    bpar[BP_M_IB:BP_M_IB + 4] = inp["mlstm_i_bias"][0]
    bpar[BP_M_FB:BP_M_FB + 4] = inp["mlstm_f_bias"][0]
    bpar[BP_H_SGN:BP_H_SGN + 24] = np.array([-1] * 8 + [1] * 8 + [1] * 4 + [-1] * 4, np.float32)
    bpar[BP_H_BIAS + 8:BP_H_BIAS + 16] = inp["gdn_dt_bias"][0]
    bpar[BP_H_BIAS + 16:BP_H_BIAS + 20] = inp["mlstm_i_bias"][0]
    bpar[BP_H_BIAS + 20:BP_H_BIAS + 24] = inp["mlstm_f_bias"][0]
    convw = np.zeros((128, 36, 5), np.float32)
    hw = inp["hyb_conv_w"][0]
    convw[:, 0:12, 0:4] = hw.reshape(4, 12, 128).transpose(2, 1, 0)
    sw = inp["ssd_conv_w"][0]
    convw[:, 12:36, 0:4] = sw.reshape(4, 24, 128).transpose(2, 1, 0)
    convw[:, 12:36, 4] = inp["ssd_conv_b"][0].reshape(24, 128).T
    colp = np.ascontiguousarray(inp["ssd_norm_w"][0].reshape(16, 128).T)
    return ws, lnp, dict(cst=cst, bpar=bpar, convw=convw, colp=colp)


def kernel(**inputs):
    inp = {k: np.asarray(v) for k, v in inputs.items()}
    layers = list(range(DEPTH))
    ws, lnp, small = prep_inputs(inp, layers)
    warr = ws.array()
    prog = Prog(SEQ, layers, ws.n, ws.index)
    nc = prog.build()
    x = inp["x"]
    in_maps = []
    for b in range(BATCH):
        m = {"x": np.ascontiguousarray(x[b]), "wsrc": warr, "lnp": lnp}
        m.update(small)
        in_maps.append(m)
    res = run_bass_kernel_spmd(nc, in_maps, core_ids=list(range(BATCH)))
    out = np.stack([res.results[b]["y"] for b in range(BATCH)], axis=0)
    return out.astype(np.float32)
```
